# Optimizing a Trainium2 kernel written in Bass

```python
import jax, jax.numpy as jnp
from jax import lax
import numpy as np

D_MODEL = 1024
BATCH = 8
SEQ = 8192
DEPTH = 2

HEAD_DIM = 64
N_HEADS_A = 8
N_HEADS_B = 8
N_KV_B = 2
GQA_GROUP = N_HEADS_B // N_KV_B
MIX_A = N_HEADS_A * HEAD_DIM
MIX_B = N_HEADS_B * HEAD_DIM
KV_B = N_KV_B * HEAD_DIM
PROJ_WIDTH = 3 * MIX_A + MIX_B + 2 * KV_B
SPLITS = (MIX_A, 2 * MIX_A, 3 * MIX_A, 3 * MIX_A + MIX_B, 3 * MIX_A + MIX_B + KV_B)
DILATED_CONFIGS = ((128, 1), (512, 4), (2048, 16))
SWA_WINDOW = 128
BLOCK = 128
ROPE_THETA = 500000.0
ROPE_DIM = HEAD_DIM // 4
N_GROUPS = 4
EXPERTS_PER_GROUP = 8
N_EXPERTS = N_GROUPS * EXPERTS_PER_GROUP
TOP_K = 2
D_EXPERT = 512
EPS = 1e-6

kernel_name = 'hybrid_dilated_swa_sink_hmoe'


def rmsnorm(x, g):
    xf = x.astype(jnp.float32)
    y = xf * lax.rsqrt(jnp.mean(xf * xf, axis=-1, keepdims=True) + EPS)
    return (y * g.astype(jnp.float32)).astype(x.dtype)


def rope_tables(positions):
    inv_freq = ROPE_THETA ** (-jnp.arange(0, ROPE_DIM, 2, dtype=jnp.float32) / ROPE_DIM)
    ang = positions.astype(jnp.float32)[..., None] * inv_freq
    return jnp.cos(ang)[:, :, None, :], jnp.sin(ang)[:, :, None, :]


def apply_partial_rope(x, cos, sin):
    half = ROPE_DIM // 2
    x1 = x[..., :half].astype(jnp.float32)
    x2 = x[..., half:ROPE_DIM].astype(jnp.float32)
    rot = jnp.concatenate([x1 * cos - x2 * sin, x2 * cos + x1 * sin], axis=-1).astype(x.dtype)
    return jnp.concatenate([rot, x[..., ROPE_DIM:]], axis=-1)


def banded_attention(q, k, v, max_back, sink=None):
    n, hd = q.shape[-2], q.shape[-1]
    nb = -(-n // BLOCK)
    pad = nb * BLOCK - n
    if pad:
        q = jnp.pad(q, [(0, 0)] * (q.ndim - 2) + [(0, pad), (0, 0)])
        k = jnp.pad(k, [(0, 0)] * (k.ndim - 2) + [(0, pad), (0, 0)])
        v = jnp.pad(v, [(0, 0)] * (v.ndim - 2) + [(0, pad), (0, 0)])
    qb = q.reshape(q.shape[:-2] + (nb, BLOCK, hd))

    def with_prev(t):
        tb = t.reshape(t.shape[:-2] + (nb, BLOCK, hd))
        prev = jnp.concatenate([jnp.zeros_like(tb[..., :1, :, :]), tb[..., :-1, :, :]], axis=-3)
        return jnp.concatenate([prev, tb], axis=-2)

    kc, vc = with_prev(k), with_prev(v)
    s = jnp.einsum('...hgnqd,...hnkd->...hgnqk', qb, kc,
                   preferred_element_type=jnp.float32) * (hd ** -0.5)
    qi = jnp.arange(BLOCK)[:, None]
    kj = jnp.arange(2 * BLOCK)[None, :]
    dist = BLOCK + qi - kj
    kpos = (jnp.arange(nb)[:, None, None] - 1) * BLOCK + kj[None]
    valid = (dist >= 0) & (dist <= max_back) & (kpos >= 0)
    s = jnp.where(valid, s, -jnp.inf)
    m = jnp.max(s, axis=-1)
    if sink is not None:
        sk = sink.astype(jnp.float32)[:, :, None, None]
        m = jnp.maximum(m, sk)
    p = jnp.exp(s - m[..., None])
    denom = jnp.sum(p, axis=-1)
    if sink is not None:
        denom = denom + jnp.exp(sk - m)
    o = jnp.einsum('...hgnqk,...hnkd->...hgnqd', p, vc.astype(jnp.float32)) / denom[..., None]
    lse = m + jnp.log(denom)
    o = o.reshape(o.shape[:-3] + (nb * BLOCK, hd))[..., :n, :]
    lse = lse.reshape(lse.shape[:-2] + (nb * BLOCK,))[..., :n]
    return o.astype(q.dtype), lse


def dilated_attention(q, k, v):
    b, s, h, hd = q.shape
    outs, lses = [], []
    for window, dil in DILATED_CONFIGS:
        n = s // dil

        def split(t):
            return t.reshape(b, n, dil, h, hd).transpose(0, 2, 3, 1, 4)

        o, lse = banded_attention(split(q)[:, :, :, None], split(k), split(v), window // dil)
        outs.append(o[:, :, :, 0].transpose(0, 3, 1, 2, 4).reshape(b, s, h, hd))
        lses.append(lse[:, :, :, 0].transpose(0, 3, 1, 2).reshape(b, s, h))
    w = jax.nn.softmax(jnp.stack(lses, axis=0), axis=0)
    o = jnp.sum(w[..., None] * jnp.stack(outs, axis=0).astype(jnp.float32), axis=0)
    return o.astype(q.dtype)


def sink_swa_attention(q, k, v, sinks):
    b, s, _, hd = q.shape
    qg = q.reshape(b, s, N_KV_B, GQA_GROUP, hd).transpose(0, 2, 3, 1, 4)
    o, _ = banded_attention(qg, k.transpose(0, 2, 1, 3), v.transpose(0, 2, 1, 3),
                            SWA_WINDOW - 1, sink=sinks.reshape(N_KV_B, GQA_GROUP))
    return o.transpose(0, 3, 1, 2, 4).reshape(b, s, N_HEADS_B * hd)


def hierarchical_moe(h, w_rg, w_re, w_gu, w_dn):
    b, s, d = h.shape
    t = h.reshape(b * s, d)
    g_prob = jax.nn.softmax(jnp.einsum('td,dg->tg', t, w_rg).astype(jnp.float32), axis=-1)
    g_top, g_idx = lax.top_k(g_prob, 1)
    e_logits = jnp.einsum('td,de->te', t, w_re).astype(jnp.float32)
    e_logits = e_logits.reshape(-1, N_GROUPS, EXPERTS_PER_GROUP)
    e_logits = jnp.einsum('tge,tg->te', e_logits,
                          jax.nn.one_hot(g_idx[:, 0], N_GROUPS, dtype=jnp.float32))
    e_top, e_idx = lax.top_k(jax.nn.softmax(e_logits, axis=-1), TOP_K)
    gate = g_top * e_top / jnp.sum(e_top, axis=-1, keepdims=True)
    flat = (g_idx * EXPERTS_PER_GROUP + e_idx).reshape(-1)
    order = jnp.argsort(flat)
    xs = t[order // TOP_K]
    sizes = jnp.bincount(flat, length=N_EXPERTS).astype(jnp.int32)
    gu = lax.ragged_dot(xs, w_gu, sizes)
    a = jax.nn.silu(gu[:, :D_EXPERT]) * gu[:, D_EXPERT:]
    ys = lax.ragged_dot(a, w_dn, sizes)
    ys = ys[jnp.argsort(order)].reshape(-1, TOP_K, d)
    y = jnp.einsum('tkd,tk->td', ys.astype(jnp.float32), gate)
    return y.astype(h.dtype).reshape(b, s, d)


def setup_inputs(seed: int = 0) -> dict:
    key = jax.random.key(seed)
    ks = jax.random.split(key, 20)
    f32 = jnp.float32
    L, D = DEPTH, D_MODEL

    def gain(k, shape):
        return 1.0 + 0.02 * jax.random.normal(k, shape, f32)

    x = jax.random.normal(ks[0], (BATCH, SEQ, D), f32)
    offset = jax.random.randint(ks[1], (BATCH, 1), 0, 4096, dtype=jnp.int32)
    positions = (jnp.arange(SEQ, dtype=jnp.int32)[None, :] + offset).astype(jnp.int32)
    return {
        'x': x,
        'positions': positions,
        'attn_norm': gain(ks[2], (L, D)),
        'w_in': jax.random.normal(ks[3], (L, D, PROJ_WIDTH), f32) * D ** -0.5,
        'q_norm_a': gain(ks[4], (L, HEAD_DIM)),
        'k_norm_a': gain(ks[5], (L, HEAD_DIM)),
        'q_norm_b': gain(ks[6], (L, HEAD_DIM)),
        'k_norm_b': gain(ks[7], (L, HEAD_DIM)),
        'sinks_b': 0.5 * jax.random.normal(ks[8], (L, N_HEADS_B), f32),
        'out_norm_a': gain(ks[9], (L, MIX_A)),
        'out_norm_b': gain(ks[10], (L, MIX_B)),
        'w_out': jax.random.normal(ks[11], (L, MIX_A + MIX_B, D), f32) * (MIX_A + MIX_B) ** -0.5,
        'ffn_norm': gain(ks[12], (L, D)),
        'w_router_group': jax.random.normal(ks[13], (L, D, N_GROUPS), f32) * D ** -0.5,
        'w_router_expert': jax.random.normal(ks[14], (L, D, N_EXPERTS), f32) * D ** -0.5,
        'w_gate_up': jax.random.normal(ks[15], (L, N_EXPERTS, D, 2 * D_EXPERT), f32) * D ** -0.5,
        'w_down': jax.random.normal(ks[16], (L, N_EXPERTS, D_EXPERT, D), f32) * D_EXPERT ** -0.5,
    }


def reference(x, positions, attn_norm, w_in, q_norm_a, k_norm_a, q_norm_b, k_norm_b, sinks_b,
              out_norm_a, out_norm_b, w_out, ffn_norm, w_router_group, w_router_expert,
              w_gate_up, w_down):
    b, s, _ = x.shape
    cos, sin = rope_tables(positions)
    for l in range(DEPTH):
        h = rmsnorm(x, attn_norm[l])
        proj = jnp.einsum('bsd,dp->bsp', h, w_in[l])
        qa, ka, va, qb, kb, vb = jnp.split(proj, SPLITS, axis=-1)
        qa = qa.reshape(b, s, N_HEADS_A, HEAD_DIM)
        ka = ka.reshape(b, s, N_HEADS_A, HEAD_DIM)
        va = va.reshape(b, s, N_HEADS_A, HEAD_DIM)
        qb = qb.reshape(b, s, N_HEADS_B, HEAD_DIM)
        kb = kb.reshape(b, s, N_KV_B, HEAD_DIM)
        vb = vb.reshape(b, s, N_KV_B, HEAD_DIM)
        qa = apply_partial_rope(rmsnorm(qa, q_norm_a[l]), cos, sin)
        ka = apply_partial_rope(rmsnorm(ka, k_norm_a[l]), cos, sin)
        qb = apply_partial_rope(rmsnorm(qb, q_norm_b[l]), cos, sin)
        kb = apply_partial_rope(rmsnorm(kb, k_norm_b[l]), cos, sin)
        oa = dilated_attention(qa, ka, va).reshape(b, s, MIX_A)
        ob = sink_swa_attention(qb, kb, vb, sinks_b[l])
        mixed = jnp.concatenate([rmsnorm(oa, out_norm_a[l]), rmsnorm(ob, out_norm_b[l])], axis=-1)
        x = x + jnp.einsum('bsm,md->bsd', mixed, w_out[l])
        h = rmsnorm(x, ffn_norm[l])
        x = x + hierarchical_moe(h, w_router_group[l], w_router_expert[l], w_gate_up[l], w_down[l])
    return x
```

```python
import numpy as np
from contextlib import ExitStack
import concourse.bass as bass
import concourse.mybir as mybir
from concourse.bass_utils import run_bass_kernel_spmd

F32 = mybir.dt.float32
BF16 = mybir.dt.bfloat16
I32 = mybir.dt.int32
ALU = mybir.AluOpType
AF = mybir.ActivationFunctionType
AX = mybir.AxisListType

D = 1024
PW = 2304
NH = 8
HD = 64
NE = 32
FE = 512
VW = 80
EPS = 1e-6
TWO_PI_HI = 6.28125
TWO_PI_LO = 2.0 * np.pi - 6.28125


class Buf:
    __slots__ = ("name", "w", "r", "excl")

    def __init__(self, name):
        self.name = name
        self.w = None
        self.r = {}
        self.excl = False


class T:
    def __init__(self, t, name):
        self.t = t
        self.b = Buf(name)


class Prog:
    def __init__(self, nc, es, n_dsem=22):
        self.nc = nc
        self.es = es
        self.eng = dict(pe=nc.tensor, act=nc.scalar, dve=nc.vector, pool=nc.gpsimd, sp=nc.sync)
        self.sem = {e: es.enter_context(nc.semaphore("s_" + e)) for e in ("pe", "act", "dve", "pool")}
        self.cnt = {e: 0 for e in self.sem}
        self.dsem = [es.enter_context(nc.semaphore("d%d" % i)) for i in range(n_dsem)]
        self.dcnt = [0] * n_dsem
        self.dnext = {"hw": 0, "sw": 0}
        self.n_sw = 6
        self.seen = {e: {} for e in self.eng}

    def _semof(self, key):
        return self.sem[key] if isinstance(key, str) else self.dsem[key[1]]

    def _wait(self, e, tok):
        if tok is None:
            return
        key, val = tok
        if key == e and e == "pe":
            return
        if self.seen[e].get(key, 0) >= val:
            return
        self.eng[e].wait_ge(self._semof(key), val)
        self.seen[e][key] = val

    def _deps(self, e, reads, writes):
        for b in reads:
            self._wait(e, b.w)
        for b in writes:
            self._wait(e, b.w)
            for k, v in b.r.items():
                self._wait(e, (k, v))

    def _record(self, tok, reads, writes):
        key, val = tok
        for b in reads:
            if b.r.get(key, 0) < val:
                b.r[key] = val
        for b in writes:
            b.w = tok
            b.r = {}

    def op(self, e, fn, reads=(), writes=()):
        reads = [x.b if isinstance(x, T) else x for x in reads]
        writes = [x.b if isinstance(x, T) else x for x in writes]
        writes = writes + [b for b in reads if b.excl]
        reads = [b for b in reads if not b.excl]
        self._deps(e, reads, writes)
        ins = fn(self.eng[e])
        self.cnt[e] += 1
        ins.then_inc(self.sem[e], 1)
        self._record((e, self.cnt[e]), reads, writes)

    def dma(self, q, out, in_, reads=(), writes=()):
        reads = [x.b if isinstance(x, T) else x for x in reads]
        writes = [x.b if isinstance(x, T) else x for x in writes]
        if q == "pool":
            i = self.dnext["sw"]
            self.dnext["sw"] = (i + 1) % self.n_sw
        else:
            i = self.n_sw + self.dnext["hw"]
            self.dnext["hw"] = (self.dnext["hw"] + 1) % (len(self.dsem) - self.n_sw)
        if self.dcnt[i]:
            self._wait(q, (("d", i), self.dcnt[i]))
        self._deps(q, reads, writes)
        ins = self.eng[q].dma_start(out=out, in_=in_)
        self.dcnt[i] += 16
        ins.then_inc(self.dsem[i], 16)
        self._record((("d", i), self.dcnt[i]), reads, writes)

    def barrier(self):
        for e in self.eng:
            for o in self.sem:
                if o != e and self.cnt[o]:
                    self._wait(e, (o, self.cnt[o]))
            for i, c in enumerate(self.dcnt):
                if c:
                    self._wait(e, (("d", i), c))

    def final_wait(self):
        for i, c in enumerate(self.dcnt):
            if c:
                self._wait("sp", (("d", i), c))


class Ring:
    def __init__(self, tiles):
        self.tiles = tiles
        self.i = 0

    def next(self):
        t = self.tiles[self.i % len(self.tiles)]
        self.i += 1
        return t


LIMIT_NT = None
DBG = dict(c_stop=9, b_passes=None, b_blocks=None, act_dma=False, split_exp=True)


def build(S, L, n_exp=NE, phases="ABCD", dbg=False):
    NT = S // 128
    NTL = NT if LIMIT_NT is None else min(NT, LIMIT_NT)
    TS = min(2048, S)
    nc = bass.Bass("TRN2", target_bir_lowering=False)
    dram = lambda name, shape, dt, kind="ExternalInput": nc.dram_tensor(name, shape, dt, kind=kind).ap()
    x_in = dram("x", [S, D], F32)
    pos_t = dram("pos_t", [128, NT], I32)
    c_ident = dram("c_ident", [128, 128], F32)
    c_mask = dram("c_mask", [128, 4, 128], F32)
    c_invf = dram("c_invf", [128, 8], F32)
    gvec = dram("gvec", [L, 128, 3, D], F32)
    gqk = dram("gqk", [L, 128, 26 * HD], F32)
    sinks = dram("sinks", [L, 128, NH], F32)
    w_in = dram("w_in", [L, D, PW], F32)
    w_out = dram("w_out", [L, D, D], F32)
    w_r = dram("w_r", [L, D, 36], F32)
    w_gu = dram("w_gu", [L, NE, D, 2 * FE], F32)
    w_dn = dram("w_dn", [L, NE, FE, D], F32)
    out = dram("out", [S, D], F32, kind="ExternalOutput")
    okind = "ExternalOutput" if dbg else "Internal"
    QKV = dram("QKV", [S, PW], BF16, kind=okind)
    ACC = dram("ACC", [4, S, NH * 65], F32, kind=okind)
    XM = dram("XM", [S, D], F32, kind=okind)
    H2T = dram("H2T", [D, S], BF16, kind=okind)
    GS = dram("GS", [S, NE], F32, kind=okind)
    X1 = dram("X1", [S, D], F32, kind=okind)

    with ExitStack() as es:
        P = Prog(nc, es)

        uid = [0]

        def sb(stack, name, shape, dt):
            uid[0] += 1
            name = "%s_%d" % (name, uid[0])
            return T(stack.enter_context(nc.sbuf_tensor(name, shape, dt)), name)

        def ps(stack, name, shape, dt):
            uid[0] += 1
            name = "%s_%d" % (name, uid[0])
            nfull = 512 if dt == F32 else 1024
            full = stack.enter_context(nc.psum_tensor(name, [128, nfull], dt))
            n = int(np.prod(shape[1:]))
            assert n <= nfull, (name, shape)
            v = full[:, 0:n]
            if len(shape) == 3:
                v = v.rearrange("p (a b) -> p a b", a=shape[1], b=shape[2])
            elif len(shape) == 4:
                v = v.rearrange("p (a b c) -> p a b c", a=shape[1], b=shape[2], c=shape[3])
            t = T(v, name)
            t.b.excl = True
            return t

        ident_f = sb(es, "ident_f", [128, 128], F32)
        ident_b = sb(es, "ident_b", [128, 128], BF16)
        mask_f = sb(es, "mask_f", [128, 4, 128], F32)
        mask_b = sb(es, "mask_b", [128, 4, 128], BF16)
        invf = sb(es, "invf", [128, 8], F32)
        cos_t = sb(es, "cos_t", [128, NT, 8], F32)
        sin_t = sb(es, "sin_t", [128, NT, 8], F32)
        P.dma("sp", ident_f.t[:], c_ident[:, :], writes=[ident_f])
        P.dma("sp", mask_f.t[:], c_mask[:, :, :], writes=[mask_f])
        P.dma("sp", invf.t[:], c_invf[:, :], writes=[invf])
        P.op("dve", lambda e: e.tensor_copy(out=ident_b.t[:], in_=ident_f.t[:]), [ident_f], [ident_b])
        P.op("dve", lambda e: e.tensor_copy(out=mask_b.t[:], in_=mask_f.t[:]), [mask_f], [mask_b])
        with ExitStack() as s0:
            pos_i = sb(s0, "pos_i", [128, NT], I32)
            pos_f = sb(s0, "pos_f", [128, NT], F32)
            ang = sb(s0, "ang", [128, NT, 8], F32)
            ang2 = sb(s0, "ang2", [128, NT, 8], F32)
            kf = sb(s0, "kf", [128, NT, 8], F32)
            ki = sb(s0, "ki", [128, NT, 8], I32)
            P.dma("sp", pos_i.t[:], pos_t[:, :], writes=[pos_i])
            P.op("dve", lambda e: e.tensor_copy(out=pos_f.t[:], in_=pos_i.t[:]), [pos_i], [pos_f])
            P.op("dve", lambda e: e.tensor_tensor(
                out=ang.t[:], in0=pos_f.t[:].unsqueeze(2).to_broadcast([128, NT, 8]),
                in1=invf.t[:].unsqueeze(1).to_broadcast([128, NT, 8]), op=ALU.mult), [pos_f, invf], [ang])
            for which, dst in ((0, sin_t), (1, cos_t)):
                src = ang
                if which == 1:
                    P.op("dve", lambda e: e.tensor_scalar(out=ang2.t[:], in0=ang.t[:], scalar1=float(np.pi / 2),
                                                          scalar2=None, op0=ALU.add), [ang], [ang2])
                    src = ang2
                P.op("dve", lambda e: e.tensor_scalar(out=kf.t[:], in0=src.t[:], scalar1=float(1.0 / (2 * np.pi)),
                                                      scalar2=None, op0=ALU.mult), [src], [kf])
                P.op("dve", lambda e: e.tensor_copy(out=ki.t[:], in_=kf.t[:]), [kf], [ki])
                P.op("dve", lambda e: e.tensor_copy(out=kf.t[:], in_=ki.t[:]), [ki], [kf])
                P.op("dve", lambda e: e.scalar_tensor_tensor(out=dst.t[:], in0=kf.t[:], scalar=-TWO_PI_HI, in1=src.t[:],
                                                             op0=ALU.mult, op1=ALU.add), [kf, src], [dst])
                P.op("dve", lambda e: e.scalar_tensor_tensor(out=dst.t[:], in0=kf.t[:], scalar=-TWO_PI_LO, in1=dst.t[:],
                                                             op0=ALU.mult, op1=ALU.add), [kf, dst], [dst])
                P.op("dve", lambda e: e.tensor_scalar(out=dst.t[:], in0=dst.t[:], scalar1=float(np.pi), scalar2=float(-np.pi),
                                                      op0=ALU.min, op1=ALU.max), [dst], [dst])
                P.op("act", lambda e: e.activation(out=dst.t[:], in_=dst.t[:], func=AF.Sin), [dst], [dst])
            P.barrier()

        def rstd_from_ss(ss_ap, n_feat, out_t, tmp_t, deps_r):
            P.op("dve", lambda e: e.tensor_scalar(out=tmp_t.t[:], in0=ss_ap, scalar1=1.0 / n_feat, scalar2=EPS,
                                                  op0=ALU.mult, op1=ALU.add), deps_r, [tmp_t])
            P.op("act", lambda e: e.activation(out=tmp_t.t[:], in_=tmp_t.t[:], func=AF.Sqrt), [tmp_t], [tmp_t])
            P.op("dve", lambda e: e.reciprocal(out=out_t.t[:], in_=tmp_t.t[:]), [tmp_t], [out_t])

        for l in range(L):
            xsrc = x_in if l == 0 else X1
            xdst = out if l == L - 1 else X1
            if "A" in phases:
              with ExitStack() as sa:
                win = sb(sa, "win", [128, 8, PW], BF16)
                g_attn = sb(sa, "g_attn", [128, D], F32)
                g_qk = sb(sa, "g_qk", [128, 26, HD], F32)
                for c in range(8):
                    P.dma("pool", win.t[:, c, :], w_in[l, c * 128:(c + 1) * 128, :], writes=[win])
                P.dma("sp", g_attn.t[:], gvec[l, :, 0, :], writes=[g_attn])
                P.dma("sp", g_qk.t[:].rearrange("p h d -> p (h d)"), gqk[l, :, :], writes=[g_qk])
                xt_r = Ring([sb(sa, "xt%d" % i, [128, D], F32) for i in range(2)])
                junk = sb(sa, "junkA", [128, D], BF16)
                xg_r = Ring([sb(sa, "xg%d" % i, [128, D], BF16) for i in range(2)])
                xT_r = Ring([sb(sa, "xT%d" % i, [128, 8, 128], BF16) for i in range(2)])
                ss_r = Ring([sb(sa, "ssA%d" % i, [128, 1], F32) for i in range(2)])
                rs_r = Ring([sb(sa, "rsA%d" % i, [128, 1], F32) for i in range(2)])
                tm_r = Ring([sb(sa, "tmA%d" % i, [128, 1], F32) for i in range(2)])
                proj_r = Ring([sb(sa, "proj%d" % i, [128, PW], F32) for i in range(2)])
                sq = sb(sa, "sqA", [128, 26, HD], F32)
                ssh = sb(sa, "ssh", [128, 26], F32)
                rsh = sb(sa, "rsh", [128, 26], F32)
                tmh = sb(sa, "tmh", [128, 26], F32)
                r1 = sb(sa, "r1", [128, 26, 8], F32)
                r2 = sb(sa, "r2", [128, 26, 8], F32)
                r3 = sb(sa, "r3", [128, 26, 8], F32)
                r4 = sb(sa, "r4", [128, 26, 8], F32)
                qkvb_r = Ring([sb(sa, "qkvb%d" % i, [128, PW], BF16) for i in range(2)])
                pT_r = Ring([ps(sa, "pTA%d" % i, [128, 8, 128], BF16) for i in range(2)])
                pP_r = [Ring([ps(sa, "pP%d_%d" % (g, i), [128, 512], F32) for i in range(1)]) for g in range(5)]
                for i in range(NTL):
                    xt = xt_r.next(); xg = xg_r.next(); xT = xT_r.next(); ss = ss_r.next(); rs = rs_r.next()
                    tm = tm_r.next(); proj = proj_r.next(); qkvb = qkvb_r.next(); pT = pT_r.next()
                    P.dma("sp", xt.t[:], xsrc[i * 128:(i + 1) * 128, :], writes=[xt])
                    P.op("act", lambda e: e.activation(out=junk.t[:], in_=xt.t[:], func=AF.Square, accum_out=ss.t[:]),
                         [xt], [junk, ss])
                    P.op("dve", lambda e: e.tensor_tensor(out=xg.t[:], in0=xt.t[:], in1=g_attn.t[:], op=ALU.mult),
                         [xt, g_attn], [xg])

                    def tr(e):
                        for c in range(8):
                            ins = e.transpose(pT.t[:, c, :], xg.t[:, c * 128:(c + 1) * 128], ident_b.t[:])
                        return ins
                    P.op("pe", tr, [xg, ident_b], [pT])
                    P.op("act", lambda e: e.copy(out=xT.t[:], in_=pT.t[:]), [pT], [xT])
                    rstd_from_ss(ss.t[:], D, rs, tm, [ss])
                    for g in range(5):
                        w = 512 if g < 4 else 256
                        pP = pP_r[g].next()

                        def mm(e):
                            for c in range(8):
                                ins = e.matmul(pP.t[:, 0:w], lhsT=xT.t[:, c, :], rhs=win.t[:, c, g * 512:g * 512 + w],
                                               start=(c == 0), stop=(c == 7))
                            return ins
                        P.op("pe", mm, [xT, win], [pP])
                        eng = "act" if g % 2 == 0 else "dve"
                        if eng == "act":
                            P.op("act", lambda e: e.activation(out=proj.t[:, g * 512:g * 512 + w], in_=pP.t[:, 0:w],
                                                               func=AF.Copy, scale=rs.t[:, 0:1]), [pP, rs], [proj])
                        else:
                            P.op("dve", lambda e: e.tensor_scalar(out=proj.t[:, g * 512:g * 512 + w], in0=pP.t[:, 0:w],
                                                                  scalar1=rs.t[:, 0:1], scalar2=None, op0=ALU.mult),
                                 [pP, rs], [proj])
                    qk = proj.t[:, 0:26 * HD].rearrange("p (h d) -> p h d", d=HD)
                    P.op("pool", lambda e: e.tensor_tensor(out=sq.t[:], in0=qk, in1=qk, op=ALU.mult), [proj], [sq])
                    P.op("dve", lambda e: e.tensor_reduce(out=ssh.t[:], in_=sq.t[:], axis=AX.X, op=ALU.add), [sq], [ssh])
                    rstd_from_ss(ssh.t[:], HD, rsh, tmh, [ssh])
                    P.op("dve", lambda e: e.tensor_tensor(out=sq.t[:], in0=qk,
                                                          in1=rsh.t[:].unsqueeze(2).to_broadcast([128, 26, HD]),
                                                          op=ALU.mult), [proj, rsh], [sq])
                    P.op("pool", lambda e: e.tensor_tensor(out=sq.t[:], in0=sq.t[:], in1=g_qk.t[:], op=ALU.mult),
                         [sq, g_qk], [sq])
                    P.op("act", lambda e: e.copy(out=qkvb.t[:, 0:26 * HD], in_=sq.t[:].rearrange("p h d -> p (h d)")),
                         [sq], [qkvb])
                    P.op("pool", lambda e: e.tensor_copy(out=qkvb.t[:, 26 * HD:PW], in_=proj.t[:, 26 * HD:PW]),
                         [proj], [qkvb])
                    cb = cos_t.t[:, i, :].unsqueeze(1).to_broadcast([128, 26, 8])
                    sn = sin_t.t[:, i, :].unsqueeze(1).to_broadcast([128, 26, 8])
                    x1 = sq.t[:, :, 0:8]
                    x2 = sq.t[:, :, 8:16]
                    P.op("dve", lambda e: e.tensor_tensor(out=r1.t[:], in0=x1, in1=cb, op=ALU.mult), [sq, cos_t], [r1])
                    P.op("dve", lambda e: e.tensor_tensor(out=r2.t[:], in0=x2, in1=sn, op=ALU.mult), [sq, sin_t], [r2])
                    P.op("pool", lambda e: e.tensor_tensor(out=r3.t[:], in0=x2, in1=cb, op=ALU.mult), [sq, cos_t], [r3])
                    P.op("pool", lambda e: e.tensor_tensor(out=r4.t[:], in0=x1, in1=sn, op=ALU.mult), [sq, sin_t], [r4])
                    qv = qkvb.t[:, 0:26 * HD].rearrange("p (h d) -> p h d", d=HD)
                    P.op("dve", lambda e: e.tensor_tensor(out=qv[:, :, 0:8], in0=r1.t[:], in1=r2.t[:], op=ALU.subtract),
                         [r1, r2], [qkvb])
                    P.op("dve", lambda e: e.tensor_tensor(out=qv[:, :, 8:16], in0=r3.t[:], in1=r4.t[:], op=ALU.add),
                         [r3, r4], [qkvb])
                    P.dma("sp", QKV[i * 128:(i + 1) * 128, :], qkvb.t[:], reads=[qkvb])
                P.barrier()

            if "B" in phases:
              with ExitStack() as sbk:
                qrow_r = Ring([sb(sbk, "qrow%d" % i, [128, 512], BF16) for i in range(2)])
                krow_r = Ring([sb(sbk, "krow%d" % i, [128, 512], BF16) for i in range(2)])
                kdup_r = Ring([sb(sbk, "kdup%d" % i, [128, 2, 2, HD], BF16) for i in range(2)])
                vaug_r = Ring([sb(sbk, "vaug%d" % i, [128, NH, VW], BF16) for i in range(3)])
                for v in vaug_r.tiles:
                    P.op("pool", lambda e: e.memset(v.t[:], 1.0), [], [v])
                qT_r = Ring([sb(sbk, "qT%d" % i, [128, 4, 2, 128], BF16) for i in range(2)])
                for v in qT_r.tiles:
                    P.op("pool", lambda e: e.memset(v.t[:], 0.0), [], [v])
                kT_r = Ring([sb(sbk, "kT%d" % i, [128, 4, 128], BF16) for i in range(3)])
                pt_r = Ring([sb(sbk, "pt%d" % i, [128, 4, 2, 128], BF16) for i in range(3)])
                osb_r = Ring([sb(sbk, "osb%d" % i, [128, NH, 65], F32) for i in range(2)])
                pTq = ps(sbk, "pTq", [128, 4, 128], BF16)
                pTk = ps(sbk, "pTk", [128, 4, 128], BF16)
                pS_r = Ring([(ps(sbk, "pSa%d" % i, [128, 2, 2, 128], F32), ps(sbk, "pSb%d" % i, [128, 2, 2, 128], F32)) for i in range(2)])
                pO_r = Ring([ps(sbk, "pO%d" % i, [128, 4, VW], F32) for i in range(2)])
                passes = [(0, 1, r) for r in range(1)] + [(1, 4, r) for r in range(4)] + \
                         [(2, 16, r) for r in range(16)] + [(3, 1, 0)]
                if DBG['b_passes'] is not None:
                    passes = passes[:DBG['b_passes']]
                for (pi, d, r) in passes:
                    isB = (pi == 3)
                    nb = S // d // 128
                    if DBG['b_blocks'] is not None:
                        nb = min(nb, DBG['b_blocks'])
                    qc0 = 512 if isB else 0
                    kc0, kw = (1536, 128) if isB else (1024, 512)
                    vc0, nvh = (2176, 2) if isB else (1664, 8)
                    rows = QKV.rearrange("(m d) c -> d m c", d=d)
                    accv = ACC.rearrange("a (m d) c -> a d m c", d=d)
                    prev = None
                    for j in range(nb):
                        qrow = qrow_r.next(); krow = krow_r.next(); vaug = vaug_r.next(); qT = qT_r.next()
                        kT = kT_r.next(); osb = osb_r.next()
                        rs_ = slice(j * 128, (j + 1) * 128)
                        P.dma("sp", qrow.t[:], rows[r, rs_, qc0:qc0 + 512], writes=[qrow])
                        P.dma("sp", krow.t[:, 0:kw], rows[r, rs_, kc0:kc0 + kw], writes=[krow])
                        P.dma("act" if DBG['act_dma'] else "sp", vaug.t[:, 0:nvh, 0:HD],
                              rows[r, rs_, vc0:vc0 + nvh * HD].rearrange("p (h d) -> p h d", d=HD), writes=[vaug])

                        def trq(e):
                            for c in range(4):
                                ins = e.transpose(pTq.t[:, c, :], qrow.t[:, c * 128:(c + 1) * 128], ident_b.t[:])
                            return ins
                        P.op("pe", trq, [qrow, ident_b], [pTq])
                        P.op("dve", lambda e: e.tensor_copy(out=qT.t[0:64, :, 0, :], in_=pTq.t[0:64, :, :]), [pTq], [qT])
                        P.op("dve", lambda e: e.tensor_copy(out=qT.t[64:128, :, 1, :], in_=pTq.t[64:128, :, :]), [pTq], [qT])
                        if isB:
                            kdup = kdup_r.next()
                            kv = krow.t[:, 0:128].rearrange("p (g d) -> p g d", d=HD)
                            P.op("pool", lambda e: e.tensor_copy(
                                out=kdup.t[:], in_=kv.unsqueeze(2).to_broadcast([128, 2, 2, HD])), [krow], [kdup])

                            def trk(e):
                                for c in range(2):
                                    ins = e.transpose(pTk.t[:, c, :], kdup.t[:, c, :, :].rearrange("p a d -> p (a d)"),
                                                      ident_b.t[:])
                                return ins
                            P.op("pe", trk, [kdup, ident_b], [pTk])
                            P.op("act", lambda e: e.copy(out=kT.t[:, 0:2, :], in_=pTk.t[:, 0:2, :]), [pTk], [kT])
                        else:
                            def trk(e):
                                for c in range(4):
                                    ins = e.transpose(pTk.t[:, c, :], krow.t[:, c * 128:(c + 1) * 128], ident_b.t[:])
                                return ins
                            P.op("pe", trk, [krow, ident_b], [pTk])
                            P.op("act", lambda e: e.copy(out=kT.t[:], in_=pTk.t[:]), [pTk], [kT])
                        for hg in range(2):
                            pS = pS_r.next(); pO = pO_r.next(); pt = pt_r.next()
                            s0 = 0 if prev is not None else 1

                            def kslice(kt, h):
                                if isB:
                                    return kt.t[:, h // 4, :]
                                return kt.t[:, h // 2, :]

                            def sc(e):
                                for hh in range(4):
                                    h = hg * 4 + hh
                                    qs = qT.t[:, h // 2, h % 2, :]
                                    pSh = pS[hh // 2]
                                    if prev is not None:
                                        e.matmul(pSh.t[:, hh % 2, 0, :], lhsT=kslice(prev[0], h), rhs=qs, start=True, stop=True)
                                    ins = e.matmul(pSh.t[:, hh % 2, 1, :], lhsT=kslice(kT, h), rhs=qs, start=True, stop=True)
                                return ins
                            rd = [qT, kT] + ([prev[0]] if prev is not None else [])
                            P.op("pe", sc, rd, [pS[0], pS[1]])
                            for hh2 in range(2):
                                P.op("act", lambda e: e.activation(
                                    out=pt.t[:, 2 * hh2:2 * hh2 + 2, s0:2, :], in_=pS[hh2].t[:, :, s0:2, :],
                                    func=AF.Exp, scale=0.125), [pS[hh2]], [pt])
                            mk = mask_b.t[:, 2 * (1 if isB else 0) + s0:2 * (1 if isB else 0) + 2, :]
                            meng = "dve" if hg == 0 else "pool"
                            P.op(meng, lambda e: e.tensor_tensor(
                                out=pt.t[:, :, s0:2, :], in0=pt.t[:, :, s0:2, :],
                                in1=mk.unsqueeze(1).to_broadcast([128, 4, 2 - s0, 128]), op=ALU.mult), [pt, mask_b], [pt])

                            def pv(e):
                                for hh in range(4):
                                    h = hg * 4 + hh
                                    vh = (h // 4) if isB else h
                                    if prev is not None:
                                        e.matmul(pO.t[:, hh, :], lhsT=pt.t[:, hh, 0, :], rhs=prev[1].t[:, vh, :],
                                                 start=True, stop=False)
                                    ins = e.matmul(pO.t[:, hh, :], lhsT=pt.t[:, hh, 1, :], rhs=vaug.t[:, vh, :],
                                                   start=(prev is None), stop=True)
                                return ins
                            rd = [pt, vaug] + ([prev[1]] if prev is not None else [])
                            P.op("pe", pv, rd, [pO])
                            if hg == 0:
                                P.op("act", lambda e: e.copy(out=osb.t[:, 0:4, :], in_=pO.t[:, :, 0:65]), [pO], [osb])
                            else:
                                P.op("dve", lambda e: e.tensor_copy(out=osb.t[:, 4:8, :], in_=pO.t[:, :, 0:65]), [pO], [osb])
                        P.dma("sp", accv[pi, r, rs_, :], osb.t[:].rearrange("p h d -> p (h d)"), reads=[osb])
                        prev = (kT, vaug)
                P.barrier()

            if "C" in phases:
              with ExitStack() as sc_:
                wout = sb(sc_, "wout", [128, 8, D], BF16)
                wr = sb(sc_, "wr", [128, 8, 36], F32)
                g_out = sb(sc_, "g_out", [128, D], F32)
                g_ffn = sb(sc_, "g_ffn", [128, D], F32)
                esink = sb(sc_, "esink", [128, NH], F32)
                for c in range(8):
                    P.dma("pool", wout.t[:, c, :], w_out[l, c * 128:(c + 1) * 128, :], writes=[wout])
                    P.dma("sp", wr.t[:, c, :], w_r[l, c * 128:(c + 1) * 128, :], writes=[wr])
                P.dma("sp", g_out.t[:], gvec[l, :, 1, :], writes=[g_out])
                P.dma("sp", g_ffn.t[:], gvec[l, :, 2, :], writes=[g_ffn])
                P.dma("sp", esink.t[:], sinks[l, :, :], writes=[esink])
                P.op("act", lambda e: e.activation(out=esink.t[:], in_=esink.t[:], func=AF.Exp), [esink], [esink])
                acc_r = Ring([sb(sc_, "acc%d" % i, [128, 4, NH, 65], F32) for i in range(2)])
                xt_r = Ring([sb(sc_, "xtC%d" % i, [128, D], F32) for i in range(2)])
                den = sb(sc_, "den", [128, 2, NH], F32)
                o_t = sb(sc_, "o_t", [128, 2, NH, HD], F32)
                junk = sb(sc_, "junkC", [128, D], BF16)
                ss2 = sb(sc_, "ss2", [128, 2], F32)
                rs2 = sb(sc_, "rs2", [128, 2], F32)
                tm2 = sb(sc_, "tm2", [128, 2], F32)
                mix = sb(sc_, "mix", [128, D], BF16)
                mT = sb(sc_, "mT", [128, 8, 128], BF16)
                xm_r = Ring([sb(sc_, "xm%d" % i, [128, D], F32) for i in range(2)])
                ss3 = sb(sc_, "ss3", [128, 1], F32)
                rs3 = sb(sc_, "rs3", [128, 1], F32)
                tm3 = sb(sc_, "tm3", [128, 1], F32)
                h2 = sb(sc_, "h2", [128, D], F32)
                h2Tf = sb(sc_, "h2Tf", [128, 8, 128], F32)
                h2Tb_r = Ring([sb(sc_, "h2Tb%d" % i, [128, 8, 128], BF16) for i in range(2)])
                lg = sb(sc_, "lg", [128, 36], F32)
                gmx = sb(sc_, "gmx", [128, 8], F32)
                gm = sb(sc_, "gm", [128, 4], F32)
                ex4 = sb(sc_, "ex4", [128, 4], F32)
                lem = sb(sc_, "lem", [128, 4, 8], F32)
                m1k = sb(sc_, "m1k", [128, 32], F32)
                m2k = sb(sc_, "m2k", [128, 32], F32)
                G_r = Ring([sb(sc_, "G%d" % i, [128, NE], F32) for i in range(2)])
                pTm = ps(sc_, "pTm", [128, 8, 128], BF16)
                pOa = ps(sc_, "pOa", [128, 512], F32)
                pOb = ps(sc_, "pOb", [128, 512], F32)
                pTh = [ps(sc_, "pTh%d" % i, [128, 4, 128], F32) for i in range(2)]
                pR = ps(sc_, "pR", [128, 36], F32)
                for i in range(NTL):
                    acc = acc_r.next(); xt = xt_r.next(); xm = xm_r.next(); h2Tb = h2Tb_r.next(); G = G_r.next()
                    rs_ = slice(i * 128, (i + 1) * 128)
                    for a in range(4):
                        P.dma("sp", acc.t[:, a, :, :].rearrange("p h d -> p (h d)"),
                              ACC[a, rs_, :], writes=[acc])
                    P.dma("sp", xt.t[:], xsrc[rs_, :], writes=[xt])
                    P.op("dve", lambda e: e.tensor_tensor(out=acc.t[:, 0], in0=acc.t[:, 0], in1=acc.t[:, 1], op=ALU.add),
                         [acc], [acc])
                    P.op("dve", lambda e: e.tensor_tensor(out=acc.t[:, 0], in0=acc.t[:, 0], in1=acc.t[:, 2], op=ALU.add),
                         [acc], [acc])
                    P.op("dve", lambda e: e.tensor_copy(out=den.t[:, 0, :], in_=acc.t[:, 0, :, 64]), [acc], [den])
                    P.op("dve", lambda e: e.tensor_tensor(out=den.t[:, 1, :], in0=acc.t[:, 3, :, 64], in1=esink.t[:],
                                                          op=ALU.add), [acc, esink], [den])
                    P.op("dve", lambda e: e.reciprocal(out=den.t[:], in_=den.t[:]), [den], [den])
                    P.op("dve", lambda e: e.tensor_tensor(
                        out=o_t.t[:, 0], in0=acc.t[:, 0, :, 0:HD],
                        in1=den.t[:, 0, :].unsqueeze(2).to_broadcast([128, NH, HD]), op=ALU.mult), [acc, den], [o_t])
                    P.op("pool", lambda e: e.tensor_tensor(
                        out=o_t.t[:, 1], in0=acc.t[:, 3, :, 0:HD],
                        in1=den.t[:, 1, :].unsqueeze(2).to_broadcast([128, NH, HD]), op=ALU.mult), [acc, den], [o_t])
                    of = o_t.t[:].rearrange("p a h d -> p (a h d)")
                    if DBG['c_stop'] <= 1:
                        continue
                    for a in range(2):
                        P.op("act", lambda e: e.activation(out=junk.t[:, a * 512:(a + 1) * 512],
                                                           in_=of[:, a * 512:(a + 1) * 512], func=AF.Square,
                                                           accum_out=ss2.t[:, a:a + 1]), [o_t], [junk, ss2])
                    rstd_from_ss(ss2.t[:], 512, rs2, tm2, [ss2])
                    for a in range(2):
                        P.op("dve", lambda e: e.scalar_tensor_tensor(
                            out=mix.t[:, a * 512:(a + 1) * 512], in0=of[:, a * 512:(a + 1) * 512],
                            scalar=rs2.t[:, a:a + 1], in1=g_out.t[:, a * 512:(a + 1) * 512],
                            op0=ALU.mult, op1=ALU.mult), [o_t, rs2, g_out], [mix])

                    def trm(e):
                        for c in range(8):
                            ins = e.transpose(pTm.t[:, c, :], mix.t[:, c * 128:(c + 1) * 128], ident_b.t[:])
                        return ins
                    P.op("pe", trm, [mix, ident_b], [pTm])
                    P.op("act", lambda e: e.copy(out=mT.t[:], in_=pTm.t[:]), [pTm], [mT])
                    for hf, pO in ((0, pOa), (1, pOb)):
                        def mmo(e):
                            for c in range(8):
                                ins = e.matmul(pO.t[:], lhsT=mT.t[:, c, :], rhs=wout.t[:, c, hf * 512:(hf + 1) * 512],
                                               start=(c == 0), stop=(c == 7))
                            return ins
                        P.op("pe", mmo, [mT, wout], [pO])
                        P.op("dve", lambda e: e.tensor_tensor(out=xm.t[:, hf * 512:(hf + 1) * 512],
                                                              in0=xt.t[:, hf * 512:(hf + 1) * 512], in1=pO.t[:],
                                                              op=ALU.add), [xt, pO], [xm])
                    P.dma("sp", XM[rs_, :], xm.t[:], reads=[xm])
                    if DBG['c_stop'] <= 2:
                        continue
                    P.op("act", lambda e: e.activation(out=junk.t[:], in_=xm.t[:], func=AF.Square, accum_out=ss3.t[:]),
                         [xm], [junk, ss3])
                    rstd_from_ss(ss3.t[:], D, rs3, tm3, [ss3])
                    P.op("dve", lambda e: e.scalar_tensor_tensor(out=h2.t[:], in0=xm.t[:], scalar=rs3.t[:, 0:1],
                                                                 in1=g_ffn.t[:], op0=ALU.mult, op1=ALU.mult),
                         [xm, rs3, g_ffn], [h2])
                    for hf in range(2):
                        def trh(e):
                            for c in range(4):
                                cc = hf * 4 + c
                                ins = e.transpose(pTh[hf].t[:, c, :], h2.t[:, cc * 128:(cc + 1) * 128], ident_f.t[:])
                            return ins
                        P.op("pe", trh, [h2, ident_f], [pTh[hf]])
                        P.op("act", lambda e: e.copy(out=h2Tf.t[:, hf * 4:(hf + 1) * 4, :], in_=pTh[hf].t[:]),
                             [pTh[hf]], [h2Tf])
                        P.op("dve", lambda e: e.tensor_copy(out=h2Tb.t[:, hf * 4:(hf + 1) * 4, :], in_=pTh[hf].t[:]),
                             [pTh[hf]], [h2Tb])
                    P.dma("sp", H2T.rearrange("(c p) t -> p c t", p=128)[:, :, rs_], h2Tb.t[:], reads=[h2Tb])

                    if DBG['c_stop'] <= 3:
                        continue

                    def mmr(e):
                        for c in range(8):
                            ins = e.matmul(pR.t[:], lhsT=h2Tf.t[:, c, :], rhs=wr.t[:, c, :], start=(c == 0), stop=(c == 7))
                        return ins
                    P.op("pe", mmr, [h2Tf, wr], [pR])
                    P.op("act", lambda e: e.copy(out=lg.t[:], in_=pR.t[:]), [pR], [lg])
                    BIG = 1e30
                    P.op("dve", lambda e: e.tensor_reduce(out=gmx.t[:, 0:1], in_=lg.t[:, 0:4], axis=AX.X, op=ALU.max),
                         [lg], [gmx])
                    P.op("dve", lambda e: e.tensor_scalar(out=ex4.t[:], in0=lg.t[:, 0:4], scalar1=gmx.t[:, 0:1],
                                                          scalar2=None, op0=ALU.subtract), [lg, gmx], [ex4])
                    P.op("act", lambda e: e.activation(out=ex4.t[:], in_=ex4.t[:], func=AF.Exp,
                                                       accum_out=gmx.t[:, 1:2]), [ex4], [ex4, gmx])
                    P.op("dve", lambda e: e.reciprocal(out=gmx.t[:, 2:3], in_=gmx.t[:, 1:2]), [gmx], [gmx])
                    P.op("dve", lambda e: e.tensor_scalar(out=gm.t[:], in0=lg.t[:, 0:4], scalar1=gmx.t[:, 0:1],
                                                          scalar2=-BIG, op0=ALU.is_lt, op1=ALU.mult), [lg, gmx], [gm])
                    P.op("dve", lambda e: e.tensor_tensor(
                        out=lem.t[:], in0=lg.t[:, 4:36].rearrange("p (g e) -> p g e", e=8),
                        in1=gm.t[:].unsqueeze(2).to_broadcast([128, 4, 8]), op=ALU.add), [lg, gm], [lem])
                    lef = lem.t[:].rearrange("p g e -> p (g e)")
                    P.op("dve", lambda e: e.tensor_reduce(out=gmx.t[:, 3:4], in_=lef, axis=AX.X, op=ALU.max), [lem], [gmx])
                    P.op("dve", lambda e: e.tensor_scalar(out=m1k.t[:], in0=lef, scalar1=gmx.t[:, 3:4], scalar2=None,
                                                          op0=ALU.is_ge), [lem, gmx], [m1k])
                    P.op("dve", lambda e: e.scalar_tensor_tensor(out=lef, in0=m1k.t[:], scalar=-BIG, in1=lef,
                                                                 op0=ALU.mult, op1=ALU.add), [m1k, lem], [lem])
                    P.op("dve", lambda e: e.tensor_reduce(out=gmx.t[:, 4:5], in_=lef, axis=AX.X, op=ALU.max), [lem], [gmx])
                    P.op("dve", lambda e: e.tensor_scalar(out=m2k.t[:], in0=lef, scalar1=gmx.t[:, 4:5], scalar2=None,
                                                          op0=ALU.is_ge), [lem, gmx], [m2k])
                    P.op("dve", lambda e: e.tensor_tensor(out=gmx.t[:, 5:6], in0=gmx.t[:, 3:4], in1=gmx.t[:, 4:5],
                                                          op=ALU.subtract), [gmx], [gmx])
                    P.op("act", lambda e: e.activation(out=gmx.t[:, 5:6], in_=gmx.t[:, 5:6], func=AF.Exp, scale=-1.0),
                         [gmx], [gmx])
                    P.op("dve", lambda e: e.tensor_scalar(out=gmx.t[:, 5:6], in0=gmx.t[:, 5:6], scalar1=1.0, scalar2=None,
                                                          op0=ALU.add), [gmx], [gmx])
                    P.op("dve", lambda e: e.reciprocal(out=gmx.t[:, 5:6], in_=gmx.t[:, 5:6]), [gmx], [gmx])
                    P.op("dve", lambda e: e.tensor_tensor(out=gmx.t[:, 6:7], in0=gmx.t[:, 5:6], in1=gmx.t[:, 2:3],
                                                          op=ALU.mult), [gmx], [gmx])
                    P.op("dve", lambda e: e.tensor_tensor(out=gmx.t[:, 7:8], in0=gmx.t[:, 2:3], in1=gmx.t[:, 6:7],
                                                          op=ALU.subtract), [gmx], [gmx])
                    P.op("dve", lambda e: e.tensor_scalar(out=G.t[:], in0=m1k.t[:], scalar1=gmx.t[:, 6:7], scalar2=None,
                                                          op0=ALU.mult), [m1k, gmx], [G])
                    P.op("dve", lambda e: e.scalar_tensor_tensor(out=G.t[:], in0=m2k.t[:], scalar=gmx.t[:, 7:8],
                                                                 in1=G.t[:], op0=ALU.mult, op1=ALU.add),
                         [m2k, gmx, G], [G])
                    P.dma("sp", GS[rs_, :], G.t[:], reads=[G])
                P.barrier()

            if "D" in phases:
              with ExitStack() as sd:
                NTS = TS // 128
                hT = sb(sd, "hT", [128, 8, TS], BF16)
                Gt = sb(sd, "Gt", [128, NTS, NE], F32)
                yacc = sb(sd, "yacc", [128, NTS, D], F32)
                wgu_r = Ring([sb(sd, "wgu%d" % i, [128, 8, 2 * FE], BF16) for i in range(2)])
                wdn_r = Ring([sb(sd, "wdn%d" % i, [128, 4, D], BF16) for i in range(2)])
                sg_r = Ring([sb(sd, "sg%d" % i, [128, 512], F32) for i in range(2)])
                aT_r = Ring([sb(sd, "aT%d" % i, [128, 4, 512], BF16) for i in range(2)])
                xm_r = Ring([sb(sd, "xmD%d" % i, [128, D], F32) for i in range(2)])
                pg_r = Ring([ps(sd, "pg%d" % i, [128, 512], F32) for i in range(2)])
                pu_r = Ring([ps(sd, "pu%d" % i, [128, 512], F32) for i in range(2)])
                py_r = Ring([(ps(sd, "pya%d" % i, [128, 512], F32), ps(sd, "pyb%d" % i, [128, 512], F32))
                             for i in range(2)])
                for s_ in range(S // TS):
                    t0 = s_ * TS
                    P.dma("sp", hT.t[:], H2T.rearrange("(c p) t -> p c t", p=128)[:, :, t0:t0 + TS], writes=[hT])
                    P.dma("sp", Gt.t[:], GS[t0:t0 + TS, :].rearrange("(i p) e -> p i e", p=128), writes=[Gt])
                    P.op("dve", lambda e: e.memset(yacc.t[:], 0.0), [], [yacc])
                    for ex in range(n_exp):
                        wgu = wgu_r.next(); wdn = wdn_r.next()
                        for hf in range(2):
                            P.dma("pool", wgu.t[:, hf * 4:(hf + 1) * 4, :],
                                  w_gu[l, ex, hf * 512:(hf + 1) * 512, :].rearrange("(c p) n -> p c n", p=128),
                                  writes=[wgu])
                        P.dma("pool", wdn.t[:], w_dn[l, ex, :, :].rearrange("(c p) n -> p c n", p=128), writes=[wdn])
                        for g in range(TS // 512):
                            aT = aT_r.next()
                            tk = slice(g * 512, (g + 1) * 512)
                            for c in range(4):
                                pg = pg_r.next(); pu = pu_r.next(); sg = sg_r.next()

                                def mmg(e):
                                    for k in range(8):
                                        ins = e.matmul(pg.t[:], lhsT=wgu.t[:, k, c * 128:(c + 1) * 128], rhs=hT.t[:, k, tk],
                                                       start=(k == 0), stop=(k == 7))
                                    return ins

                                def mmu(e):
                                    for k in range(8):
                                        ins = e.matmul(pu.t[:], lhsT=wgu.t[:, k, FE + c * 128:FE + (c + 1) * 128],
                                                       rhs=hT.t[:, k, tk], start=(k == 0), stop=(k == 7))
                                    return ins
                                P.op("pe", mmg, [wgu, hT], [pg])
                                P.op("pe", mmu, [wgu, hT], [pu])
                                P.op("act", lambda e: e.activation(out=sg.t[:], in_=pg.t[:], func=AF.Silu), [pg], [sg])
                                P.op("dve", lambda e: e.tensor_tensor(out=aT.t[:, c, :], in0=sg.t[:], in1=pu.t[:],
                                                                      op=ALU.mult), [sg, pu], [aT])
                            for t in range(4):
                                ti = g * 4 + t
                                pya, pyb = py_r.next()
                                for hf, py in ((0, pya), (1, pyb)):
                                    def mmd(e):
                                        for c in range(4):
                                            ins = e.matmul(py.t[:], lhsT=aT.t[:, c, t * 128:(t + 1) * 128],
                                                           rhs=wdn.t[:, c, hf * 512:(hf + 1) * 512],
                                                           start=(c == 0), stop=(c == 3))
                                        return ins
                                    P.op("pe", mmd, [aT, wdn], [py])
                                    P.op("dve", lambda e: e.scalar_tensor_tensor(
                                        out=yacc.t[:, ti, hf * 512:(hf + 1) * 512], in0=py.t[:],
                                        scalar=Gt.t[:, ti, ex:ex + 1], in1=yacc.t[:, ti, hf * 512:(hf + 1) * 512],
                                        op0=ALU.mult, op1=ALU.add), [py, Gt, yacc], [yacc])
                    for ti in range(NTS):
                        xm = xm_r.next()
                        rs_ = slice(t0 + ti * 128, t0 + (ti + 1) * 128)
                        P.dma("sp", xm.t[:], XM[rs_, :], writes=[xm])
                        P.op("dve", lambda e: e.tensor_tensor(out=xm.t[:], in0=xm.t[:], in1=yacc.t[:, ti, :], op=ALU.add),
                             [xm, yacc], [xm])
                        P.dma("sp", xdst[rs_, :], xm.t[:], reads=[xm])
                P.barrier()
        P.barrier()
    return nc


def _consts():
    k = np.arange(128)[:, None]
    q = np.arange(128)[None, :]
    mask = np.stack([(k >= q), (k <= q), (k > q), (k <= q)], axis=1).astype(np.float32)
    ident = np.eye(128, dtype=np.float32)
    invf = (500000.0 ** (-np.arange(0, 16, 2, dtype=np.float32) / np.float32(16))).astype(np.float32)
    invf = np.ascontiguousarray(np.broadcast_to(invf[None, :], (128, 8)))
    return ident, np.ascontiguousarray(mask), invf


def prep_shared(inputs, L):
    f = lambda k: np.asarray(inputs[k], dtype=np.float32)
    rep = lambda v: np.broadcast_to(v[:, None, :], (L, 128, v.shape[-1]))
    gvec = np.stack([rep(f("attn_norm")), rep(np.concatenate([f("out_norm_a"), f("out_norm_b")], axis=-1)),
                     rep(f("ffn_norm"))], axis=2)
    qa, ka, qb, kb = f("q_norm_a"), f("k_norm_a"), f("q_norm_b"), f("k_norm_b")
    gq = np.concatenate([np.tile(qa, (1, 8)), np.tile(qb, (1, 8)), np.tile(ka, (1, 8)), np.tile(kb, (1, 2))], axis=-1)
    w = f("w_in")
    w_in_p = np.concatenate([w[:, :, 0:512], w[:, :, 1536:2048], w[:, :, 512:1024], w[:, :, 2048:2176],
                             w[:, :, 1024:1536], w[:, :, 2176:2304]], axis=-1)
    ident, mask, invf = _consts()
    return dict(
        c_ident=ident, c_mask=mask, c_invf=invf,
        gvec=np.ascontiguousarray(gvec), gqk=np.ascontiguousarray(rep(gq)),
        sinks=np.ascontiguousarray(rep(f("sinks_b"))),
        w_in=np.ascontiguousarray(w_in_p), w_out=f("w_out"),
        w_r=np.ascontiguousarray(np.concatenate([f("w_router_group"), f("w_router_expert")], axis=-1)),
        w_gu=f("w_gate_up"), w_dn=f("w_down"))


def core_map(shared, x_b, pos_b):
    S = x_b.shape[0]
    m = dict(shared)
    m["x"] = np.ascontiguousarray(x_b, dtype=np.float32)
    m["pos_t"] = np.ascontiguousarray(np.asarray(pos_b, dtype=np.int32).reshape(S // 128, 128).T)
    return m


def kernel(**inputs):
    x = np.asarray(inputs["x"])
    pos = np.asarray(inputs["positions"])
    B, S, _ = x.shape
    L = np.asarray(inputs["attn_norm"]).shape[0]
    shared = prep_shared(inputs, L)
    nc = build(S, L)
    in_maps = [core_map(shared, x[b], pos[b]) for b in range(B)]
    res = run_bass_kernel_spmd(nc, in_maps, core_ids=list(range(B)))
    return np.stack([np.asarray(r["out"]).reshape(S, D) for r in res.results], axis=0).astype(np.float32)
```

```python
import numpy as np
from contextlib import ExitStack
import concourse.bass as bass
import concourse.mybir as mybir
from concourse.bass_utils import run_bass_kernel_spmd

F32 = mybir.dt.float32
BF16 = mybir.dt.bfloat16
I32 = mybir.dt.int32
ALU = mybir.AluOpType
AF = mybir.ActivationFunctionType
AX = mybir.AxisListType

D = 1024
PW = 2304
NH = 8
HD = 64
NE = 32
FE = 512
VW = 80
EPS = 1e-6
TWO_PI_HI = 6.28125
TWO_PI_LO = 2.0 * np.pi - 6.28125


class Buf:
    __slots__ = ("name", "w", "r", "excl")

    def __init__(self, name):
        self.name = name
        self.w = None
        self.r = {}
        self.excl = False


class T:
    def __init__(self, t, name):
        self.t = t
        self.b = Buf(name)


class Prog:
    def __init__(self, nc, es, n_dsem=22):
        self.nc = nc
        self.es = es
        self.eng = dict(pe=nc.tensor, act=nc.scalar, dve=nc.vector, pool=nc.gpsimd, sp=nc.sync)
        self.sem = {e: es.enter_context(nc.semaphore("s_" + e)) for e in ("pe", "act", "dve", "pool")}
        self.cnt = {e: 0 for e in self.sem}
        self.dsem = [es.enter_context(nc.semaphore("d%d" % i)) for i in range(n_dsem)]
        self.dcnt = [0] * n_dsem
        self.dnext = {"hw": 0, "sw": 0}
        self.n_sw = 6
        self.seen = {e: {} for e in self.eng}

    def _semof(self, key):
        return self.sem[key] if isinstance(key, str) else self.dsem[key[1]]

    def _wait(self, e, tok):
        if tok is None:
            return
        key, val = tok
        if key == e and e == "pe":
            return
        if self.seen[e].get(key, 0) >= val:
            return
        self.eng[e].wait_ge(self._semof(key), val)
        self.seen[e][key] = val

    def _deps(self, e, reads, writes):
        for b in reads:
            self._wait(e, b.w)
        for b in writes:
            self._wait(e, b.w)
            for k, v in b.r.items():
                self._wait(e, (k, v))

    def _record(self, tok, reads, writes):
        key, val = tok
        for b in reads:
            if b.r.get(key, 0) < val:
                b.r[key] = val
        for b in writes:
            b.w = tok
            b.r = {}

    def op(self, e, fn, reads=(), writes=()):
        reads = [x.b if isinstance(x, T) else x for x in reads]
        writes = [x.b if isinstance(x, T) else x for x in writes]
        writes = writes + [b for b in reads if b.excl]
        reads = [b for b in reads if not b.excl]
        self._deps(e, reads, writes)
        ins = fn(self.eng[e])
        self.cnt[e] += 1
        ins.then_inc(self.sem[e], 1)
        self._record((e, self.cnt[e]), reads, writes)

    def dma(self, q, out, in_, reads=(), writes=()):
        reads = [x.b if isinstance(x, T) else x for x in reads]
        writes = [x.b if isinstance(x, T) else x for x in writes]
        if q == "pool":
            i = self.dnext["sw"]
            self.dnext["sw"] = (i + 1) % self.n_sw
        else:
            i = self.n_sw + self.dnext["hw"]
            self.dnext["hw"] = (self.dnext["hw"] + 1) % (len(self.dsem) - self.n_sw)
        if self.dcnt[i]:
            self._wait(q, (("d", i), self.dcnt[i]))
        self._deps(q, reads, writes)
        ins = self.eng[q].dma_start(out=out, in_=in_)
        self.dcnt[i] += 16
        ins.then_inc(self.dsem[i], 16)
        self._record((("d", i), self.dcnt[i]), reads, writes)

    def barrier(self):
        for e in self.eng:
            for o in self.sem:
                if o != e and self.cnt[o]:
                    self._wait(e, (o, self.cnt[o]))
            for i, c in enumerate(self.dcnt):
                if c:
                    self._wait(e, (("d", i), c))

    def final_wait(self):
        for i, c in enumerate(self.dcnt):
            if c:
                self._wait("sp", (("d", i), c))


def run_pipeline(items, body):
    gens = []
    it = iter(items)
    pending = True
    while pending or gens:
        nxt = next(it, None) if pending else None
        if nxt is None:
            pending = False
        else:
            gens.append(body(*nxt) if isinstance(nxt, tuple) else body(nxt))
        alive = []
        for g in gens:
            try:
                next(g)
                alive.append(g)
            except StopIteration:
                pass
        gens = alive


class Ring:
    def __init__(self, tiles):
        self.tiles = tiles
        self.i = 0

    def next(self):
        t = self.tiles[self.i % len(self.tiles)]
        self.i += 1
        return t


LIMIT_NT = None
DBG = dict(c_stop=9, b_passes=None, b_blocks=None, act_dma=False, split_exp=True)


def build(S, L, n_exp=NE, phases="ABCD", dbg=False):
    NT = S // 128
    NTL = NT if LIMIT_NT is None else min(NT, LIMIT_NT)
    TS = min(2048, S)
    nc = bass.Bass("TRN2", target_bir_lowering=False)
    dram = lambda name, shape, dt, kind="ExternalInput": nc.dram_tensor(name, shape, dt, kind=kind).ap()
    x_in = dram("x", [S, D], F32)
    pos_t = dram("pos_t", [128, NT], I32)
    c_ident = dram("c_ident", [128, 128], F32)
    c_mask = dram("c_mask", [128, 4, 128], F32)
    c_invf = dram("c_invf", [128, 8], F32)
    gvec = dram("gvec", [L, 128, 3, D], F32)
    gqk = dram("gqk", [L, 128, 26 * HD], F32)
    sinks = dram("sinks", [L, 128, NH], F32)
    w_in = dram("w_in", [L, D, PW], F32)
    w_out = dram("w_out", [L, D, D], F32)
    w_r = dram("w_r", [L, D, 36], F32)
    w_gu = dram("w_gu", [L, NE, D, 2 * FE], F32)
    w_dn = dram("w_dn", [L, NE, FE, D], F32)
    out = dram("out", [S, D], F32, kind="ExternalOutput")
    okind = "ExternalOutput" if dbg else "Internal"
    QKV = dram("QKV", [S, PW], BF16, kind=okind)
    ACC = dram("ACC", [4, S, NH * 65], F32, kind=okind)
    XM = dram("XM", [S, D], F32, kind=okind)
    H2T = dram("H2T", [D, S], BF16, kind=okind)
    GS = dram("GS", [S, NE], F32, kind=okind)
    X1 = dram("X1", [S, D], F32, kind=okind)

    with ExitStack() as es:
        P = Prog(nc, es)

        uid = [0]

        def sb(stack, name, shape, dt):
            uid[0] += 1
            name = "%s_%d" % (name, uid[0])
            return T(stack.enter_context(nc.sbuf_tensor(name, shape, dt)), name)

        def ps(stack, name, shape, dt):
            uid[0] += 1
            name = "%s_%d" % (name, uid[0])
            nfull = 512 if dt == F32 else 1024
            full = stack.enter_context(nc.psum_tensor(name, [128, nfull], dt))
            n = int(np.prod(shape[1:]))
            assert n <= nfull, (name, shape)
            v = full[:, 0:n]
            if len(shape) == 3:
                v = v.rearrange("p (a b) -> p a b", a=shape[1], b=shape[2])
            elif len(shape) == 4:
                v = v.rearrange("p (a b c) -> p a b c", a=shape[1], b=shape[2], c=shape[3])
            t = T(v, name)
            t.b.excl = True
            return t

        ident_f = sb(es, "ident_f", [128, 128], F32)
        ident_b = sb(es, "ident_b", [128, 128], BF16)
        mask_f = sb(es, "mask_f", [128, 4, 128], F32)
        mask_b = sb(es, "mask_b", [128, 4, 128], BF16)
        invf = sb(es, "invf", [128, 8], F32)
        cos_t = sb(es, "cos_t", [128, NT, 8], F32)
        sin_t = sb(es, "sin_t", [128, NT, 8], F32)
        P.dma("sp", ident_f.t[:], c_ident[:, :], writes=[ident_f])
        P.dma("sp", mask_f.t[:], c_mask[:, :, :], writes=[mask_f])
        P.dma("sp", invf.t[:], c_invf[:, :], writes=[invf])
        P.op("dve", lambda e: e.tensor_copy(out=ident_b.t[:], in_=ident_f.t[:]), [ident_f], [ident_b])
        P.op("dve", lambda e: e.tensor_copy(out=mask_b.t[:], in_=mask_f.t[:]), [mask_f], [mask_b])
        with ExitStack() as s0:
            pos_i = sb(s0, "pos_i", [128, NT], I32)
            pos_f = sb(s0, "pos_f", [128, NT], F32)
            ang = sb(s0, "ang", [128, NT, 8], F32)
            ang2 = sb(s0, "ang2", [128, NT, 8], F32)
            kf = sb(s0, "kf", [128, NT, 8], F32)
            ki = sb(s0, "ki", [128, NT, 8], I32)
            P.dma("sp", pos_i.t[:], pos_t[:, :], writes=[pos_i])
            P.op("dve", lambda e: e.tensor_copy(out=pos_f.t[:], in_=pos_i.t[:]), [pos_i], [pos_f])
            P.op("dve", lambda e: e.tensor_tensor(
                out=ang.t[:], in0=pos_f.t[:].unsqueeze(2).to_broadcast([128, NT, 8]),
                in1=invf.t[:].unsqueeze(1).to_broadcast([128, NT, 8]), op=ALU.mult), [pos_f, invf], [ang])
            for which, dst in ((0, sin_t), (1, cos_t)):
                src = ang
                if which == 1:
                    P.op("dve", lambda e: e.tensor_scalar(out=ang2.t[:], in0=ang.t[:], scalar1=float(np.pi / 2),
                                                          scalar2=None, op0=ALU.add), [ang], [ang2])
                    src = ang2
                P.op("dve", lambda e: e.tensor_scalar(out=kf.t[:], in0=src.t[:], scalar1=float(1.0 / (2 * np.pi)),
                                                      scalar2=None, op0=ALU.mult), [src], [kf])
                P.op("dve", lambda e: e.tensor_copy(out=ki.t[:], in_=kf.t[:]), [kf], [ki])
                P.op("dve", lambda e: e.tensor_copy(out=kf.t[:], in_=ki.t[:]), [ki], [kf])
                P.op("dve", lambda e: e.scalar_tensor_tensor(out=dst.t[:], in0=kf.t[:], scalar=-TWO_PI_HI, in1=src.t[:],
                                                             op0=ALU.mult, op1=ALU.add), [kf, src], [dst])
                P.op("dve", lambda e: e.scalar_tensor_tensor(out=dst.t[:], in0=kf.t[:], scalar=-TWO_PI_LO, in1=dst.t[:],
                                                             op0=ALU.mult, op1=ALU.add), [kf, dst], [dst])
                P.op("dve", lambda e: e.tensor_scalar(out=dst.t[:], in0=dst.t[:], scalar1=float(np.pi), scalar2=float(-np.pi),
                                                      op0=ALU.min, op1=ALU.max), [dst], [dst])
                P.op("act", lambda e: e.activation(out=dst.t[:], in_=dst.t[:], func=AF.Sin), [dst], [dst])
            P.barrier()

        def rstd_from_ss(ss_ap, n_feat, out_t, tmp_t, deps_r):
            P.op("dve", lambda e: e.tensor_scalar(out=tmp_t.t[:], in0=ss_ap, scalar1=1.0 / n_feat, scalar2=EPS,
                                                  op0=ALU.mult, op1=ALU.add), deps_r, [tmp_t])
            P.op("act", lambda e: e.activation(out=tmp_t.t[:], in_=tmp_t.t[:], func=AF.Sqrt), [tmp_t], [tmp_t])
            P.op("dve", lambda e: e.reciprocal(out=out_t.t[:], in_=tmp_t.t[:]), [tmp_t], [out_t])

        for l in range(L):
            xsrc = x_in if l == 0 else X1
            xdst = out if l == L - 1 else X1
            if "A" in phases:
              with ExitStack() as sa:
                win = sb(sa, "win", [128, 8, PW], BF16)
                g_attn = sb(sa, "g_attn", [128, D], F32)
                g_qk = sb(sa, "g_qk", [128, 26, HD], F32)
                for c in range(8):
                    P.dma("pool", win.t[:, c, :], w_in[l, c * 128:(c + 1) * 128, :], writes=[win])
                P.dma("sp", g_attn.t[:], gvec[l, :, 0, :], writes=[g_attn])
                P.dma("sp", g_qk.t[:].rearrange("p h d -> p (h d)"), gqk[l, :, :], writes=[g_qk])
                xt_r = Ring([sb(sa, "xt%d" % i, [128, D], F32) for i in range(2)])
                junk = sb(sa, "junkA", [128, D], BF16)
                xg_r = Ring([sb(sa, "xg%d" % i, [128, D], BF16) for i in range(2)])
                xT_r = Ring([sb(sa, "xT%d" % i, [128, 8, 128], BF16) for i in range(3)])
                ss_r = Ring([sb(sa, "ssA%d" % i, [128, 1], F32) for i in range(3)])
                rs_r = Ring([sb(sa, "rsA%d" % i, [128, 1], F32) for i in range(3)])
                tm_r = Ring([sb(sa, "tmA%d" % i, [128, 1], F32) for i in range(3)])
                proj_r = Ring([sb(sa, "proj%d" % i, [128, PW], F32) for i in range(3)])
                sq_r = Ring([sb(sa, "sqA%d" % i, [128, 26, HD], F32) for i in range(3)])
                ssh_r = Ring([sb(sa, "ssh%d" % i, [128, 26], F32) for i in range(3)])
                rsh_r = Ring([sb(sa, "rsh%d" % i, [128, 26], F32) for i in range(3)])
                tmh_r = Ring([sb(sa, "tmh%d" % i, [128, 26], F32) for i in range(3)])
                rr_r = Ring([[sb(sa, "r%d_%d" % (k, i), [128, 26, 8], F32) for k in range(4)] for i in range(2)])
                qkvb_r = Ring([sb(sa, "qkvb%d" % i, [128, PW], BF16) for i in range(2)])
                pT_r = Ring([ps(sa, "pTA%d" % i, [128, 8, 128], BF16) for i in range(2)])
                pP_r = [Ring([ps(sa, "pP%d_%d" % (g, i), [128, 512], F32) for i in range(1)]) for g in range(5)]

                def bodyA(i):
                    sq = sq_r.next(); ssh = ssh_r.next(); rsh = rsh_r.next(); tmh = tmh_r.next()
                    r1, r2, r3, r4 = rr_r.next()
                    xt = xt_r.next(); xg = xg_r.next(); xT = xT_r.next(); ss = ss_r.next(); rs = rs_r.next()
                    tm = tm_r.next(); proj = proj_r.next(); qkvb = qkvb_r.next(); pT = pT_r.next()
                    P.dma("sp", xt.t[:], xsrc[i * 128:(i + 1) * 128, :], writes=[xt])
                    P.op("act", lambda e: e.activation(out=junk.t[:], in_=xt.t[:], func=AF.Square, accum_out=ss.t[:]),
                         [xt], [junk, ss])
                    P.op("dve", lambda e: e.tensor_tensor(out=xg.t[:], in0=xt.t[:], in1=g_attn.t[:], op=ALU.mult),
                         [xt, g_attn], [xg])

                    def tr(e):
                        for c in range(8):
                            ins = e.transpose(pT.t[:, c, :], xg.t[:, c * 128:(c + 1) * 128], ident_b.t[:])
                        return ins
                    P.op("pe", tr, [xg, ident_b], [pT])
                    P.op("act", lambda e: e.copy(out=xT.t[:], in_=pT.t[:]), [pT], [xT])
                    rstd_from_ss(ss.t[:], D, rs, tm, [ss])
                    yield
                    for g in range(5):
                        w = 512 if g < 4 else 256
                        pP = pP_r[g].next()

                        def mm(e):
                            for c in range(8):
                                ins = e.matmul(pP.t[:, 0:w], lhsT=xT.t[:, c, :], rhs=win.t[:, c, g * 512:g * 512 + w],
                                               start=(c == 0), stop=(c == 7))
                            return ins
                        P.op("pe", mm, [xT, win], [pP])
                        eng = "act" if g % 2 == 0 else "dve"
                        if eng == "act":
                            P.op("act", lambda e: e.activation(out=proj.t[:, g * 512:g * 512 + w], in_=pP.t[:, 0:w],
                                                               func=AF.Copy, scale=rs.t[:, 0:1]), [pP, rs], [proj])
                        else:
                            P.op("dve", lambda e: e.tensor_scalar(out=proj.t[:, g * 512:g * 512 + w], in0=pP.t[:, 0:w],
                                                                  scalar1=rs.t[:, 0:1], scalar2=None, op0=ALU.mult),
                                 [pP, rs], [proj])
                    qk = proj.t[:, 0:26 * HD].rearrange("p (h d) -> p h d", d=HD)
                    P.op("pool", lambda e: e.tensor_tensor(out=sq.t[:], in0=qk, in1=qk, op=ALU.mult), [proj], [sq])
                    P.op("dve", lambda e: e.tensor_reduce(out=ssh.t[:], in_=sq.t[:], axis=AX.X, op=ALU.add), [sq], [ssh])
                    rstd_from_ss(ssh.t[:], HD, rsh, tmh, [ssh])
                    yield
                    P.op("dve", lambda e: e.tensor_tensor(out=sq.t[:], in0=qk,
                                                          in1=rsh.t[:].unsqueeze(2).to_broadcast([128, 26, HD]),
                                                          op=ALU.mult), [proj, rsh], [sq])
                    P.op("pool", lambda e: e.tensor_tensor(out=sq.t[:], in0=sq.t[:], in1=g_qk.t[:], op=ALU.mult),
                         [sq, g_qk], [sq])
                    P.op("act", lambda e: e.copy(out=qkvb.t[:, 0:26 * HD], in_=sq.t[:].rearrange("p h d -> p (h d)")),
                         [sq], [qkvb])
                    P.op("pool", lambda e: e.tensor_copy(out=qkvb.t[:, 26 * HD:PW], in_=proj.t[:, 26 * HD:PW]),
                         [proj], [qkvb])
                    cb = cos_t.t[:, i, :].unsqueeze(1).to_broadcast([128, 26, 8])
                    sn = sin_t.t[:, i, :].unsqueeze(1).to_broadcast([128, 26, 8])
                    x1 = sq.t[:, :, 0:8]
                    x2 = sq.t[:, :, 8:16]
                    P.op("dve", lambda e: e.tensor_tensor(out=r1.t[:], in0=x1, in1=cb, op=ALU.mult), [sq, cos_t], [r1])
                    P.op("dve", lambda e: e.tensor_tensor(out=r2.t[:], in0=x2, in1=sn, op=ALU.mult), [sq, sin_t], [r2])
                    P.op("pool", lambda e: e.tensor_tensor(out=r3.t[:], in0=x2, in1=cb, op=ALU.mult), [sq, cos_t], [r3])
                    P.op("pool", lambda e: e.tensor_tensor(out=r4.t[:], in0=x1, in1=sn, op=ALU.mult), [sq, sin_t], [r4])
                    qv = qkvb.t[:, 0:26 * HD].rearrange("p (h d) -> p h d", d=HD)
                    P.op("dve", lambda e: e.tensor_tensor(out=qv[:, :, 0:8], in0=r1.t[:], in1=r2.t[:], op=ALU.subtract),
                         [r1, r2], [qkvb])
                    P.op("dve", lambda e: e.tensor_tensor(out=qv[:, :, 8:16], in0=r3.t[:], in1=r4.t[:], op=ALU.add),
                         [r3, r4], [qkvb])
                    P.dma("sp", QKV[i * 128:(i + 1) * 128, :], qkvb.t[:], reads=[qkvb])
                run_pipeline(range(NTL), bodyA)
                P.barrier()

            if "B" in phases:
              with ExitStack() as sbk:
                qrow_r = Ring([sb(sbk, "qrow%d" % i, [128, 512], BF16) for i in range(2)])
                krow_r = Ring([sb(sbk, "krow%d" % i, [128, 512], BF16) for i in range(2)])
                kdup_r = Ring([sb(sbk, "kdup%d" % i, [128, 2, 2, HD], BF16) for i in range(2)])
                vaug_r = Ring([sb(sbk, "vaug%d" % i, [128, NH, VW], BF16) for i in range(5)])
                for v in vaug_r.tiles:
                    P.op("pool", lambda e: e.memset(v.t[:], 1.0), [], [v])
                qT_r = Ring([sb(sbk, "qT%d" % i, [128, 4, 2, 128], BF16) for i in range(4)])
                for v in qT_r.tiles:
                    P.op("pool", lambda e: e.memset(v.t[:], 0.0), [], [v])
                kT_r = Ring([sb(sbk, "kT%d" % i, [128, 4, 128], BF16) for i in range(5)])
                pt_r = Ring([sb(sbk, "pt%d" % i, [128, 4, 2, 128], BF16) for i in range(3)])
                osb_r = Ring([sb(sbk, "osb%d" % i, [128, NH, 65], F32) for i in range(3)])
                pTq = ps(sbk, "pTq", [128, 4, 128], BF16)
                pTk = ps(sbk, "pTk", [128, 4, 128], BF16)
                pS_r = Ring([(ps(sbk, "pSa%d" % i, [128, 2, 2, 128], F32), ps(sbk, "pSb%d" % i, [128, 2, 2, 128], F32)) for i in range(2)])
                pO_r = Ring([ps(sbk, "pO%d" % i, [128, 4, VW], F32) for i in range(2)])
                passes = [(0, 1, r) for r in range(1)] + [(1, 4, r) for r in range(4)] + \
                         [(2, 16, r) for r in range(16)] + [(3, 1, 0)]
                if DBG['b_passes'] is not None:
                    passes = passes[:DBG['b_passes']]
                items = []
                for (pi, d, r) in passes:
                    nb = S // d // 128
                    if DBG['b_blocks'] is not None:
                        nb = min(nb, DBG['b_blocks'])
                    items += [(pi, d, r, j) for j in range(nb)]
                state = dict(prev=None)

                def bodyB(pi, d, r, j):
                    isB = (pi == 3)
                    qc0 = 512 if isB else 0
                    kc0, kw = (1536, 128) if isB else (1024, 512)
                    vc0, nvh = (2176, 2) if isB else (1664, 8)
                    rows = QKV.rearrange("(m d) c -> d m c", d=d)
                    accv = ACC.rearrange("a (m d) c -> a d m c", d=d)
                    qrow = qrow_r.next(); krow = krow_r.next(); vaug = vaug_r.next(); qT = qT_r.next()
                    kT = kT_r.next(); osb = osb_r.next()
                    prev = state['prev'] if j > 0 else None
                    state['prev'] = (kT, vaug)
                    rs_ = slice(j * 128, (j + 1) * 128)
                    P.dma("sp", qrow.t[:], rows[r, rs_, qc0:qc0 + 512], writes=[qrow])
                    P.dma("sp", krow.t[:, 0:kw], rows[r, rs_, kc0:kc0 + kw], writes=[krow])
                    P.dma("act" if DBG['act_dma'] else "sp", vaug.t[:, 0:nvh, 0:HD],
                          rows[r, rs_, vc0:vc0 + nvh * HD].rearrange("p (h d) -> p h d", d=HD), writes=[vaug])

                    def trq(e):
                        for c in range(4):
                            ins = e.transpose(pTq.t[:, c, :], qrow.t[:, c * 128:(c + 1) * 128], ident_b.t[:])
                        return ins
                    P.op("pe", trq, [qrow, ident_b], [pTq])
                    P.op("dve", lambda e: e.tensor_copy(out=qT.t[0:64, :, 0, :], in_=pTq.t[0:64, :, :]), [pTq], [qT])
                    P.op("dve", lambda e: e.tensor_copy(out=qT.t[64:128, :, 1, :], in_=pTq.t[64:128, :, :]), [pTq], [qT])
                    if isB:
                        kdup = kdup_r.next()
                        kv = krow.t[:, 0:128].rearrange("p (g d) -> p g d", d=HD)
                        P.op("pool", lambda e: e.tensor_copy(
                            out=kdup.t[:], in_=kv.unsqueeze(2).to_broadcast([128, 2, 2, HD])), [krow], [kdup])

                        def trk(e):
                            for c in range(2):
                                ins = e.transpose(pTk.t[:, c, :], kdup.t[:, c, :, :].rearrange("p a d -> p (a d)"),
                                                  ident_b.t[:])
                            return ins
                        P.op("pe", trk, [kdup, ident_b], [pTk])
                        P.op("act", lambda e: e.copy(out=kT.t[:, 0:2, :], in_=pTk.t[:, 0:2, :]), [pTk], [kT])
                    else:
                        def trk(e):
                            for c in range(4):
                                ins = e.transpose(pTk.t[:, c, :], krow.t[:, c * 128:(c + 1) * 128], ident_b.t[:])
                            return ins
                        P.op("pe", trk, [krow, ident_b], [pTk])
                        P.op("act", lambda e: e.copy(out=kT.t[:], in_=pTk.t[:]), [pTk], [kT])
                    for hg in range(2):
                        yield
                        pS = pS_r.next(); pO = pO_r.next(); pt = pt_r.next()
                        s0 = 0 if prev is not None else 1

                        def kslice(kt, h):
                            if isB:
                                return kt.t[:, h // 4, :]
                            return kt.t[:, h // 2, :]

                        def sc(e):
                            for hh in range(4):
                                h = hg * 4 + hh
                                qs = qT.t[:, h // 2, h % 2, :]
                                pSh = pS[hh // 2]
                                if prev is not None:
                                    e.matmul(pSh.t[:, hh % 2, 0, :], lhsT=kslice(prev[0], h), rhs=qs, start=True, stop=True)
                                ins = e.matmul(pSh.t[:, hh % 2, 1, :], lhsT=kslice(kT, h), rhs=qs, start=True, stop=True)
                            return ins
                        rd = [qT, kT] + ([prev[0]] if prev is not None else [])
                        P.op("pe", sc, rd, [pS[0], pS[1]])
                        for hh2 in range(2):
                            P.op("act", lambda e: e.activation(
                                out=pt.t[:, 2 * hh2:2 * hh2 + 2, s0:2, :], in_=pS[hh2].t[:, :, s0:2, :],
                                func=AF.Exp, scale=0.125), [pS[hh2]], [pt])
                        mk = mask_b.t[:, 2 * (1 if isB else 0) + s0:2 * (1 if isB else 0) + 2, :]
                        meng = "dve" if hg == 0 else "pool"
                        P.op(meng, lambda e: e.tensor_tensor(
                            out=pt.t[:, :, s0:2, :], in0=pt.t[:, :, s0:2, :],
                            in1=mk.unsqueeze(1).to_broadcast([128, 4, 2 - s0, 128]), op=ALU.mult), [pt, mask_b], [pt])

                        def pv(e):
                            for hh in range(4):
                                h = hg * 4 + hh
                                vh = (h // 4) if isB else h
                                if prev is not None:
                                    e.matmul(pO.t[:, hh, :], lhsT=pt.t[:, hh, 0, :], rhs=prev[1].t[:, vh, :],
                                             start=True, stop=False)
                                ins = e.matmul(pO.t[:, hh, :], lhsT=pt.t[:, hh, 1, :], rhs=vaug.t[:, vh, :],
                                               start=(prev is None), stop=True)
                            return ins
                        rd = [pt, vaug] + ([prev[1]] if prev is not None else [])
                        P.op("pe", pv, rd, [pO])
                        if hg == 0:
                            P.op("act", lambda e: e.copy(out=osb.t[:, 0:4, :], in_=pO.t[:, :, 0:65]), [pO], [osb])
                        else:
                            P.op("dve", lambda e: e.tensor_copy(out=osb.t[:, 4:8, :], in_=pO.t[:, :, 0:65]), [pO], [osb])
                    P.dma("sp", accv[pi, r, rs_, :], osb.t[:].rearrange("p h d -> p (h d)"), reads=[osb])
                run_pipeline(items, bodyB)
                P.barrier()

            if "C" in phases:
              with ExitStack() as sc_:
                wout = sb(sc_, "wout", [128, 8, D], BF16)
                wr = sb(sc_, "wr", [128, 8, 36], F32)
                g_out = sb(sc_, "g_out", [128, D], F32)
                g_ffn = sb(sc_, "g_ffn", [128, D], F32)
                esink = sb(sc_, "esink", [128, NH], F32)
                for c in range(8):
                    P.dma("pool", wout.t[:, c, :], w_out[l, c * 128:(c + 1) * 128, :], writes=[wout])
                    P.dma("sp", wr.t[:, c, :], w_r[l, c * 128:(c + 1) * 128, :], writes=[wr])
                P.dma("sp", g_out.t[:], gvec[l, :, 1, :], writes=[g_out])
                P.dma("sp", g_ffn.t[:], gvec[l, :, 2, :], writes=[g_ffn])
                P.dma("sp", esink.t[:], sinks[l, :, :], writes=[esink])
                P.op("act", lambda e: e.activation(out=esink.t[:], in_=esink.t[:], func=AF.Exp), [esink], [esink])
                acc_rC = Ring([sb(sc_, "accC%d" % i, [128, 4, NH, 65], F32) for i in range(2)])
                xt_rC = Ring([sb(sc_, "xtC%d" % i, [128, D], F32) for i in range(3)])
                den_rC = Ring([sb(sc_, "denC%d" % i, [128, 2, NH], F32) for i in range(2)])
                o_t_rC = Ring([sb(sc_, "o_tC%d" % i, [128, 2, NH, HD], F32) for i in range(2)])
                ss2_rC = Ring([sb(sc_, "ss2C%d" % i, [128, 2], F32) for i in range(2)])
                rs2_rC = Ring([sb(sc_, "rs2C%d" % i, [128, 2], F32) for i in range(2)])
                tm2_rC = Ring([sb(sc_, "tm2C%d" % i, [128, 2], F32) for i in range(2)])
                mix_rC = Ring([sb(sc_, "mixC%d" % i, [128, D], BF16) for i in range(3)])
                mT_rC = Ring([sb(sc_, "mTC%d" % i, [128, 8, 128], BF16) for i in range(2)])
                xm_rC = Ring([sb(sc_, "xmC%d" % i, [128, D], F32) for i in range(2)])
                ss3_rC = Ring([sb(sc_, "ss3C%d" % i, [128, 1], F32) for i in range(2)])
                rs3_rC = Ring([sb(sc_, "rs3C%d" % i, [128, 1], F32) for i in range(2)])
                tm3_rC = Ring([sb(sc_, "tm3C%d" % i, [128, 1], F32) for i in range(2)])
                h2_rC = Ring([sb(sc_, "h2C%d" % i, [128, D], F32) for i in range(3)])
                h2Tf_rC = Ring([sb(sc_, "h2TfC%d" % i, [128, 8, 128], F32) for i in range(2)])
                h2Tb_rC = Ring([sb(sc_, "h2TbC%d" % i, [128, 8, 128], BF16) for i in range(2)])
                lg_rC = Ring([sb(sc_, "lgC%d" % i, [128, 36], F32) for i in range(3)])
                gmx_rC = Ring([sb(sc_, "gmxC%d" % i, [128, 8], F32) for i in range(2)])
                gm_rC = Ring([sb(sc_, "gmC%d" % i, [128, 4], F32) for i in range(2)])
                ex4_rC = Ring([sb(sc_, "ex4C%d" % i, [128, 4], F32) for i in range(2)])
                lem_rC = Ring([sb(sc_, "lemC%d" % i, [128, 4, 8], F32) for i in range(2)])
                m1k_rC = Ring([sb(sc_, "m1kC%d" % i, [128, 32], F32) for i in range(2)])
                m2k_rC = Ring([sb(sc_, "m2kC%d" % i, [128, 32], F32) for i in range(2)])
                G_rC = Ring([sb(sc_, "GC%d" % i, [128, NE], F32) for i in range(2)])
                junk = sb(sc_, "junkC", [128, D], BF16)
                pTm = ps(sc_, "pTm", [128, 8, 128], BF16)
                pOa = ps(sc_, "pOa", [128, 512], F32)
                pOb = ps(sc_, "pOb", [128, 512], F32)
                pTh = [ps(sc_, "pTh%d" % i, [128, 4, 128], F32) for i in range(2)]
                pR = ps(sc_, "pR", [128, 36], F32)
                def bodyC(i):
                    acc = acc_rC.next()
                    xt = xt_rC.next()
                    den = den_rC.next()
                    o_t = o_t_rC.next()
                    ss2 = ss2_rC.next()
                    rs2 = rs2_rC.next()
                    tm2 = tm2_rC.next()
                    mix = mix_rC.next()
                    mT = mT_rC.next()
                    xm = xm_rC.next()
                    ss3 = ss3_rC.next()
                    rs3 = rs3_rC.next()
                    tm3 = tm3_rC.next()
                    h2 = h2_rC.next()
                    h2Tf = h2Tf_rC.next()
                    h2Tb = h2Tb_rC.next()
                    lg = lg_rC.next()
                    gmx = gmx_rC.next()
                    gm = gm_rC.next()
                    ex4 = ex4_rC.next()
                    lem = lem_rC.next()
                    m1k = m1k_rC.next()
                    m2k = m2k_rC.next()
                    G = G_rC.next()
                    rs_ = slice(i * 128, (i + 1) * 128)
                    for a in range(4):
                        P.dma("sp", acc.t[:, a, :, :].rearrange("p h d -> p (h d)"),
                              ACC[a, rs_, :], writes=[acc])
                    P.dma("sp", xt.t[:], xsrc[rs_, :], writes=[xt])
                    P.op("dve", lambda e: e.tensor_tensor(out=acc.t[:, 0], in0=acc.t[:, 0], in1=acc.t[:, 1], op=ALU.add),
                         [acc], [acc])
                    P.op("dve", lambda e: e.tensor_tensor(out=acc.t[:, 0], in0=acc.t[:, 0], in1=acc.t[:, 2], op=ALU.add),
                         [acc], [acc])
                    P.op("dve", lambda e: e.tensor_copy(out=den.t[:, 0, :], in_=acc.t[:, 0, :, 64]), [acc], [den])
                    P.op("dve", lambda e: e.tensor_tensor(out=den.t[:, 1, :], in0=acc.t[:, 3, :, 64], in1=esink.t[:],
                                                          op=ALU.add), [acc, esink], [den])
                    P.op("dve", lambda e: e.reciprocal(out=den.t[:], in_=den.t[:]), [den], [den])
                    P.op("dve", lambda e: e.tensor_tensor(
                        out=o_t.t[:, 0], in0=acc.t[:, 0, :, 0:HD],
                        in1=den.t[:, 0, :].unsqueeze(2).to_broadcast([128, NH, HD]), op=ALU.mult), [acc, den], [o_t])
                    P.op("pool", lambda e: e.tensor_tensor(
                        out=o_t.t[:, 1], in0=acc.t[:, 3, :, 0:HD],
                        in1=den.t[:, 1, :].unsqueeze(2).to_broadcast([128, NH, HD]), op=ALU.mult), [acc, den], [o_t])
                    of = o_t.t[:].rearrange("p a h d -> p (a h d)")
                    for a in range(2):
                        P.op("act", lambda e: e.activation(out=junk.t[:, a * 512:(a + 1) * 512],
                                                           in_=of[:, a * 512:(a + 1) * 512], func=AF.Square,
                                                           accum_out=ss2.t[:, a:a + 1]), [o_t], [junk, ss2])
                    rstd_from_ss(ss2.t[:], 512, rs2, tm2, [ss2])
                    for a in range(2):
                        P.op("dve", lambda e: e.scalar_tensor_tensor(
                            out=mix.t[:, a * 512:(a + 1) * 512], in0=of[:, a * 512:(a + 1) * 512],
                            scalar=rs2.t[:, a:a + 1], in1=g_out.t[:, a * 512:(a + 1) * 512],
                            op0=ALU.mult, op1=ALU.mult), [o_t, rs2, g_out], [mix])

                    yield

                    def trm(e):
                        for c in range(8):
                            ins = e.transpose(pTm.t[:, c, :], mix.t[:, c * 128:(c + 1) * 128], ident_b.t[:])
                        return ins
                    P.op("pe", trm, [mix, ident_b], [pTm])
                    P.op("act", lambda e: e.copy(out=mT.t[:], in_=pTm.t[:]), [pTm], [mT])
                    for hf, pO in ((0, pOa), (1, pOb)):
                        def mmo(e):
                            for c in range(8):
                                ins = e.matmul(pO.t[:], lhsT=mT.t[:, c, :], rhs=wout.t[:, c, hf * 512:(hf + 1) * 512],
                                               start=(c == 0), stop=(c == 7))
                            return ins
                        P.op("pe", mmo, [mT, wout], [pO])
                        P.op("dve", lambda e: e.tensor_tensor(out=xm.t[:, hf * 512:(hf + 1) * 512],
                                                              in0=xt.t[:, hf * 512:(hf + 1) * 512], in1=pO.t[:],
                                                              op=ALU.add), [xt, pO], [xm])
                    P.dma("sp", XM[rs_, :], xm.t[:], reads=[xm])
                    P.op("act", lambda e: e.activation(out=junk.t[:], in_=xm.t[:], func=AF.Square, accum_out=ss3.t[:]),
                         [xm], [junk, ss3])
                    rstd_from_ss(ss3.t[:], D, rs3, tm3, [ss3])
                    P.op("dve", lambda e: e.scalar_tensor_tensor(out=h2.t[:], in0=xm.t[:], scalar=rs3.t[:, 0:1],
                                                                 in1=g_ffn.t[:], op0=ALU.mult, op1=ALU.mult),
                         [xm, rs3, g_ffn], [h2])
                    yield
                    for hf in range(2):
                        def trh(e):
                            for c in range(4):
                                cc = hf * 4 + c
                                ins = e.transpose(pTh[hf].t[:, c, :], h2.t[:, cc * 128:(cc + 1) * 128], ident_f.t[:])
                            return ins
                        P.op("pe", trh, [h2, ident_f], [pTh[hf]])
                        P.op("act", lambda e: e.copy(out=h2Tf.t[:, hf * 4:(hf + 1) * 4, :], in_=pTh[hf].t[:]),
                             [pTh[hf]], [h2Tf])
                        P.op("dve", lambda e: e.tensor_copy(out=h2Tb.t[:, hf * 4:(hf + 1) * 4, :], in_=pTh[hf].t[:]),
                             [pTh[hf]], [h2Tb])
                    P.dma("sp", H2T.rearrange("(c p) t -> p c t", p=128)[:, :, rs_], h2Tb.t[:], reads=[h2Tb])


                    def mmr(e):
                        for c in range(8):
                            ins = e.matmul(pR.t[:], lhsT=h2Tf.t[:, c, :], rhs=wr.t[:, c, :], start=(c == 0), stop=(c == 7))
                        return ins
                    P.op("pe", mmr, [h2Tf, wr], [pR])
                    P.op("act", lambda e: e.copy(out=lg.t[:], in_=pR.t[:]), [pR], [lg])
                    yield
                    BIG = 1e30
                    P.op("dve", lambda e: e.tensor_reduce(out=gmx.t[:, 0:1], in_=lg.t[:, 0:4], axis=AX.X, op=ALU.max),
                         [lg], [gmx])
                    P.op("dve", lambda e: e.tensor_scalar(out=ex4.t[:], in0=lg.t[:, 0:4], scalar1=gmx.t[:, 0:1],
                                                          scalar2=None, op0=ALU.subtract), [lg, gmx], [ex4])
                    P.op("act", lambda e: e.activation(out=ex4.t[:], in_=ex4.t[:], func=AF.Exp,
                                                       accum_out=gmx.t[:, 1:2]), [ex4], [ex4, gmx])
                    P.op("dve", lambda e: e.reciprocal(out=gmx.t[:, 2:3], in_=gmx.t[:, 1:2]), [gmx], [gmx])
                    P.op("dve", lambda e: e.tensor_scalar(out=gm.t[:], in0=lg.t[:, 0:4], scalar1=gmx.t[:, 0:1],
                                                          scalar2=-BIG, op0=ALU.is_lt, op1=ALU.mult), [lg, gmx], [gm])
                    P.op("dve", lambda e: e.tensor_tensor(
                        out=lem.t[:], in0=lg.t[:, 4:36].rearrange("p (g e) -> p g e", e=8),
                        in1=gm.t[:].unsqueeze(2).to_broadcast([128, 4, 8]), op=ALU.add), [lg, gm], [lem])
                    lef = lem.t[:].rearrange("p g e -> p (g e)")
                    P.op("dve", lambda e: e.tensor_reduce(out=gmx.t[:, 3:4], in_=lef, axis=AX.X, op=ALU.max), [lem], [gmx])
                    P.op("dve", lambda e: e.tensor_scalar(out=m1k.t[:], in0=lef, scalar1=gmx.t[:, 3:4], scalar2=None,
                                                          op0=ALU.is_ge), [lem, gmx], [m1k])
                    P.op("dve", lambda e: e.scalar_tensor_tensor(out=lef, in0=m1k.t[:], scalar=-BIG, in1=lef,
                                                                 op0=ALU.mult, op1=ALU.add), [m1k, lem], [lem])
                    P.op("dve", lambda e: e.tensor_reduce(out=gmx.t[:, 4:5], in_=lef, axis=AX.X, op=ALU.max), [lem], [gmx])
                    P.op("dve", lambda e: e.tensor_scalar(out=m2k.t[:], in0=lef, scalar1=gmx.t[:, 4:5], scalar2=None,
                                                          op0=ALU.is_ge), [lem, gmx], [m2k])
                    P.op("dve", lambda e: e.tensor_tensor(out=gmx.t[:, 5:6], in0=gmx.t[:, 3:4], in1=gmx.t[:, 4:5],
                                                          op=ALU.subtract), [gmx], [gmx])
                    P.op("act", lambda e: e.activation(out=gmx.t[:, 5:6], in_=gmx.t[:, 5:6], func=AF.Exp, scale=-1.0),
                         [gmx], [gmx])
                    P.op("dve", lambda e: e.tensor_scalar(out=gmx.t[:, 5:6], in0=gmx.t[:, 5:6], scalar1=1.0, scalar2=None,
                                                          op0=ALU.add), [gmx], [gmx])
                    P.op("dve", lambda e: e.reciprocal(out=gmx.t[:, 5:6], in_=gmx.t[:, 5:6]), [gmx], [gmx])
                    P.op("dve", lambda e: e.tensor_tensor(out=gmx.t[:, 6:7], in0=gmx.t[:, 5:6], in1=gmx.t[:, 2:3],
                                                          op=ALU.mult), [gmx], [gmx])
                    P.op("dve", lambda e: e.tensor_tensor(out=gmx.t[:, 7:8], in0=gmx.t[:, 2:3], in1=gmx.t[:, 6:7],
                                                          op=ALU.subtract), [gmx], [gmx])
                    P.op("dve", lambda e: e.tensor_scalar(out=G.t[:], in0=m1k.t[:], scalar1=gmx.t[:, 6:7], scalar2=None,
                                                          op0=ALU.mult), [m1k, gmx], [G])
                    P.op("dve", lambda e: e.scalar_tensor_tensor(out=G.t[:], in0=m2k.t[:], scalar=gmx.t[:, 7:8],
                                                                 in1=G.t[:], op0=ALU.mult, op1=ALU.add),
                         [m2k, gmx, G], [G])
                    P.dma("sp", GS[rs_, :], G.t[:], reads=[G])
                run_pipeline(range(NTL), bodyC)
                P.barrier()

            if "D" in phases:
              with ExitStack() as sd:
                NTS = TS // 128
                hT = sb(sd, "hT", [128, 8, TS], BF16)
                Gt = sb(sd, "Gt", [128, NTS, NE], F32)
                yacc = sb(sd, "yacc", [128, NTS, D], F32)
                wgu_r = Ring([sb(sd, "wgu%d" % i, [128, 8, 2 * FE], BF16) for i in range(2)])
                wdn_r = Ring([sb(sd, "wdn%d" % i, [128, 4, D], BF16) for i in range(2)])
                sg_r = Ring([sb(sd, "sg%d" % i, [128, 512], F32) for i in range(2)])
                aT_r = Ring([sb(sd, "aT%d" % i, [128, 4, 512], BF16) for i in range(2)])
                xm_r = Ring([sb(sd, "xmD%d" % i, [128, D], F32) for i in range(2)])
                pg_r = Ring([ps(sd, "pg%d" % i, [128, 512], F32) for i in range(2)])
                pu_r = Ring([ps(sd, "pu%d" % i, [128, 512], F32) for i in range(2)])
                py_r = Ring([(ps(sd, "pya%d" % i, [128, 512], F32), ps(sd, "pyb%d" % i, [128, 512], F32))
                             for i in range(2)])
                for s_ in range(S // TS):
                    t0 = s_ * TS
                    P.dma("sp", hT.t[:], H2T.rearrange("(c p) t -> p c t", p=128)[:, :, t0:t0 + TS], writes=[hT])
                    P.dma("sp", Gt.t[:], GS[t0:t0 + TS, :].rearrange("(i p) e -> p i e", p=128), writes=[Gt])
                    P.op("dve", lambda e: e.memset(yacc.t[:], 0.0), [], [yacc])
                    for ex in range(n_exp):
                        wgu = wgu_r.next(); wdn = wdn_r.next()
                        for hf in range(2):
                            P.dma("pool", wgu.t[:, hf * 4:(hf + 1) * 4, :],
                                  w_gu[l, ex, hf * 512:(hf + 1) * 512, :].rearrange("(c p) n -> p c n", p=128),
                                  writes=[wgu])
                        P.dma("pool", wdn.t[:], w_dn[l, ex, :, :].rearrange("(c p) n -> p c n", p=128), writes=[wdn])
                        for g in range(TS // 512):
                            aT = aT_r.next()
                            tk = slice(g * 512, (g + 1) * 512)
                            for c in range(4):
                                pg = pg_r.next(); pu = pu_r.next(); sg = sg_r.next()

                                def mmg(e):
                                    for k in range(8):
                                        ins = e.matmul(pg.t[:], lhsT=wgu.t[:, k, c * 128:(c + 1) * 128], rhs=hT.t[:, k, tk],
                                                       start=(k == 0), stop=(k == 7))
                                    return ins

                                def mmu(e):
                                    for k in range(8):
                                        ins = e.matmul(pu.t[:], lhsT=wgu.t[:, k, FE + c * 128:FE + (c + 1) * 128],
                                                       rhs=hT.t[:, k, tk], start=(k == 0), stop=(k == 7))
                                    return ins
                                P.op("pe", mmg, [wgu, hT], [pg])
                                P.op("pe", mmu, [wgu, hT], [pu])
                                P.op("act", lambda e: e.activation(out=sg.t[:], in_=pg.t[:], func=AF.Silu), [pg], [sg])
                                P.op("dve", lambda e: e.tensor_tensor(out=aT.t[:, c, :], in0=sg.t[:], in1=pu.t[:],
                                                                      op=ALU.mult), [sg, pu], [aT])
                            for t in range(4):
                                ti = g * 4 + t
                                pya, pyb = py_r.next()
                                for hf, py in ((0, pya), (1, pyb)):
                                    def mmd(e):
                                        for c in range(4):
                                            ins = e.matmul(py.t[:], lhsT=aT.t[:, c, t * 128:(t + 1) * 128],
                                                           rhs=wdn.t[:, c, hf * 512:(hf + 1) * 512],
                                                           start=(c == 0), stop=(c == 3))
                                        return ins
                                    P.op("pe", mmd, [aT, wdn], [py])
                                    P.op("dve", lambda e: e.scalar_tensor_tensor(
                                        out=yacc.t[:, ti, hf * 512:(hf + 1) * 512], in0=py.t[:],
                                        scalar=Gt.t[:, ti, ex:ex + 1], in1=yacc.t[:, ti, hf * 512:(hf + 1) * 512],
                                        op0=ALU.mult, op1=ALU.add), [py, Gt, yacc], [yacc])
                    for ti in range(NTS):
                        xm = xm_r.next()
                        rs_ = slice(t0 + ti * 128, t0 + (ti + 1) * 128)
                        P.dma("sp", xm.t[:], XM[rs_, :], writes=[xm])
                        P.op("dve", lambda e: e.tensor_tensor(out=xm.t[:], in0=xm.t[:], in1=yacc.t[:, ti, :], op=ALU.add),
                             [xm, yacc], [xm])
                        P.dma("sp", xdst[rs_, :], xm.t[:], reads=[xm])
                P.barrier()
        P.barrier()
    return nc


def _consts():
    k = np.arange(128)[:, None]
    q = np.arange(128)[None, :]
    mask = np.stack([(k >= q), (k <= q), (k > q), (k <= q)], axis=1).astype(np.float32)
    ident = np.eye(128, dtype=np.float32)
    invf = (500000.0 ** (-np.arange(0, 16, 2, dtype=np.float32) / np.float32(16))).astype(np.float32)
    invf = np.ascontiguousarray(np.broadcast_to(invf[None, :], (128, 8)))
    return ident, np.ascontiguousarray(mask), invf


def prep_shared(inputs, L):
    f = lambda k: np.asarray(inputs[k], dtype=np.float32)
    rep = lambda v: np.broadcast_to(v[:, None, :], (L, 128, v.shape[-1]))
    gvec = np.stack([rep(f("attn_norm")), rep(np.concatenate([f("out_norm_a"), f("out_norm_b")], axis=-1)),
                     rep(f("ffn_norm"))], axis=2)
    qa, ka, qb, kb = f("q_norm_a"), f("k_norm_a"), f("q_norm_b"), f("k_norm_b")
    gq = np.concatenate([np.tile(qa, (1, 8)), np.tile(qb, (1, 8)), np.tile(ka, (1, 8)), np.tile(kb, (1, 2))], axis=-1)
    w = f("w_in")
    w_in_p = np.concatenate([w[:, :, 0:512], w[:, :, 1536:2048], w[:, :, 512:1024], w[:, :, 2048:2176],
                             w[:, :, 1024:1536], w[:, :, 2176:2304]], axis=-1)
    ident, mask, invf = _consts()
    return dict(
        c_ident=ident, c_mask=mask, c_invf=invf,
        gvec=np.ascontiguousarray(gvec), gqk=np.ascontiguousarray(rep(gq)),
        sinks=np.ascontiguousarray(rep(f("sinks_b"))),
        w_in=np.ascontiguousarray(w_in_p), w_out=f("w_out"),
        w_r=np.ascontiguousarray(np.concatenate([f("w_router_group"), f("w_router_expert")], axis=-1)),
        w_gu=f("w_gate_up"), w_dn=f("w_down"))


def core_map(shared, x_b, pos_b):
    S = x_b.shape[0]
    m = dict(shared)
    m["x"] = np.ascontiguousarray(x_b, dtype=np.float32)
    m["pos_t"] = np.ascontiguousarray(np.asarray(pos_b, dtype=np.int32).reshape(S // 128, 128).T)
    return m


def kernel(**inputs):
    x = np.asarray(inputs["x"])
    pos = np.asarray(inputs["positions"])
    B, S, _ = x.shape
    L = np.asarray(inputs["attn_norm"]).shape[0]
    shared = prep_shared(inputs, L)
    nc = build(S, L)
    in_maps = [core_map(shared, x[b], pos[b]) for b in range(B)]
    res = run_bass_kernel_spmd(nc, in_maps, core_ids=list(range(B)))
    return np.stack([np.asarray(r["out"]).reshape(S, D) for r in res.results], axis=0).astype(np.float32)
```

```python
import numpy as np
from contextlib import ExitStack
import concourse.bass as bass
import concourse.mybir as mybir
from concourse.bass_utils import run_bass_kernel_spmd

F32 = mybir.dt.float32
BF16 = mybir.dt.bfloat16
I32 = mybir.dt.int32
ALU = mybir.AluOpType
AF = mybir.ActivationFunctionType
AX = mybir.AxisListType

D = 1024
PW = 2304
NH = 8
HD = 64
NE = 32
FE = 512
VW = 80
EPS = 1e-6
TWO_PI_HI = 6.28125
TWO_PI_LO = 2.0 * np.pi - 6.28125


class Buf:
    __slots__ = ("name", "w", "r", "excl")

    def __init__(self, name):
        self.name = name
        self.w = None
        self.r = {}
        self.excl = False


class T:
    def __init__(self, t, name):
        self.t = t
        self.b = Buf(name)


class Prog:
    def __init__(self, nc, es, n_dsem=22):
        self.nc = nc
        self.es = es
        self.eng = dict(pe=nc.tensor, act=nc.scalar, dve=nc.vector, pool=nc.gpsimd, sp=nc.sync)
        self.sem = {e: es.enter_context(nc.semaphore("s_" + e)) for e in ("pe", "act", "dve", "pool")}
        self.cnt = {e: 0 for e in self.sem}
        self.dsem = [es.enter_context(nc.semaphore("d%d" % i)) for i in range(n_dsem)]
        self.dcnt = [0] * n_dsem
        self.dnext = {"hw": 0, "sw": 0}
        self.n_sw = 6
        self.seen = {e: {} for e in self.eng}

    def _semof(self, key):
        return self.sem[key] if isinstance(key, str) else self.dsem[key[1]]

    def _wait(self, e, tok):
        if tok is None:
            return
        key, val = tok
        if key == e and e == "pe":
            return
        if self.seen[e].get(key, 0) >= val:
            return
        self.eng[e].wait_ge(self._semof(key), val)
        self.seen[e][key] = val

    def _deps(self, e, reads, writes):
        for b in reads:
            self._wait(e, b.w)
        for b in writes:
            self._wait(e, b.w)
            for k, v in b.r.items():
                self._wait(e, (k, v))

    def _record(self, tok, reads, writes):
        key, val = tok
        for b in reads:
            if b.r.get(key, 0) < val:
                b.r[key] = val
        for b in writes:
            b.w = tok
            b.r = {}

    def op(self, e, fn, reads=(), writes=()):
        reads = [x.b if isinstance(x, T) else x for x in reads]
        writes = [x.b if isinstance(x, T) else x for x in writes]
        writes = writes + [b for b in reads if b.excl]
        reads = [b for b in reads if not b.excl]
        self._deps(e, reads, writes)
        ins = fn(self.eng[e])
        self.cnt[e] += 1
        ins.then_inc(self.sem[e], 1)
        self._record((e, self.cnt[e]), reads, writes)

    def dma(self, q, out, in_, reads=(), writes=()):
        reads = [x.b if isinstance(x, T) else x for x in reads]
        writes = [x.b if isinstance(x, T) else x for x in writes]
        if q == "pool":
            i = self.dnext["sw"]
            self.dnext["sw"] = (i + 1) % self.n_sw
        else:
            i = self.n_sw + self.dnext["hw"]
            self.dnext["hw"] = (self.dnext["hw"] + 1) % (len(self.dsem) - self.n_sw)
        if self.dcnt[i]:
            self._wait(q, (("d", i), self.dcnt[i]))
        self._deps(q, reads, writes)
        ins = self.eng[q].dma_start(out=out, in_=in_)
        self.dcnt[i] += 16
        ins.then_inc(self.dsem[i], 16)
        self._record((("d", i), self.dcnt[i]), reads, writes)

    def barrier(self):
        for e in self.eng:
            for o in self.sem:
                if o != e and self.cnt[o]:
                    self._wait(e, (o, self.cnt[o]))
            for i, c in enumerate(self.dcnt):
                if c:
                    self._wait(e, (("d", i), c))

    def final_wait(self):
        for i, c in enumerate(self.dcnt):
            if c:
                self._wait("sp", (("d", i), c))


def run_pipeline(items, body):
    gens = []
    it = iter(items)
    pending = True
    while pending or gens:
        nxt = next(it, None) if pending else None
        if nxt is None:
            pending = False
        else:
            gens.append(body(*nxt) if isinstance(nxt, tuple) else body(nxt))
        alive = []
        for g in gens:
            try:
                next(g)
                alive.append(g)
            except StopIteration:
                pass
        gens = alive


class Ring:
    def __init__(self, tiles):
        self.tiles = tiles
        self.i = 0

    def next(self):
        t = self.tiles[self.i % len(self.tiles)]
        self.i += 1
        return t


LIMIT_NT = None
DBG = dict(c_stop=9, b_passes=None, b_blocks=None, act_dma=False, split_exp=True)


def build(S, L, n_exp=NE, phases="ABCD", dbg=False):
    NT = S // 128
    NTL = NT if LIMIT_NT is None else min(NT, LIMIT_NT)
    TS = min(2048, S)
    nc = bass.Bass("TRN2", target_bir_lowering=False)
    dram = lambda name, shape, dt, kind="ExternalInput": nc.dram_tensor(name, shape, dt, kind=kind).ap()
    x_in = dram("x", [S, D], F32)
    pos_t = dram("pos_t", [128, NT], I32)
    c_ident = dram("c_ident", [128, 128], F32)
    c_mask = dram("c_mask", [128, 4, 128], F32)
    c_invf = dram("c_invf", [128, 8], F32)
    gvec = dram("gvec", [L, 128, 3, D], F32)
    gqk = dram("gqk", [L, 128, 26 * HD], F32)
    sinks = dram("sinks", [L, 128, NH], F32)
    w_in = dram("w_in", [L, D, PW], F32)
    w_out = dram("w_out", [L, D, D], F32)
    w_r = dram("w_r", [L, D, 36], F32)
    w_gu = dram("w_gu", [L, NE, D, 2 * FE], F32)
    w_dn = dram("w_dn", [L, NE, FE, D], F32)
    out = dram("out", [S, D], F32, kind="ExternalOutput")
    okind = "ExternalOutput" if dbg else "Internal"
    QKV = dram("QKV", [S, PW], BF16, kind=okind)
    ACC = dram("ACC", [4, S, NH * 65], F32, kind=okind)
    XM = dram("XM", [S, D], F32, kind=okind)
    H2T = dram("H2T", [D, S], BF16, kind=okind)
    GS = dram("GS", [S, NE], F32, kind=okind)
    X1 = dram("X1", [S, D], F32, kind=okind)

    with ExitStack() as es:
        P = Prog(nc, es)

        uid = [0]

        def sb(stack, name, shape, dt):
            uid[0] += 1
            name = "%s_%d" % (name, uid[0])
            return T(stack.enter_context(nc.sbuf_tensor(name, shape, dt)), name)

        def ps(stack, name, shape, dt):
            uid[0] += 1
            name = "%s_%d" % (name, uid[0])
            nfull = 512 if dt == F32 else 1024
            full = stack.enter_context(nc.psum_tensor(name, [128, nfull], dt))
            n = int(np.prod(shape[1:]))
            assert n <= nfull, (name, shape)
            v = full[:, 0:n]
            if len(shape) == 3:
                v = v.rearrange("p (a b) -> p a b", a=shape[1], b=shape[2])
            elif len(shape) == 4:
                v = v.rearrange("p (a b c) -> p a b c", a=shape[1], b=shape[2], c=shape[3])
            t = T(v, name)
            t.b.excl = True
            return t

        ident_f = sb(es, "ident_f", [128, 128], F32)
        ident_b = sb(es, "ident_b", [128, 128], BF16)
        mask_f = sb(es, "mask_f", [128, 4, 128], F32)
        mask_b = sb(es, "mask_b", [128, 4, 128], BF16)
        invf = sb(es, "invf", [128, 8], F32)
        cos_t = sb(es, "cos_t", [128, NT, 8], F32)
        sin_t = sb(es, "sin_t", [128, NT, 8], F32)
        P.dma("sp", ident_f.t[:], c_ident[:, :], writes=[ident_f])
        P.dma("sp", mask_f.t[:], c_mask[:, :, :], writes=[mask_f])
        P.dma("sp", invf.t[:], c_invf[:, :], writes=[invf])
        P.op("dve", lambda e: e.tensor_copy(out=ident_b.t[:], in_=ident_f.t[:]), [ident_f], [ident_b])
        P.op("dve", lambda e: e.tensor_copy(out=mask_b.t[:], in_=mask_f.t[:]), [mask_f], [mask_b])
        with ExitStack() as s0:
            pos_i = sb(s0, "pos_i", [128, NT], I32)
            pos_f = sb(s0, "pos_f", [128, NT], F32)
            ang = sb(s0, "ang", [128, NT, 8], F32)
            ang2 = sb(s0, "ang2", [128, NT, 8], F32)
            kf = sb(s0, "kf", [128, NT, 8], F32)
            ki = sb(s0, "ki", [128, NT, 8], I32)
            P.dma("sp", pos_i.t[:], pos_t[:, :], writes=[pos_i])
            P.op("dve", lambda e: e.tensor_copy(out=pos_f.t[:], in_=pos_i.t[:]), [pos_i], [pos_f])
            P.op("dve", lambda e: e.tensor_tensor(
                out=ang.t[:], in0=pos_f.t[:].unsqueeze(2).to_broadcast([128, NT, 8]),
                in1=invf.t[:].unsqueeze(1).to_broadcast([128, NT, 8]), op=ALU.mult), [pos_f, invf], [ang])
            for which, dst in ((0, sin_t), (1, cos_t)):
                src = ang
                if which == 1:
                    P.op("dve", lambda e: e.tensor_scalar(out=ang2.t[:], in0=ang.t[:], scalar1=float(np.pi / 2),
                                                          scalar2=None, op0=ALU.add), [ang], [ang2])
                    src = ang2
                P.op("dve", lambda e: e.tensor_scalar(out=kf.t[:], in0=src.t[:], scalar1=float(1.0 / (2 * np.pi)),
                                                      scalar2=None, op0=ALU.mult), [src], [kf])
                P.op("dve", lambda e: e.tensor_copy(out=ki.t[:], in_=kf.t[:]), [kf], [ki])
                P.op("dve", lambda e: e.tensor_copy(out=kf.t[:], in_=ki.t[:]), [ki], [kf])
                P.op("dve", lambda e: e.scalar_tensor_tensor(out=dst.t[:], in0=kf.t[:], scalar=-TWO_PI_HI, in1=src.t[:],
                                                             op0=ALU.mult, op1=ALU.add), [kf, src], [dst])
                P.op("dve", lambda e: e.scalar_tensor_tensor(out=dst.t[:], in0=kf.t[:], scalar=-TWO_PI_LO, in1=dst.t[:],
                                                             op0=ALU.mult, op1=ALU.add), [kf, dst], [dst])
                P.op("dve", lambda e: e.tensor_scalar(out=dst.t[:], in0=dst.t[:], scalar1=float(np.pi), scalar2=float(-np.pi),
                                                      op0=ALU.min, op1=ALU.max), [dst], [dst])
                P.op("act", lambda e: e.activation(out=dst.t[:], in_=dst.t[:], func=AF.Sin), [dst], [dst])
            P.barrier()

        def rstd_from_ss(ss_ap, n_feat, out_t, tmp_t, deps_r):
            P.op("dve", lambda e: e.tensor_scalar(out=tmp_t.t[:], in0=ss_ap, scalar1=1.0 / n_feat, scalar2=EPS,
                                                  op0=ALU.mult, op1=ALU.add), deps_r, [tmp_t])
            P.op("act", lambda e: e.activation(out=tmp_t.t[:], in_=tmp_t.t[:], func=AF.Sqrt), [tmp_t], [tmp_t])
            P.op("dve", lambda e: e.reciprocal(out=out_t.t[:], in_=tmp_t.t[:]), [tmp_t], [out_t])

        for l in range(L):
            xsrc = x_in if l == 0 else X1
            xdst = out if l == L - 1 else X1
            if "A" in phases:
              with ExitStack() as sa:
                win = sb(sa, "win", [128, 8, PW], BF16)
                g_attn = sb(sa, "g_attn", [128, D], F32)
                g_qk = sb(sa, "g_qk", [128, 26, HD], F32)
                for c in range(8):
                    P.dma("pool", win.t[:, c, :], w_in[l, c * 128:(c + 1) * 128, :], writes=[win])
                P.dma("sp", g_attn.t[:], gvec[l, :, 0, :], writes=[g_attn])
                P.dma("sp", g_qk.t[:].rearrange("p h d -> p (h d)"), gqk[l, :, :], writes=[g_qk])
                xt_r = Ring([sb(sa, "xt%d" % i, [128, D], F32) for i in range(2)])
                junk = sb(sa, "junkA", [128, D], BF16)
                xg_r = Ring([sb(sa, "xg%d" % i, [128, D], BF16) for i in range(2)])
                xT_r = Ring([sb(sa, "xT%d" % i, [128, 8, 128], BF16) for i in range(3)])
                ss_r = Ring([sb(sa, "ssA%d" % i, [128, 1], F32) for i in range(3)])
                rs_r = Ring([sb(sa, "rsA%d" % i, [128, 1], F32) for i in range(3)])
                tm_r = Ring([sb(sa, "tmA%d" % i, [128, 1], F32) for i in range(3)])
                proj_r = Ring([sb(sa, "proj%d" % i, [128, PW], F32) for i in range(3)])
                sq_r = Ring([sb(sa, "sqA%d" % i, [128, 26, HD], F32) for i in range(3)])
                ssh_r = Ring([sb(sa, "ssh%d" % i, [128, 26], F32) for i in range(3)])
                rsh_r = Ring([sb(sa, "rsh%d" % i, [128, 26], F32) for i in range(3)])
                tmh_r = Ring([sb(sa, "tmh%d" % i, [128, 26], F32) for i in range(3)])
                rr_r = Ring([[sb(sa, "r%d_%d" % (k, i), [128, 26, 8], F32) for k in range(4)] for i in range(2)])
                qkvb_r = Ring([sb(sa, "qkvb%d" % i, [128, PW], BF16) for i in range(2)])
                pT_r = Ring([ps(sa, "pTA%d" % i, [128, 8, 128], BF16) for i in range(2)])
                pP_r = [Ring([ps(sa, "pP%d_%d" % (g, i), [128, 512], F32) for i in range(1)]) for g in range(5)]

                def bodyA(i):
                    sq = sq_r.next(); ssh = ssh_r.next(); rsh = rsh_r.next(); tmh = tmh_r.next()
                    r1, r2, r3, r4 = rr_r.next()
                    xt = xt_r.next(); xg = xg_r.next(); xT = xT_r.next(); ss = ss_r.next(); rs = rs_r.next()
                    tm = tm_r.next(); proj = proj_r.next(); qkvb = qkvb_r.next(); pT = pT_r.next()
                    P.dma("sp", xt.t[:], xsrc[i * 128:(i + 1) * 128, :], writes=[xt])
                    P.op("act", lambda e: e.activation(out=junk.t[:], in_=xt.t[:], func=AF.Square, accum_out=ss.t[:]),
                         [xt], [junk, ss])
                    P.op("dve", lambda e: e.tensor_tensor(out=xg.t[:], in0=xt.t[:], in1=g_attn.t[:], op=ALU.mult),
                         [xt, g_attn], [xg])

                    def tr(e):
                        for c in range(8):
                            ins = e.transpose(pT.t[:, c, :], xg.t[:, c * 128:(c + 1) * 128], ident_b.t[:])
                        return ins
                    P.op("pe", tr, [xg, ident_b], [pT])
                    P.op("act", lambda e: e.copy(out=xT.t[:], in_=pT.t[:]), [pT], [xT])
                    rstd_from_ss(ss.t[:], D, rs, tm, [ss])
                    yield
                    for g in range(5):
                        w = 512 if g < 4 else 256
                        pP = pP_r[g].next()

                        def mm(e):
                            for c in range(8):
                                ins = e.matmul(pP.t[:, 0:w], lhsT=xT.t[:, c, :], rhs=win.t[:, c, g * 512:g * 512 + w],
                                               start=(c == 0), stop=(c == 7))
                            return ins
                        P.op("pe", mm, [xT, win], [pP])
                        eng = "act" if g % 2 == 0 else "dve"
                        if eng == "act":
                            P.op("act", lambda e: e.activation(out=proj.t[:, g * 512:g * 512 + w], in_=pP.t[:, 0:w],
                                                               func=AF.Copy, scale=rs.t[:, 0:1]), [pP, rs], [proj])
                        else:
                            P.op("dve", lambda e: e.tensor_scalar(out=proj.t[:, g * 512:g * 512 + w], in0=pP.t[:, 0:w],
                                                                  scalar1=rs.t[:, 0:1], scalar2=None, op0=ALU.mult),
                                 [pP, rs], [proj])
                    qk = proj.t[:, 0:26 * HD].rearrange("p (h d) -> p h d", d=HD)
                    P.op("pool", lambda e: e.tensor_tensor(out=sq.t[:], in0=qk, in1=qk, op=ALU.mult), [proj], [sq])
                    P.op("dve", lambda e: e.tensor_reduce(out=ssh.t[:], in_=sq.t[:], axis=AX.X, op=ALU.add), [sq], [ssh])
                    rstd_from_ss(ssh.t[:], HD, rsh, tmh, [ssh])
                    yield
                    P.op("dve", lambda e: e.tensor_tensor(out=sq.t[:], in0=qk,
                                                          in1=rsh.t[:].unsqueeze(2).to_broadcast([128, 26, HD]),
                                                          op=ALU.mult), [proj, rsh], [sq])
                    P.op("pool", lambda e: e.tensor_tensor(out=sq.t[:], in0=sq.t[:], in1=g_qk.t[:], op=ALU.mult),
                         [sq, g_qk], [sq])
                    P.op("act", lambda e: e.copy(out=qkvb.t[:, 0:26 * HD], in_=sq.t[:].rearrange("p h d -> p (h d)")),
                         [sq], [qkvb])
                    P.op("pool", lambda e: e.tensor_copy(out=qkvb.t[:, 26 * HD:PW], in_=proj.t[:, 26 * HD:PW]),
                         [proj], [qkvb])
                    cb = cos_t.t[:, i, :].unsqueeze(1).to_broadcast([128, 26, 8])
                    sn = sin_t.t[:, i, :].unsqueeze(1).to_broadcast([128, 26, 8])
                    x1 = sq.t[:, :, 0:8]
                    x2 = sq.t[:, :, 8:16]
                    P.op("dve", lambda e: e.tensor_tensor(out=r1.t[:], in0=x1, in1=cb, op=ALU.mult), [sq, cos_t], [r1])
                    P.op("dve", lambda e: e.tensor_tensor(out=r2.t[:], in0=x2, in1=sn, op=ALU.mult), [sq, sin_t], [r2])
                    P.op("pool", lambda e: e.tensor_tensor(out=r3.t[:], in0=x2, in1=cb, op=ALU.mult), [sq, cos_t], [r3])
                    P.op("pool", lambda e: e.tensor_tensor(out=r4.t[:], in0=x1, in1=sn, op=ALU.mult), [sq, sin_t], [r4])
                    qv = qkvb.t[:, 0:26 * HD].rearrange("p (h d) -> p h d", d=HD)
                    P.op("dve", lambda e: e.tensor_tensor(out=qv[:, :, 0:8], in0=r1.t[:], in1=r2.t[:], op=ALU.subtract),
                         [r1, r2], [qkvb])
                    P.op("dve", lambda e: e.tensor_tensor(out=qv[:, :, 8:16], in0=r3.t[:], in1=r4.t[:], op=ALU.add),
                         [r3, r4], [qkvb])
                    P.dma("pool", QKV[i * 128:(i + 1) * 128, :], qkvb.t[:], reads=[qkvb])
                run_pipeline(range(NTL), bodyA)
                P.barrier()

            if "B" in phases:
              with ExitStack() as sbk:
                qrow_r = Ring([sb(sbk, "qrow%d" % i, [128, 512], BF16) for i in range(2)])
                krow_r = Ring([sb(sbk, "krow%d" % i, [128, 512], BF16) for i in range(2)])
                kdup_r = Ring([sb(sbk, "kdup%d" % i, [128, 2, 2, HD], BF16) for i in range(2)])
                vaug_r = Ring([sb(sbk, "vaug%d" % i, [128, NH, VW], BF16) for i in range(5)])
                for v in vaug_r.tiles:
                    P.op("pool", lambda e: e.memset(v.t[:], 1.0), [], [v])
                qT_r = Ring([sb(sbk, "qT%d" % i, [128, 4, 2, 128], BF16) for i in range(4)])
                for v in qT_r.tiles:
                    P.op("pool", lambda e: e.memset(v.t[:], 0.0), [], [v])
                kT_r = Ring([sb(sbk, "kT%d" % i, [128, 4, 128], BF16) for i in range(5)])
                pt_r = Ring([sb(sbk, "pt%d" % i, [128, 4, 2, 128], BF16) for i in range(3)])
                osb_r = Ring([sb(sbk, "osb%d" % i, [128, NH, 65], F32) for i in range(3)])
                pTq = ps(sbk, "pTq", [128, 4, 128], BF16)
                pTk = ps(sbk, "pTk", [128, 4, 128], BF16)
                pS_r = Ring([(ps(sbk, "pSa%d" % i, [128, 2, 2, 128], F32), ps(sbk, "pSb%d" % i, [128, 2, 2, 128], F32)) for i in range(2)])
                pO_r = Ring([ps(sbk, "pO%d" % i, [128, 4, VW], F32) for i in range(2)])
                passes = [(0, 1, r) for r in range(1)] + [(1, 4, r) for r in range(4)] + \
                         [(2, 16, r) for r in range(16)] + [(3, 1, 0)]
                if DBG['b_passes'] is not None:
                    passes = passes[:DBG['b_passes']]
                items = []
                for (pi, d, r) in passes:
                    nb = S // d // 128
                    if DBG['b_blocks'] is not None:
                        nb = min(nb, DBG['b_blocks'])
                    items += [(pi, d, r, j) for j in range(nb)]
                state = dict(prev=None)

                def bodyB(pi, d, r, j):
                    isB = (pi == 3)
                    qc0 = 512 if isB else 0
                    kc0, kw = (1536, 128) if isB else (1024, 512)
                    vc0, nvh = (2176, 2) if isB else (1664, 8)
                    rows = QKV.rearrange("(m d) c -> d m c", d=d)
                    accv = ACC.rearrange("a (m d) c -> a d m c", d=d)
                    qrow = qrow_r.next(); krow = krow_r.next(); vaug = vaug_r.next(); qT = qT_r.next()
                    kT = kT_r.next(); osb = osb_r.next()
                    prev = state['prev'] if j > 0 else None
                    state['prev'] = (kT, vaug)
                    rs_ = slice(j * 128, (j + 1) * 128)
                    P.dma("sp", qrow.t[:], rows[r, rs_, qc0:qc0 + 512], writes=[qrow])
                    P.dma("sp", krow.t[:, 0:kw], rows[r, rs_, kc0:kc0 + kw], writes=[krow])
                    P.dma("act" if DBG['act_dma'] else "sp", vaug.t[:, 0:nvh, 0:HD],
                          rows[r, rs_, vc0:vc0 + nvh * HD].rearrange("p (h d) -> p h d", d=HD), writes=[vaug])

                    def trq(e):
                        for c in range(4):
                            ins = e.transpose(pTq.t[:, c, :], qrow.t[:, c * 128:(c + 1) * 128], ident_b.t[:])
                        return ins
                    P.op("pe", trq, [qrow, ident_b], [pTq])
                    P.op("dve", lambda e: e.tensor_copy(out=qT.t[0:64, :, 0, :], in_=pTq.t[0:64, :, :]), [pTq], [qT])
                    P.op("dve", lambda e: e.tensor_copy(out=qT.t[64:128, :, 1, :], in_=pTq.t[64:128, :, :]), [pTq], [qT])
                    if isB:
                        kdup = kdup_r.next()
                        kv = krow.t[:, 0:128].rearrange("p (g d) -> p g d", d=HD)
                        P.op("pool", lambda e: e.tensor_copy(
                            out=kdup.t[:], in_=kv.unsqueeze(2).to_broadcast([128, 2, 2, HD])), [krow], [kdup])

                        def trk(e):
                            for c in range(2):
                                ins = e.transpose(pTk.t[:, c, :], kdup.t[:, c, :, :].rearrange("p a d -> p (a d)"),
                                                  ident_b.t[:])
                            return ins
                        P.op("pe", trk, [kdup, ident_b], [pTk])
                        P.op("act", lambda e: e.copy(out=kT.t[:, 0:2, :], in_=pTk.t[:, 0:2, :]), [pTk], [kT])
                    else:
                        def trk(e):
                            for c in range(4):
                                ins = e.transpose(pTk.t[:, c, :], krow.t[:, c * 128:(c + 1) * 128], ident_b.t[:])
                            return ins
                        P.op("pe", trk, [krow, ident_b], [pTk])
                        P.op("act", lambda e: e.copy(out=kT.t[:], in_=pTk.t[:]), [pTk], [kT])
                    for hg in range(2):
                        yield
                        pS = pS_r.next(); pO = pO_r.next(); pt = pt_r.next()
                        s0 = 0 if prev is not None else 1

                        def kslice(kt, h):
                            if isB:
                                return kt.t[:, h // 4, :]
                            return kt.t[:, h // 2, :]

                        def sc(e):
                            for hh in range(4):
                                h = hg * 4 + hh
                                qs = qT.t[:, h // 2, h % 2, :]
                                pSh = pS[hh // 2]
                                if prev is not None:
                                    e.matmul(pSh.t[:, hh % 2, 0, :], lhsT=kslice(prev[0], h), rhs=qs, start=True, stop=True)
                                ins = e.matmul(pSh.t[:, hh % 2, 1, :], lhsT=kslice(kT, h), rhs=qs, start=True, stop=True)
                            return ins
                        rd = [qT, kT] + ([prev[0]] if prev is not None else [])
                        P.op("pe", sc, rd, [pS[0], pS[1]])
                        for hh2 in range(2):
                            P.op("act", lambda e: e.activation(
                                out=pt.t[:, 2 * hh2:2 * hh2 + 2, s0:2, :], in_=pS[hh2].t[:, :, s0:2, :],
                                func=AF.Exp, scale=0.125), [pS[hh2]], [pt])
                        mk = mask_b.t[:, 2 * (1 if isB else 0) + s0:2 * (1 if isB else 0) + 2, :]
                        meng = "dve" if hg == 0 else "pool"
                        P.op(meng, lambda e: e.tensor_tensor(
                            out=pt.t[:, :, s0:2, :], in0=pt.t[:, :, s0:2, :],
                            in1=mk.unsqueeze(1).to_broadcast([128, 4, 2 - s0, 128]), op=ALU.mult), [pt, mask_b], [pt])

                        def pv(e):
                            for hh in range(4):
                                h = hg * 4 + hh
                                vh = (h // 4) if isB else h
                                if prev is not None:
                                    e.matmul(pO.t[:, hh, :], lhsT=pt.t[:, hh, 0, :], rhs=prev[1].t[:, vh, :],
                                             start=True, stop=False)
                                ins = e.matmul(pO.t[:, hh, :], lhsT=pt.t[:, hh, 1, :], rhs=vaug.t[:, vh, :],
                                               start=(prev is None), stop=True)
                            return ins
                        rd = [pt, vaug] + ([prev[1]] if prev is not None else [])
                        P.op("pe", pv, rd, [pO])
                        if hg == 0:
                            P.op("act", lambda e: e.copy(out=osb.t[:, 0:4, :], in_=pO.t[:, :, 0:65]), [pO], [osb])
                        else:
                            P.op("dve", lambda e: e.tensor_copy(out=osb.t[:, 4:8, :], in_=pO.t[:, :, 0:65]), [pO], [osb])
                    P.dma("pool", accv[pi, r, rs_, :], osb.t[:].rearrange("p h d -> p (h d)"), reads=[osb])
                run_pipeline(items, bodyB)
                P.barrier()

            if "C" in phases:
              with ExitStack() as sc_:
                wout = sb(sc_, "wout", [128, 8, D], BF16)
                wr = sb(sc_, "wr", [128, 8, 36], F32)
                g_out = sb(sc_, "g_out", [128, D], F32)
                g_ffn = sb(sc_, "g_ffn", [128, D], F32)
                esink = sb(sc_, "esink", [128, NH], F32)
                for c in range(8):
                    P.dma("pool", wout.t[:, c, :], w_out[l, c * 128:(c + 1) * 128, :], writes=[wout])
                    P.dma("sp", wr.t[:, c, :], w_r[l, c * 128:(c + 1) * 128, :], writes=[wr])
                P.dma("sp", g_out.t[:], gvec[l, :, 1, :], writes=[g_out])
                P.dma("sp", g_ffn.t[:], gvec[l, :, 2, :], writes=[g_ffn])
                P.dma("sp", esink.t[:], sinks[l, :, :], writes=[esink])
                P.op("act", lambda e: e.activation(out=esink.t[:], in_=esink.t[:], func=AF.Exp), [esink], [esink])
                acc_rC = Ring([sb(sc_, "accC%d" % i, [128, 4, NH, 65], F32) for i in range(2)])
                xt_rC = Ring([sb(sc_, "xtC%d" % i, [128, D], F32) for i in range(3)])
                den_rC = Ring([sb(sc_, "denC%d" % i, [128, 2, NH], F32) for i in range(2)])
                o_t_rC = Ring([sb(sc_, "o_tC%d" % i, [128, 2, NH, HD], F32) for i in range(2)])
                ss2_rC = Ring([sb(sc_, "ss2C%d" % i, [128, 2], F32) for i in range(2)])
                rs2_rC = Ring([sb(sc_, "rs2C%d" % i, [128, 2], F32) for i in range(2)])
                tm2_rC = Ring([sb(sc_, "tm2C%d" % i, [128, 2], F32) for i in range(2)])
                mix_rC = Ring([sb(sc_, "mixC%d" % i, [128, D], BF16) for i in range(3)])
                mT_rC = Ring([sb(sc_, "mTC%d" % i, [128, 8, 128], BF16) for i in range(2)])
                xm_rC = Ring([sb(sc_, "xmC%d" % i, [128, D], F32) for i in range(2)])
                ss3_rC = Ring([sb(sc_, "ss3C%d" % i, [128, 1], F32) for i in range(2)])
                rs3_rC = Ring([sb(sc_, "rs3C%d" % i, [128, 1], F32) for i in range(2)])
                tm3_rC = Ring([sb(sc_, "tm3C%d" % i, [128, 1], F32) for i in range(2)])
                h2_rC = Ring([sb(sc_, "h2C%d" % i, [128, D], F32) for i in range(3)])
                h2Tf_rC = Ring([sb(sc_, "h2TfC%d" % i, [128, 8, 128], F32) for i in range(2)])
                h2Tb_rC = Ring([sb(sc_, "h2TbC%d" % i, [128, 8, 128], BF16) for i in range(2)])
                junk = sb(sc_, "junkC", [128, D], BF16)
                LG = sb(sc_, "LG", [128, NT, 36], F32)
                gmax = sb(sc_, "gmax", [128, NT], F32)
                gtop = sb(sc_, "gtop", [128, NT], F32)
                m1 = sb(sc_, "m1", [128, NT], F32)
                m2 = sb(sc_, "m2", [128, NT], F32)
                ex4 = sb(sc_, "ex4", [128, NT, 4], F32)
                gm = sb(sc_, "gm", [128, NT, 4], F32)
                lem = sb(sc_, "lem", [128, NT, 32], F32)
                m1k = sb(sc_, "m1k", [128, NT, 32], F32)
                m2k = sb(sc_, "m2k", [128, NT, 32], F32)
                pTm = ps(sc_, "pTm", [128, 8, 128], BF16)
                pOa = ps(sc_, "pOa", [128, 512], F32)
                pOb = ps(sc_, "pOb", [128, 512], F32)
                pTh = [ps(sc_, "pTh%d" % i, [128, 4, 128], F32) for i in range(2)]
                pR = ps(sc_, "pR", [128, 36], F32)
                def bodyC(i):
                    acc = acc_rC.next()
                    xt = xt_rC.next()
                    den = den_rC.next()
                    o_t = o_t_rC.next()
                    ss2 = ss2_rC.next()
                    rs2 = rs2_rC.next()
                    tm2 = tm2_rC.next()
                    mix = mix_rC.next()
                    mT = mT_rC.next()
                    xm = xm_rC.next()
                    ss3 = ss3_rC.next()
                    rs3 = rs3_rC.next()
                    tm3 = tm3_rC.next()
                    h2 = h2_rC.next()
                    h2Tf = h2Tf_rC.next()
                    h2Tb = h2Tb_rC.next()
                    rs_ = slice(i * 128, (i + 1) * 128)
                    for a in range(4):
                        P.dma("sp", acc.t[:, a, :, :].rearrange("p h d -> p (h d)"),
                              ACC[a, rs_, :], writes=[acc])
                    P.dma("sp", xt.t[:], xsrc[rs_, :], writes=[xt])
                    P.op("dve", lambda e: e.tensor_tensor(out=acc.t[:, 0], in0=acc.t[:, 0], in1=acc.t[:, 1], op=ALU.add),
                         [acc], [acc])
                    P.op("dve", lambda e: e.tensor_tensor(out=acc.t[:, 0], in0=acc.t[:, 0], in1=acc.t[:, 2], op=ALU.add),
                         [acc], [acc])
                    P.op("dve", lambda e: e.tensor_copy(out=den.t[:, 0, :], in_=acc.t[:, 0, :, 64]), [acc], [den])
                    P.op("dve", lambda e: e.tensor_tensor(out=den.t[:, 1, :], in0=acc.t[:, 3, :, 64], in1=esink.t[:],
                                                          op=ALU.add), [acc, esink], [den])
                    P.op("dve", lambda e: e.reciprocal(out=den.t[:], in_=den.t[:]), [den], [den])
                    P.op("dve", lambda e: e.tensor_tensor(
                        out=o_t.t[:, 0], in0=acc.t[:, 0, :, 0:HD],
                        in1=den.t[:, 0, :].unsqueeze(2).to_broadcast([128, NH, HD]), op=ALU.mult), [acc, den], [o_t])
                    P.op("pool", lambda e: e.tensor_tensor(
                        out=o_t.t[:, 1], in0=acc.t[:, 3, :, 0:HD],
                        in1=den.t[:, 1, :].unsqueeze(2).to_broadcast([128, NH, HD]), op=ALU.mult), [acc, den], [o_t])
                    of = o_t.t[:].rearrange("p a h d -> p (a h d)")
                    for a in range(2):
                        P.op("act", lambda e: e.activation(out=junk.t[:, a * 512:(a + 1) * 512],
                                                           in_=of[:, a * 512:(a + 1) * 512], func=AF.Square,
                                                           accum_out=ss2.t[:, a:a + 1]), [o_t], [junk, ss2])
                    rstd_from_ss(ss2.t[:], 512, rs2, tm2, [ss2])
                    for a in range(2):
                        P.op("dve", lambda e: e.scalar_tensor_tensor(
                            out=mix.t[:, a * 512:(a + 1) * 512], in0=of[:, a * 512:(a + 1) * 512],
                            scalar=rs2.t[:, a:a + 1], in1=g_out.t[:, a * 512:(a + 1) * 512],
                            op0=ALU.mult, op1=ALU.mult), [o_t, rs2, g_out], [mix])

                    yield

                    def trm(e):
                        for c in range(8):
                            ins = e.transpose(pTm.t[:, c, :], mix.t[:, c * 128:(c + 1) * 128], ident_b.t[:])
                        return ins
                    P.op("pe", trm, [mix, ident_b], [pTm])
                    P.op("act", lambda e: e.copy(out=mT.t[:], in_=pTm.t[:]), [pTm], [mT])
                    for hf, pO in ((0, pOa), (1, pOb)):
                        def mmo(e):
                            for c in range(8):
                                ins = e.matmul(pO.t[:], lhsT=mT.t[:, c, :], rhs=wout.t[:, c, hf * 512:(hf + 1) * 512],
                                               start=(c == 0), stop=(c == 7))
                            return ins
                        P.op("pe", mmo, [mT, wout], [pO])
                        P.op("dve", lambda e: e.tensor_tensor(out=xm.t[:, hf * 512:(hf + 1) * 512],
                                                              in0=xt.t[:, hf * 512:(hf + 1) * 512], in1=pO.t[:],
                                                              op=ALU.add), [xt, pO], [xm])
                    P.dma("pool", XM[rs_, :], xm.t[:], reads=[xm])
                    P.op("act", lambda e: e.activation(out=junk.t[:], in_=xm.t[:], func=AF.Square, accum_out=ss3.t[:]),
                         [xm], [junk, ss3])
                    rstd_from_ss(ss3.t[:], D, rs3, tm3, [ss3])
                    P.op("dve", lambda e: e.scalar_tensor_tensor(out=h2.t[:], in0=xm.t[:], scalar=rs3.t[:, 0:1],
                                                                 in1=g_ffn.t[:], op0=ALU.mult, op1=ALU.mult),
                         [xm, rs3, g_ffn], [h2])
                    yield
                    for hf in range(2):
                        def trh(e):
                            for c in range(4):
                                cc = hf * 4 + c
                                ins = e.transpose(pTh[hf].t[:, c, :], h2.t[:, cc * 128:(cc + 1) * 128], ident_f.t[:])
                            return ins
                        P.op("pe", trh, [h2, ident_f], [pTh[hf]])
                        P.op("act", lambda e: e.copy(out=h2Tf.t[:, hf * 4:(hf + 1) * 4, :], in_=pTh[hf].t[:]),
                             [pTh[hf]], [h2Tf])
                        P.op("dve", lambda e: e.tensor_copy(out=h2Tb.t[:, hf * 4:(hf + 1) * 4, :], in_=pTh[hf].t[:]),
                             [pTh[hf]], [h2Tb])
                    P.dma("pool", H2T.rearrange("(c p) t -> p c t", p=128)[:, :, rs_], h2Tb.t[:], reads=[h2Tb])


                    def mmr(e):
                        for c in range(8):
                            ins = e.matmul(pR.t[:], lhsT=h2Tf.t[:, c, :], rhs=wr.t[:, c, :], start=(c == 0), stop=(c == 7))
                        return ins
                    P.op("pe", mmr, [h2Tf, wr], [pR])
                    P.op("act", lambda e: e.copy(out=LG.t[:, i, :], in_=pR.t[:]), [pR], [LG])
                run_pipeline(range(NTL), bodyC)
                BIG = 1e30
                NTc = NTL
                lgG = LG.t[:, 0:NTc, 0:4]
                bc4 = lambda t_: t_.t[:, 0:NTc].unsqueeze(2).to_broadcast([128, NTc, 4])
                bc32 = lambda t_: t_.t[:, 0:NTc].unsqueeze(2).to_broadcast([128, NTc, 32])
                P.op("dve", lambda e: e.tensor_reduce(out=gmax.t[:, 0:NTc], in_=lgG, axis=AX.X, op=ALU.max), [LG], [gmax])
                P.op("dve", lambda e: e.tensor_tensor(out=ex4.t[:, 0:NTc, :], in0=lgG, in1=bc4(gmax), op=ALU.subtract),
                     [LG, gmax], [ex4])
                P.op("act", lambda e: e.activation(out=ex4.t[:, 0:NTc, :], in_=ex4.t[:, 0:NTc, :], func=AF.Exp), [ex4], [ex4])
                P.op("dve", lambda e: e.tensor_reduce(out=gtop.t[:, 0:NTc], in_=ex4.t[:, 0:NTc, :], axis=AX.X, op=ALU.add),
                     [ex4], [gtop])
                P.op("dve", lambda e: e.reciprocal(out=gtop.t[:, 0:NTc], in_=gtop.t[:, 0:NTc]), [gtop], [gtop])
                P.op("dve", lambda e: e.tensor_tensor(out=gm.t[:, 0:NTc, :], in0=lgG, in1=bc4(gmax), op=ALU.is_lt),
                     [LG, gmax], [gm])
                P.op("dve", lambda e: e.tensor_scalar(out=gm.t[:, 0:NTc, :], in0=gm.t[:, 0:NTc, :], scalar1=-BIG, scalar2=None,
                                                      op0=ALU.mult), [gm], [gm])
                P.op("dve", lambda e: e.tensor_tensor(
                    out=lem.t[:, 0:NTc, :].rearrange("p t (g e) -> p t g e", e=8),
                    in0=LG.t[:, 0:NTc, 4:36].rearrange("p t (g e) -> p t g e", e=8),
                    in1=gm.t[:, 0:NTc, :].unsqueeze(3).to_broadcast([128, NTc, 4, 8]), op=ALU.add), [LG, gm], [lem])
                lv = lem.t[:, 0:NTc, :]
                P.op("dve", lambda e: e.tensor_reduce(out=m1.t[:, 0:NTc], in_=lv, axis=AX.X, op=ALU.max), [lem], [m1])
                P.op("dve", lambda e: e.tensor_tensor(out=m1k.t[:, 0:NTc, :], in0=lv, in1=bc32(m1), op=ALU.is_ge),
                     [lem, m1], [m1k])
                P.op("dve", lambda e: e.tensor_scalar(out=m2k.t[:, 0:NTc, :], in0=m1k.t[:, 0:NTc, :], scalar1=-BIG, scalar2=None,
                                                      op0=ALU.mult), [m1k], [m2k])
                P.op("dve", lambda e: e.tensor_tensor(out=lv, in0=lv, in1=m2k.t[:, 0:NTc, :], op=ALU.add), [lem, m2k], [lem])
                P.op("dve", lambda e: e.tensor_reduce(out=m2.t[:, 0:NTc], in_=lv, axis=AX.X, op=ALU.max), [lem], [m2])
                P.op("dve", lambda e: e.tensor_tensor(out=m2k.t[:, 0:NTc, :], in0=lv, in1=bc32(m2), op=ALU.is_ge),
                     [lem, m2], [m2k])
                P.op("dve", lambda e: e.tensor_tensor(out=m1.t[:, 0:NTc], in0=m1.t[:, 0:NTc], in1=m2.t[:, 0:NTc],
                                                      op=ALU.subtract), [m1, m2], [m1])
                P.op("act", lambda e: e.activation(out=m1.t[:, 0:NTc], in_=m1.t[:, 0:NTc], func=AF.Exp, scale=-1.0), [m1], [m1])
                P.op("dve", lambda e: e.tensor_scalar(out=m1.t[:, 0:NTc], in0=m1.t[:, 0:NTc], scalar1=1.0, scalar2=None,
                                                      op0=ALU.add), [m1], [m1])
                P.op("dve", lambda e: e.reciprocal(out=m1.t[:, 0:NTc], in_=m1.t[:, 0:NTc]), [m1], [m1])
                P.op("dve", lambda e: e.tensor_tensor(out=m1.t[:, 0:NTc], in0=m1.t[:, 0:NTc], in1=gtop.t[:, 0:NTc],
                                                      op=ALU.mult), [m1, gtop], [m1])
                P.op("dve", lambda e: e.tensor_tensor(out=m2.t[:, 0:NTc], in0=gtop.t[:, 0:NTc], in1=m1.t[:, 0:NTc],
                                                      op=ALU.subtract), [gtop, m1], [m2])
                P.op("dve", lambda e: e.tensor_tensor(out=m1k.t[:, 0:NTc, :], in0=m1k.t[:, 0:NTc, :], in1=bc32(m1),
                                                      op=ALU.mult), [m1k, m1], [m1k])
                P.op("dve", lambda e: e.tensor_tensor(out=m2k.t[:, 0:NTc, :], in0=m2k.t[:, 0:NTc, :], in1=bc32(m2),
                                                      op=ALU.mult), [m2k, m2], [m2k])
                P.op("dve", lambda e: e.tensor_tensor(out=m1k.t[:, 0:NTc, :], in0=m1k.t[:, 0:NTc, :], in1=m2k.t[:, 0:NTc, :],
                                                      op=ALU.add), [m1k, m2k], [m1k])
                P.dma("sp", GS[0:NTc * 128, :].rearrange("(i p) e -> p i e", p=128), m1k.t[:, 0:NTc, :], reads=[m1k])
                P.barrier()

            if "D" in phases:
              with ExitStack() as sd:
                NTS = TS // 128
                hT = sb(sd, "hT", [128, 8, TS], BF16)
                Gt = sb(sd, "Gt", [128, NTS, NE], F32)
                yacc = sb(sd, "yacc", [128, NTS, D], F32)
                wgu_r = Ring([sb(sd, "wgu%d" % i, [128, 8, 2 * FE], BF16) for i in range(2)])
                wdn_r = Ring([sb(sd, "wdn%d" % i, [128, 4, D], BF16) for i in range(2)])
                sg_r = Ring([sb(sd, "sg%d" % i, [128, 512], F32) for i in range(2)])
                aT_r = Ring([sb(sd, "aT%d" % i, [128, 4, 512], BF16) for i in range(2)])
                xm_r = Ring([sb(sd, "xmD%d" % i, [128, D], F32) for i in range(2)])
                pg_r = Ring([ps(sd, "pg%d" % i, [128, 512], F32) for i in range(2)])
                pu_r = Ring([ps(sd, "pu%d" % i, [128, 512], F32) for i in range(2)])
                py_r = Ring([(ps(sd, "pya%d" % i, [128, 512], F32), ps(sd, "pyb%d" % i, [128, 512], F32))
                             for i in range(2)])
                for s_ in range(S // TS):
                    t0 = s_ * TS
                    P.dma("sp", hT.t[:], H2T.rearrange("(c p) t -> p c t", p=128)[:, :, t0:t0 + TS], writes=[hT])
                    P.dma("sp", Gt.t[:], GS[t0:t0 + TS, :].rearrange("(i p) e -> p i e", p=128), writes=[Gt])
                    P.op("dve", lambda e: e.memset(yacc.t[:], 0.0), [], [yacc])
                    for ex in range(n_exp):
                        wgu = wgu_r.next(); wdn = wdn_r.next()
                        for hf in range(2):
                            P.dma("pool", wgu.t[:, hf * 4:(hf + 1) * 4, :],
                                  w_gu[l, ex, hf * 512:(hf + 1) * 512, :].rearrange("(c p) n -> p c n", p=128),
                                  writes=[wgu])
                        P.dma("pool", wdn.t[:], w_dn[l, ex, :, :].rearrange("(c p) n -> p c n", p=128), writes=[wdn])
                        for g in range(TS // 512):
                            aT = aT_r.next()
                            tk = slice(g * 512, (g + 1) * 512)
                            for c in range(4):
                                pg = pg_r.next(); pu = pu_r.next(); sg = sg_r.next()

                                def mmg(e):
                                    for k in range(8):
                                        ins = e.matmul(pg.t[:], lhsT=wgu.t[:, k, c * 128:(c + 1) * 128], rhs=hT.t[:, k, tk],
                                                       start=(k == 0), stop=(k == 7))
                                    return ins

                                def mmu(e):
                                    for k in range(8):
                                        ins = e.matmul(pu.t[:], lhsT=wgu.t[:, k, FE + c * 128:FE + (c + 1) * 128],
                                                       rhs=hT.t[:, k, tk], start=(k == 0), stop=(k == 7))
                                    return ins
                                P.op("pe", mmg, [wgu, hT], [pg])
                                P.op("pe", mmu, [wgu, hT], [pu])
                                P.op("act", lambda e: e.activation(out=sg.t[:], in_=pg.t[:], func=AF.Silu), [pg], [sg])
                                P.op("dve", lambda e: e.tensor_tensor(out=aT.t[:, c, :], in0=sg.t[:], in1=pu.t[:],
                                                                      op=ALU.mult), [sg, pu], [aT])
                            for t in range(4):
                                ti = g * 4 + t
                                pya, pyb = py_r.next()
                                for hf, py in ((0, pya), (1, pyb)):
                                    def mmd(e):
                                        for c in range(4):
                                            ins = e.matmul(py.t[:], lhsT=aT.t[:, c, t * 128:(t + 1) * 128],
                                                           rhs=wdn.t[:, c, hf * 512:(hf + 1) * 512],
                                                           start=(c == 0), stop=(c == 3))
                                        return ins
                                    P.op("pe", mmd, [aT, wdn], [py])
                                    P.op("dve", lambda e: e.scalar_tensor_tensor(
                                        out=yacc.t[:, ti, hf * 512:(hf + 1) * 512], in0=py.t[:],
                                        scalar=Gt.t[:, ti, ex:ex + 1], in1=yacc.t[:, ti, hf * 512:(hf + 1) * 512],
                                        op0=ALU.mult, op1=ALU.add), [py, Gt, yacc], [yacc])
                    for ti in range(NTS):
                        xm = xm_r.next()
                        rs_ = slice(t0 + ti * 128, t0 + (ti + 1) * 128)
                        P.dma("sp", xm.t[:], XM[rs_, :], writes=[xm])
                        P.op("dve", lambda e: e.tensor_tensor(out=xm.t[:], in0=xm.t[:], in1=yacc.t[:, ti, :], op=ALU.add),
                             [xm, yacc], [xm])
                        P.dma("sp", xdst[rs_, :], xm.t[:], reads=[xm])
                P.barrier()
        P.barrier()
    return nc


def _consts():
    k = np.arange(128)[:, None]
    q = np.arange(128)[None, :]
    mask = np.stack([(k >= q), (k <= q), (k > q), (k <= q)], axis=1).astype(np.float32)
    ident = np.eye(128, dtype=np.float32)
    invf = (500000.0 ** (-np.arange(0, 16, 2, dtype=np.float32) / np.float32(16))).astype(np.float32)
    invf = np.ascontiguousarray(np.broadcast_to(invf[None, :], (128, 8)))
    return ident, np.ascontiguousarray(mask), invf


def prep_shared(inputs, L):
    f = lambda k: np.asarray(inputs[k], dtype=np.float32)
    rep = lambda v: np.broadcast_to(v[:, None, :], (L, 128, v.shape[-1]))
    gvec = np.stack([rep(f("attn_norm")), rep(np.concatenate([f("out_norm_a"), f("out_norm_b")], axis=-1)),
                     rep(f("ffn_norm"))], axis=2)
    qa, ka, qb, kb = f("q_norm_a"), f("k_norm_a"), f("q_norm_b"), f("k_norm_b")
    gq = np.concatenate([np.tile(qa, (1, 8)), np.tile(qb, (1, 8)), np.tile(ka, (1, 8)), np.tile(kb, (1, 2))], axis=-1)
    w = f("w_in")
    w_in_p = np.concatenate([w[:, :, 0:512], w[:, :, 1536:2048], w[:, :, 512:1024], w[:, :, 2048:2176],
                             w[:, :, 1024:1536], w[:, :, 2176:2304]], axis=-1)
    ident, mask, invf = _consts()
    return dict(
        c_ident=ident, c_mask=mask, c_invf=invf,
        gvec=np.ascontiguousarray(gvec), gqk=np.ascontiguousarray(rep(gq)),
        sinks=np.ascontiguousarray(rep(f("sinks_b"))),
        w_in=np.ascontiguousarray(w_in_p), w_out=f("w_out"),
        w_r=np.ascontiguousarray(np.concatenate([f("w_router_group"), f("w_router_expert")], axis=-1)),
        w_gu=f("w_gate_up"), w_dn=f("w_down"))


def core_map(shared, x_b, pos_b):
    S = x_b.shape[0]
    m = dict(shared)
    m["x"] = np.ascontiguousarray(x_b, dtype=np.float32)
    m["pos_t"] = np.ascontiguousarray(np.asarray(pos_b, dtype=np.int32).reshape(S // 128, 128).T)
    return m


def kernel(**inputs):
    x = np.asarray(inputs["x"])
    pos = np.asarray(inputs["positions"])
    B, S, _ = x.shape
    L = np.asarray(inputs["attn_norm"]).shape[0]
    shared = prep_shared(inputs, L)
    nc = build(S, L)
    in_maps = [core_map(shared, x[b], pos[b]) for b in range(B)]
    res = run_bass_kernel_spmd(nc, in_maps, core_ids=list(range(B)))
    return np.stack([np.asarray(r["out"]).reshape(S, D) for r in res.results], axis=0).astype(np.float32)
```

```python
import numpy as np
from contextlib import ExitStack
import concourse.bass as bass
import concourse.mybir as mybir
from concourse.bass_utils import run_bass_kernel_spmd

F32 = mybir.dt.float32
BF16 = mybir.dt.bfloat16
I32 = mybir.dt.int32
ALU = mybir.AluOpType
AF = mybir.ActivationFunctionType
AX = mybir.AxisListType

D = 1024
PW = 2304
NH = 8
HD = 64
NE = 32
FE = 512
VW = 80
EPS = 1e-6
TWO_PI_HI = 6.28125
TWO_PI_LO = 2.0 * np.pi - 6.28125


class Buf:
    __slots__ = ("name", "w", "r", "excl")

    def __init__(self, name):
        self.name = name
        self.w = None
        self.r = {}
        self.excl = False


class T:
    def __init__(self, t, name):
        self.t = t
        self.b = Buf(name)


class Prog:
    def __init__(self, nc, es, n_dsem=22):
        self.nc = nc
        self.es = es
        self.eng = dict(pe=nc.tensor, act=nc.scalar, dve=nc.vector, pool=nc.gpsimd, sp=nc.sync)
        self.sem = {e: es.enter_context(nc.semaphore("s_" + e)) for e in ("pe", "act", "dve", "pool")}
        self.cnt = {e: 0 for e in self.sem}
        self.dsem = [es.enter_context(nc.semaphore("d%d" % i)) for i in range(n_dsem)]
        self.dcnt = [0] * n_dsem
        self.dnext = {"hw": 0, "sw": 0}
        self.n_sw = 6
        self.seen = {e: {} for e in self.eng}

    def _semof(self, key):
        return self.sem[key] if isinstance(key, str) else self.dsem[key[1]]

    def _wait(self, e, tok):
        if tok is None:
            return
        key, val = tok
        if key == e and e == "pe":
            return
        if self.seen[e].get(key, 0) >= val:
            return
        self.eng[e].wait_ge(self._semof(key), val)
        self.seen[e][key] = val

    def _deps(self, e, reads, writes):
        for b in reads:
            self._wait(e, b.w)
        for b in writes:
            self._wait(e, b.w)
            for k, v in b.r.items():
                self._wait(e, (k, v))

    def _record(self, tok, reads, writes):
        key, val = tok
        for b in reads:
            if b.r.get(key, 0) < val:
                b.r[key] = val
        for b in writes:
            b.w = tok
            b.r = {}

    def op(self, e, fn, reads=(), writes=()):
        reads = [x.b if isinstance(x, T) else x for x in reads]
        writes = [x.b if isinstance(x, T) else x for x in writes]
        writes = writes + [b for b in reads if b.excl]
        reads = [b for b in reads if not b.excl]
        self._deps(e, reads, writes)
        ins = fn(self.eng[e])
        self.cnt[e] += 1
        ins.then_inc(self.sem[e], 1)
        self._record((e, self.cnt[e]), reads, writes)

    def dma(self, q, out, in_, reads=(), writes=()):
        reads = [x.b if isinstance(x, T) else x for x in reads]
        writes = [x.b if isinstance(x, T) else x for x in writes]
        if q == "pool":
            i = self.dnext["sw"]
            self.dnext["sw"] = (i + 1) % self.n_sw
        else:
            i = self.n_sw + self.dnext["hw"]
            self.dnext["hw"] = (self.dnext["hw"] + 1) % (len(self.dsem) - self.n_sw)
        if self.dcnt[i]:
            self._wait(q, (("d", i), self.dcnt[i]))
        self._deps(q, reads, writes)
        ins = self.eng[q].dma_start(out=out, in_=in_)
        self.dcnt[i] += 16
        ins.then_inc(self.dsem[i], 16)
        self._record((("d", i), self.dcnt[i]), reads, writes)

    def barrier(self):
        for e in self.eng:
            for o in self.sem:
                if o != e and self.cnt[o]:
                    self._wait(e, (o, self.cnt[o]))
            for i, c in enumerate(self.dcnt):
                if c:
                    self._wait(e, (("d", i), c))

    def final_wait(self):
        for i, c in enumerate(self.dcnt):
            if c:
                self._wait("sp", (("d", i), c))


def run_pipeline(items, body):
    gens = []
    it = iter(items)
    pending = True
    while pending or gens:
        nxt = next(it, None) if pending else None
        if nxt is None:
            pending = False
        else:
            gens.append(body(*nxt) if isinstance(nxt, tuple) else body(nxt))
        alive = []
        for g in gens:
            try:
                next(g)
                alive.append(g)
            except StopIteration:
                pass
        gens = alive


class Ring:
    def __init__(self, tiles):
        self.tiles = tiles
        self.i = 0

    def next(self):
        t = self.tiles[self.i % len(self.tiles)]
        self.i += 1
        return t


LIMIT_NT = None
DBG = dict(c_stop=9, b_passes=None, b_blocks=None, act_dma=False, split_exp=True)


def build(S, L, n_exp=NE, phases="ABCD", dbg=False):
    NT = S // 128
    NTL = NT if LIMIT_NT is None else min(NT, LIMIT_NT)
    TS = min(2048, S)
    nc = bass.Bass("TRN2", target_bir_lowering=False)
    dram = lambda name, shape, dt, kind="ExternalInput": nc.dram_tensor(name, shape, dt, kind=kind).ap()
    x_in = dram("x", [S, D], F32)
    pos_t = dram("pos_t", [128, NT], I32)
    c_ident = dram("c_ident", [128, 128], F32)
    c_mask = dram("c_mask", [128, 4, 128], F32)
    c_invf = dram("c_invf", [128, 8], F32)
    gvec = dram("gvec", [L, 128, 3, D], F32)
    gqk = dram("gqk", [L, 128, 26 * HD], F32)
    sinks = dram("sinks", [L, 128, NH], F32)
    w_in = dram("w_in", [L, D, PW], F32)
    w_out = dram("w_out", [L, D, D], F32)
    w_r = dram("w_r", [L, D, 36], F32)
    w_gu = dram("w_gu", [L, NE, D, 2 * FE], F32)
    w_dn = dram("w_dn", [L, NE, FE, D], F32)
    out = dram("out", [S, D], F32, kind="ExternalOutput")
    okind = "ExternalOutput" if dbg else "Internal"
    QKV = dram("QKV", [S, PW], BF16, kind=okind)
    ACC = dram("ACC", [4, S, NH * 65], F32, kind=okind)
    XM = dram("XM", [S, D], F32, kind=okind)
    H2T = dram("H2T", [D, S], BF16, kind=okind)
    GS = dram("GS", [S, NE], F32, kind=okind)
    X1 = dram("X1", [S, D], F32, kind=okind)

    with ExitStack() as es:
        P = Prog(nc, es)

        uid = [0]

        def sb(stack, name, shape, dt):
            uid[0] += 1
            name = "%s_%d" % (name, uid[0])
            return T(stack.enter_context(nc.sbuf_tensor(name, shape, dt)), name)

        def ps(stack, name, shape, dt):
            uid[0] += 1
            name = "%s_%d" % (name, uid[0])
            nfull = 512 if dt == F32 else 1024
            full = stack.enter_context(nc.psum_tensor(name, [128, nfull], dt))
            n = int(np.prod(shape[1:]))
            assert n <= nfull, (name, shape)
            v = full[:, 0:n]
            if len(shape) == 3:
                v = v.rearrange("p (a b) -> p a b", a=shape[1], b=shape[2])
            elif len(shape) == 4:
                v = v.rearrange("p (a b c) -> p a b c", a=shape[1], b=shape[2], c=shape[3])
            t = T(v, name)
            t.b.excl = True
            return t

        ident_f = sb(es, "ident_f", [128, 128], F32)
        ident_b = sb(es, "ident_b", [128, 128], BF16)
        mask_f = sb(es, "mask_f", [128, 4, 128], F32)
        mask_b = sb(es, "mask_b", [128, 4, 128], BF16)
        invf = sb(es, "invf", [128, 8], F32)
        cos_t = sb(es, "cos_t", [128, NT, 8], F32)
        sin_t = sb(es, "sin_t", [128, NT, 8], F32)
        P.dma("sp", ident_f.t[:], c_ident[:, :], writes=[ident_f])
        P.dma("sp", mask_f.t[:], c_mask[:, :, :], writes=[mask_f])
        P.dma("sp", invf.t[:], c_invf[:, :], writes=[invf])
        P.op("dve", lambda e: e.tensor_copy(out=ident_b.t[:], in_=ident_f.t[:]), [ident_f], [ident_b])
        P.op("dve", lambda e: e.tensor_copy(out=mask_b.t[:], in_=mask_f.t[:]), [mask_f], [mask_b])
        with ExitStack() as s0:
            pos_i = sb(s0, "pos_i", [128, NT], I32)
            pos_f = sb(s0, "pos_f", [128, NT], F32)
            ang = sb(s0, "ang", [128, NT, 8], F32)
            ang2 = sb(s0, "ang2", [128, NT, 8], F32)
            kf = sb(s0, "kf", [128, NT, 8], F32)
            ki = sb(s0, "ki", [128, NT, 8], I32)
            P.dma("sp", pos_i.t[:], pos_t[:, :], writes=[pos_i])
            P.op("dve", lambda e: e.tensor_copy(out=pos_f.t[:], in_=pos_i.t[:]), [pos_i], [pos_f])
            P.op("dve", lambda e: e.tensor_tensor(
                out=ang.t[:], in0=pos_f.t[:].unsqueeze(2).to_broadcast([128, NT, 8]),
                in1=invf.t[:].unsqueeze(1).to_broadcast([128, NT, 8]), op=ALU.mult), [pos_f, invf], [ang])
            for which, dst in ((0, sin_t), (1, cos_t)):
                src = ang
                if which == 1:
                    P.op("dve", lambda e: e.tensor_scalar(out=ang2.t[:], in0=ang.t[:], scalar1=float(np.pi / 2),
                                                          scalar2=None, op0=ALU.add), [ang], [ang2])
                    src = ang2
                P.op("dve", lambda e: e.tensor_scalar(out=kf.t[:], in0=src.t[:], scalar1=float(1.0 / (2 * np.pi)),
                                                      scalar2=None, op0=ALU.mult), [src], [kf])
                P.op("dve", lambda e: e.tensor_copy(out=ki.t[:], in_=kf.t[:]), [kf], [ki])
                P.op("dve", lambda e: e.tensor_copy(out=kf.t[:], in_=ki.t[:]), [ki], [kf])
                P.op("dve", lambda e: e.scalar_tensor_tensor(out=dst.t[:], in0=kf.t[:], scalar=-TWO_PI_HI, in1=src.t[:],
                                                             op0=ALU.mult, op1=ALU.add), [kf, src], [dst])
                P.op("dve", lambda e: e.scalar_tensor_tensor(out=dst.t[:], in0=kf.t[:], scalar=-TWO_PI_LO, in1=dst.t[:],
                                                             op0=ALU.mult, op1=ALU.add), [kf, dst], [dst])
                P.op("dve", lambda e: e.tensor_scalar(out=dst.t[:], in0=dst.t[:], scalar1=float(np.pi), scalar2=float(-np.pi),
                                                      op0=ALU.min, op1=ALU.max), [dst], [dst])
                P.op("act", lambda e: e.activation(out=dst.t[:], in_=dst.t[:], func=AF.Sin), [dst], [dst])
            P.barrier()

        def rstd_from_ss(ss_ap, n_feat, out_t, tmp_t, deps_r):
            P.op("dve", lambda e: e.tensor_scalar(out=tmp_t.t[:], in0=ss_ap, scalar1=1.0 / n_feat, scalar2=EPS,
                                                  op0=ALU.mult, op1=ALU.add), deps_r, [tmp_t])
            P.op("act", lambda e: e.activation(out=tmp_t.t[:], in_=tmp_t.t[:], func=AF.Sqrt), [tmp_t], [tmp_t])
            P.op("dve", lambda e: e.reciprocal(out=out_t.t[:], in_=tmp_t.t[:]), [tmp_t], [out_t])

        for l in range(L):
            xsrc = x_in if l == 0 else X1
            xdst = out if l == L - 1 else X1
            if "A" in phases:
              with ExitStack() as sa:
                win = sb(sa, "win", [128, 8, PW], BF16)
                g_attn = sb(sa, "g_attn", [128, D], F32)
                g_qk = sb(sa, "g_qk", [128, 26, HD], F32)
                for c in range(8):
                    P.dma("pool", win.t[:, c, :], w_in[l, c * 128:(c + 1) * 128, :], writes=[win])
                P.dma("sp", g_attn.t[:], gvec[l, :, 0, :], writes=[g_attn])
                P.dma("sp", g_qk.t[:].rearrange("p h d -> p (h d)"), gqk[l, :, :], writes=[g_qk])
                xt_r = Ring([sb(sa, "xt%d" % i, [128, D], F32) for i in range(2)])
                junk = sb(sa, "junkA", [128, D], BF16)
                xg_r = Ring([sb(sa, "xg%d" % i, [128, D], BF16) for i in range(2)])
                xT_r = Ring([sb(sa, "xT%d" % i, [128, 8, 128], BF16) for i in range(3)])
                ss_r = Ring([sb(sa, "ssA%d" % i, [128, 1], F32) for i in range(3)])
                rs_r = Ring([sb(sa, "rsA%d" % i, [128, 1], F32) for i in range(3)])
                tm_r = Ring([sb(sa, "tmA%d" % i, [128, 1], F32) for i in range(3)])
                proj_r = Ring([sb(sa, "proj%d" % i, [128, PW], F32) for i in range(3)])
                sq_r = Ring([sb(sa, "sqA%d" % i, [128, 26, HD], F32) for i in range(3)])
                ssh_r = Ring([sb(sa, "ssh%d" % i, [128, 26], F32) for i in range(3)])
                rsh_r = Ring([sb(sa, "rsh%d" % i, [128, 26], F32) for i in range(3)])
                tmh_r = Ring([sb(sa, "tmh%d" % i, [128, 26], F32) for i in range(3)])
                rr_r = Ring([[sb(sa, "r%d_%d" % (k, i), [128, 26, 8], F32) for k in range(4)] for i in range(2)])
                qkvb_r = Ring([sb(sa, "qkvb%d" % i, [128, PW], BF16) for i in range(2)])
                pT_r = Ring([ps(sa, "pTA%d" % i, [128, 8, 128], BF16) for i in range(2)])
                pP_r = [Ring([ps(sa, "pP%d_%d" % (g, i), [128, 512], F32) for i in range(1)]) for g in range(5)]

                def bodyA(i):
                    sq = sq_r.next(); ssh = ssh_r.next(); rsh = rsh_r.next(); tmh = tmh_r.next()
                    r1, r2, r3, r4 = rr_r.next()
                    xt = xt_r.next(); xg = xg_r.next(); xT = xT_r.next(); ss = ss_r.next(); rs = rs_r.next()
                    tm = tm_r.next(); proj = proj_r.next(); qkvb = qkvb_r.next(); pT = pT_r.next()
                    P.dma("sp", xt.t[:], xsrc[i * 128:(i + 1) * 128, :], writes=[xt])
                    P.op("act", lambda e: e.activation(out=junk.t[:], in_=xt.t[:], func=AF.Square, accum_out=ss.t[:]),
                         [xt], [junk, ss])
                    P.op("dve", lambda e: e.tensor_tensor(out=xg.t[:], in0=xt.t[:], in1=g_attn.t[:], op=ALU.mult),
                         [xt, g_attn], [xg])

                    def tr(e):
                        for c in range(8):
                            ins = e.transpose(pT.t[:, c, :], xg.t[:, c * 128:(c + 1) * 128], ident_b.t[:])
                        return ins
                    P.op("pe", tr, [xg, ident_b], [pT])
                    P.op("act", lambda e: e.copy(out=xT.t[:], in_=pT.t[:]), [pT], [xT])
                    rstd_from_ss(ss.t[:], D, rs, tm, [ss])
                    yield
                    for g in range(5):
                        w = 512 if g < 4 else 256
                        pP = pP_r[g].next()

                        def mm(e):
                            for c in range(8):
                                ins = e.matmul(pP.t[:, 0:w], lhsT=xT.t[:, c, :], rhs=win.t[:, c, g * 512:g * 512 + w],
                                               start=(c == 0), stop=(c == 7))
                            return ins
                        P.op("pe", mm, [xT, win], [pP])
                        eng = "act" if g % 2 == 0 else "dve"
                        if eng == "act":
                            P.op("act", lambda e: e.activation(out=proj.t[:, g * 512:g * 512 + w], in_=pP.t[:, 0:w],
                                                               func=AF.Copy, scale=rs.t[:, 0:1]), [pP, rs], [proj])
                        else:
                            P.op("dve", lambda e: e.tensor_scalar(out=proj.t[:, g * 512:g * 512 + w], in0=pP.t[:, 0:w],
                                                                  scalar1=rs.t[:, 0:1], scalar2=None, op0=ALU.mult),
                                 [pP, rs], [proj])
                    qk = proj.t[:, 0:26 * HD].rearrange("p (h d) -> p h d", d=HD)
                    P.op("pool", lambda e: e.tensor_tensor(out=sq.t[:], in0=qk, in1=qk, op=ALU.mult), [proj], [sq])
                    P.op("dve", lambda e: e.tensor_reduce(out=ssh.t[:], in_=sq.t[:], axis=AX.X, op=ALU.add), [sq], [ssh])
                    rstd_from_ss(ssh.t[:], HD, rsh, tmh, [ssh])
                    yield
                    P.op("dve", lambda e: e.tensor_tensor(out=sq.t[:], in0=qk,
                                                          in1=rsh.t[:].unsqueeze(2).to_broadcast([128, 26, HD]),
                                                          op=ALU.mult), [proj, rsh], [sq])
                    P.op("pool", lambda e: e.tensor_tensor(out=sq.t[:], in0=sq.t[:], in1=g_qk.t[:], op=ALU.mult),
                         [sq, g_qk], [sq])
                    P.op("act", lambda e: e.copy(out=qkvb.t[:, 0:26 * HD], in_=sq.t[:].rearrange("p h d -> p (h d)")),
                         [sq], [qkvb])
                    P.op("pool", lambda e: e.tensor_copy(out=qkvb.t[:, 26 * HD:PW], in_=proj.t[:, 26 * HD:PW]),
                         [proj], [qkvb])
                    cb = cos_t.t[:, i, :].unsqueeze(1).to_broadcast([128, 26, 8])
                    sn = sin_t.t[:, i, :].unsqueeze(1).to_broadcast([128, 26, 8])
                    x1 = sq.t[:, :, 0:8]
                    x2 = sq.t[:, :, 8:16]
                    P.op("dve", lambda e: e.tensor_tensor(out=r1.t[:], in0=x1, in1=cb, op=ALU.mult), [sq, cos_t], [r1])
                    P.op("dve", lambda e: e.tensor_tensor(out=r2.t[:], in0=x2, in1=sn, op=ALU.mult), [sq, sin_t], [r2])
                    P.op("pool", lambda e: e.tensor_tensor(out=r3.t[:], in0=x2, in1=cb, op=ALU.mult), [sq, cos_t], [r3])
                    P.op("pool", lambda e: e.tensor_tensor(out=r4.t[:], in0=x1, in1=sn, op=ALU.mult), [sq, sin_t], [r4])
                    qv = qkvb.t[:, 0:26 * HD].rearrange("p (h d) -> p h d", d=HD)
                    P.op("dve", lambda e: e.tensor_tensor(out=qv[:, :, 0:8], in0=r1.t[:], in1=r2.t[:], op=ALU.subtract),
                         [r1, r2], [qkvb])
                    P.op("dve", lambda e: e.tensor_tensor(out=qv[:, :, 8:16], in0=r3.t[:], in1=r4.t[:], op=ALU.add),
                         [r3, r4], [qkvb])
                    P.dma("pool", QKV[i * 128:(i + 1) * 128, :], qkvb.t[:], reads=[qkvb])
                run_pipeline(range(NTL), bodyA)
                P.barrier()

            if "B" in phases:
              with ExitStack() as sbk:
                qrow_r = Ring([sb(sbk, "qrow%d" % i, [128, 512], BF16) for i in range(2)])
                krow_r = Ring([sb(sbk, "krow%d" % i, [128, 512], BF16) for i in range(2)])
                kdup_r = Ring([sb(sbk, "kdup%d" % i, [128, 2, 2, HD], BF16) for i in range(2)])
                vaug_r = Ring([sb(sbk, "vaug%d" % i, [128, NH, VW], BF16) for i in range(7)])
                for v in vaug_r.tiles:
                    P.op("pool", lambda e: e.memset(v.t[:], 1.0), [], [v])
                qT_r = Ring([sb(sbk, "qT%d" % i, [128, 4, 2, 128], BF16) for i in range(6)])
                for v in qT_r.tiles:
                    P.op("pool", lambda e: e.memset(v.t[:], 0.0), [], [v])
                kT_r = Ring([sb(sbk, "kT%d" % i, [128, 4, 128], BF16) for i in range(7)])
                pt_r = Ring([sb(sbk, "pt%d" % i, [128, 4, 2, 128], BF16) for i in range(6)])
                osb_r = Ring([sb(sbk, "osb%d" % i, [128, NH, 65], F32) for i in range(4)])
                pTq = ps(sbk, "pTq", [128, 4, 128], BF16)
                pTk = ps(sbk, "pTk", [128, 4, 128], BF16)
                pS_r = Ring([(ps(sbk, "pSa%d" % i, [128, 2, 2, 128], F32), ps(sbk, "pSb%d" % i, [128, 2, 2, 128], F32)) for i in range(2)])
                pO_r = Ring([ps(sbk, "pO%d" % i, [128, 4, VW], F32) for i in range(2)])
                passes = [(0, 1, r) for r in range(1)] + [(1, 4, r) for r in range(4)] + \
                         [(2, 16, r) for r in range(16)] + [(3, 1, 0)]
                if DBG['b_passes'] is not None:
                    passes = passes[:DBG['b_passes']]
                items = []
                for (pi, d, r) in passes:
                    nb = S // d // 128
                    if DBG['b_blocks'] is not None:
                        nb = min(nb, DBG['b_blocks'])
                    items += [(pi, d, r, j) for j in range(nb)]
                state = dict(prev=None)

                def bodyB(pi, d, r, j):
                    isB = (pi == 3)
                    qc0 = 512 if isB else 0
                    kc0, kw = (1536, 128) if isB else (1024, 512)
                    vc0, nvh = (2176, 2) if isB else (1664, 8)
                    rows = QKV.rearrange("(m d) c -> d m c", d=d)
                    accv = ACC.rearrange("a (m d) c -> a d m c", d=d)
                    qrow = qrow_r.next(); krow = krow_r.next(); vaug = vaug_r.next(); qT = qT_r.next()
                    kT = kT_r.next(); osb = osb_r.next()
                    prev = state['prev'] if j > 0 else None
                    state['prev'] = (kT, vaug)
                    rs_ = slice(j * 128, (j + 1) * 128)
                    P.dma("sp", qrow.t[:], rows[r, rs_, qc0:qc0 + 512], writes=[qrow])
                    P.dma("sp", krow.t[:, 0:kw], rows[r, rs_, kc0:kc0 + kw], writes=[krow])
                    P.dma("act" if DBG['act_dma'] else "sp", vaug.t[:, 0:nvh, 0:HD],
                          rows[r, rs_, vc0:vc0 + nvh * HD].rearrange("p (h d) -> p h d", d=HD), writes=[vaug])

                    def trq(e):
                        for c in range(4):
                            ins = e.transpose(pTq.t[:, c, :], qrow.t[:, c * 128:(c + 1) * 128], ident_b.t[:])
                        return ins
                    P.op("pe", trq, [qrow, ident_b], [pTq])
                    P.op("dve", lambda e: e.tensor_copy(out=qT.t[0:64, :, 0, :], in_=pTq.t[0:64, :, :]), [pTq], [qT])
                    P.op("dve", lambda e: e.tensor_copy(out=qT.t[64:128, :, 1, :], in_=pTq.t[64:128, :, :]), [pTq], [qT])
                    if isB:
                        kdup = kdup_r.next()
                        kv = krow.t[:, 0:128].rearrange("p (g d) -> p g d", d=HD)
                        P.op("pool", lambda e: e.tensor_copy(
                            out=kdup.t[:], in_=kv.unsqueeze(2).to_broadcast([128, 2, 2, HD])), [krow], [kdup])

                        def trk(e):
                            for c in range(2):
                                ins = e.transpose(pTk.t[:, c, :], kdup.t[:, c, :, :].rearrange("p a d -> p (a d)"),
                                                  ident_b.t[:])
                            return ins
                        P.op("pe", trk, [kdup, ident_b], [pTk])
                        P.op("act", lambda e: e.copy(out=kT.t[:, 0:2, :], in_=pTk.t[:, 0:2, :]), [pTk], [kT])
                    else:
                        def trk(e):
                            for c in range(4):
                                ins = e.transpose(pTk.t[:, c, :], krow.t[:, c * 128:(c + 1) * 128], ident_b.t[:])
                            return ins
                        P.op("pe", trk, [krow, ident_b], [pTk])
                        P.op("act", lambda e: e.copy(out=kT.t[:], in_=pTk.t[:]), [pTk], [kT])
                    for hg in range(2):
                        yield
                        pS = pS_r.next(); pt = pt_r.next()
                        s0 = 0 if prev is not None else 1

                        def kslice(kt, h):
                            if isB:
                                return kt.t[:, h // 4, :]
                            return kt.t[:, h // 2, :]

                        def sc(e):
                            for hh in range(4):
                                h = hg * 4 + hh
                                qs = qT.t[:, h // 2, h % 2, :]
                                pSh = pS[hh // 2]
                                if prev is not None:
                                    e.matmul(pSh.t[:, hh % 2, 0, :], lhsT=kslice(prev[0], h), rhs=qs, start=True, stop=True)
                                ins = e.matmul(pSh.t[:, hh % 2, 1, :], lhsT=kslice(kT, h), rhs=qs, start=True, stop=True)
                            return ins
                        rd = [qT, kT] + ([prev[0]] if prev is not None else [])
                        P.op("pe", sc, rd, [pS[0], pS[1]])
                        for hh2 in range(2):
                            P.op("act", lambda e: e.activation(
                                out=pt.t[:, 2 * hh2:2 * hh2 + 2, s0:2, :], in_=pS[hh2].t[:, :, s0:2, :],
                                func=AF.Exp, scale=0.125), [pS[hh2]], [pt])
                        mk = mask_b.t[:, 2 * (1 if isB else 0) + s0:2 * (1 if isB else 0) + 2, :]
                        meng = "dve" if hg == 0 else "pool"
                        P.op(meng, lambda e: e.tensor_tensor(
                            out=pt.t[:, :, s0:2, :], in0=pt.t[:, :, s0:2, :],
                            in1=mk.unsqueeze(1).to_broadcast([128, 4, 2 - s0, 128]), op=ALU.mult), [pt, mask_b], [pt])

                        yield
                        pO = pO_r.next()

                        def pv(e):
                            for hh in range(4):
                                h = hg * 4 + hh
                                vh = (h // 4) if isB else h
                                if prev is not None:
                                    e.matmul(pO.t[:, hh, :], lhsT=pt.t[:, hh, 0, :], rhs=prev[1].t[:, vh, :],
                                             start=True, stop=False)
                                ins = e.matmul(pO.t[:, hh, :], lhsT=pt.t[:, hh, 1, :], rhs=vaug.t[:, vh, :],
                                               start=(prev is None), stop=True)
                            return ins
                        rd = [pt, vaug] + ([prev[1]] if prev is not None else [])
                        P.op("pe", pv, rd, [pO])
                        if hg == 0:
                            P.op("act", lambda e: e.copy(out=osb.t[:, 0:4, :], in_=pO.t[:, :, 0:65]), [pO], [osb])
                        else:
                            P.op("dve", lambda e: e.tensor_copy(out=osb.t[:, 4:8, :], in_=pO.t[:, :, 0:65]), [pO], [osb])
                    P.dma("pool", accv[pi, r, rs_, :], osb.t[:].rearrange("p h d -> p (h d)"), reads=[osb])
                run_pipeline(items, bodyB)
                P.barrier()

            if "C" in phases:
              with ExitStack() as sc_:
                wout = sb(sc_, "wout", [128, 8, D], BF16)
                wr = sb(sc_, "wr", [128, 8, 36], F32)
                g_out = sb(sc_, "g_out", [128, D], F32)
                g_ffn = sb(sc_, "g_ffn", [128, D], F32)
                esink = sb(sc_, "esink", [128, NH], F32)
                for c in range(8):
                    P.dma("pool", wout.t[:, c, :], w_out[l, c * 128:(c + 1) * 128, :], writes=[wout])
                    P.dma("sp", wr.t[:, c, :], w_r[l, c * 128:(c + 1) * 128, :], writes=[wr])
                P.dma("sp", g_out.t[:], gvec[l, :, 1, :], writes=[g_out])
                P.dma("sp", g_ffn.t[:], gvec[l, :, 2, :], writes=[g_ffn])
                P.dma("sp", esink.t[:], sinks[l, :, :], writes=[esink])
                P.op("act", lambda e: e.activation(out=esink.t[:], in_=esink.t[:], func=AF.Exp), [esink], [esink])
                acc_rC = Ring([sb(sc_, "accC%d" % i, [128, 4, NH, 65], F32) for i in range(2)])
                xt_rC = Ring([sb(sc_, "xtC%d" % i, [128, D], F32) for i in range(3)])
                den_rC = Ring([sb(sc_, "denC%d" % i, [128, 2, NH], F32) for i in range(2)])
                o_t_rC = Ring([sb(sc_, "o_tC%d" % i, [128, 2, NH, HD], F32) for i in range(2)])
                ss2_rC = Ring([sb(sc_, "ss2C%d" % i, [128, 2], F32) for i in range(2)])
                rs2_rC = Ring([sb(sc_, "rs2C%d" % i, [128, 2], F32) for i in range(2)])
                tm2_rC = Ring([sb(sc_, "tm2C%d" % i, [128, 2], F32) for i in range(2)])
                mix_rC = Ring([sb(sc_, "mixC%d" % i, [128, D], BF16) for i in range(3)])
                mT_rC = Ring([sb(sc_, "mTC%d" % i, [128, 8, 128], BF16) for i in range(2)])
                xm_rC = Ring([sb(sc_, "xmC%d" % i, [128, D], F32) for i in range(2)])
                ss3_rC = Ring([sb(sc_, "ss3C%d" % i, [128, 1], F32) for i in range(2)])
                rs3_rC = Ring([sb(sc_, "rs3C%d" % i, [128, 1], F32) for i in range(2)])
                tm3_rC = Ring([sb(sc_, "tm3C%d" % i, [128, 1], F32) for i in range(2)])
                h2_rC = Ring([sb(sc_, "h2C%d" % i, [128, D], F32) for i in range(3)])
                h2Tf_rC = Ring([sb(sc_, "h2TfC%d" % i, [128, 8, 128], F32) for i in range(2)])
                h2Tb_rC = Ring([sb(sc_, "h2TbC%d" % i, [128, 8, 128], BF16) for i in range(2)])
                junk = sb(sc_, "junkC", [128, D], BF16)
                LG = sb(sc_, "LG", [128, NT, 36], F32)
                gmax = sb(sc_, "gmax", [128, NT], F32)
                gtop = sb(sc_, "gtop", [128, NT], F32)
                m1 = sb(sc_, "m1", [128, NT], F32)
                m2 = sb(sc_, "m2", [128, NT], F32)
                ex4 = sb(sc_, "ex4", [128, NT, 4], F32)
                gm = sb(sc_, "gm", [128, NT, 4], F32)
                lem = sb(sc_, "lem", [128, NT, 32], F32)
                m1k = sb(sc_, "m1k", [128, NT, 32], F32)
                m2k = sb(sc_, "m2k", [128, NT, 32], F32)
                pTm = ps(sc_, "pTm", [128, 8, 128], BF16)
                pOa = ps(sc_, "pOa", [128, 512], F32)
                pOb = ps(sc_, "pOb", [128, 512], F32)
                pTh = [ps(sc_, "pTh%d" % i, [128, 4, 128], F32) for i in range(2)]
                pR = ps(sc_, "pR", [128, 36], F32)
                def bodyC(i):
                    acc = acc_rC.next()
                    xt = xt_rC.next()
                    den = den_rC.next()
                    o_t = o_t_rC.next()
                    ss2 = ss2_rC.next()
                    rs2 = rs2_rC.next()
                    tm2 = tm2_rC.next()
                    mix = mix_rC.next()
                    mT = mT_rC.next()
                    xm = xm_rC.next()
                    ss3 = ss3_rC.next()
                    rs3 = rs3_rC.next()
                    tm3 = tm3_rC.next()
                    h2 = h2_rC.next()
                    h2Tf = h2Tf_rC.next()
                    h2Tb = h2Tb_rC.next()
                    rs_ = slice(i * 128, (i + 1) * 128)
                    for a in range(4):
                        P.dma("sp", acc.t[:, a, :, :].rearrange("p h d -> p (h d)"),
                              ACC[a, rs_, :], writes=[acc])
                    P.dma("sp", xt.t[:], xsrc[rs_, :], writes=[xt])
                    P.op("dve", lambda e: e.tensor_tensor(out=acc.t[:, 0], in0=acc.t[:, 0], in1=acc.t[:, 1], op=ALU.add),
                         [acc], [acc])
                    P.op("dve", lambda e: e.tensor_tensor(out=acc.t[:, 0], in0=acc.t[:, 0], in1=acc.t[:, 2], op=ALU.add),
                         [acc], [acc])
                    P.op("dve", lambda e: e.tensor_copy(out=den.t[:, 0, :], in_=acc.t[:, 0, :, 64]), [acc], [den])
                    P.op("dve", lambda e: e.tensor_tensor(out=den.t[:, 1, :], in0=acc.t[:, 3, :, 64], in1=esink.t[:],
                                                          op=ALU.add), [acc, esink], [den])
                    P.op("dve", lambda e: e.reciprocal(out=den.t[:], in_=den.t[:]), [den], [den])
                    P.op("dve", lambda e: e.tensor_tensor(
                        out=o_t.t[:, 0], in0=acc.t[:, 0, :, 0:HD],
                        in1=den.t[:, 0, :].unsqueeze(2).to_broadcast([128, NH, HD]), op=ALU.mult), [acc, den], [o_t])
                    P.op("pool", lambda e: e.tensor_tensor(
                        out=o_t.t[:, 1], in0=acc.t[:, 3, :, 0:HD],
                        in1=den.t[:, 1, :].unsqueeze(2).to_broadcast([128, NH, HD]), op=ALU.mult), [acc, den], [o_t])
                    of = o_t.t[:].rearrange("p a h d -> p (a h d)")
                    for a in range(2):
                        P.op("act", lambda e: e.activation(out=junk.t[:, a * 512:(a + 1) * 512],
                                                           in_=of[:, a * 512:(a + 1) * 512], func=AF.Square,
                                                           accum_out=ss2.t[:, a:a + 1]), [o_t], [junk, ss2])
                    rstd_from_ss(ss2.t[:], 512, rs2, tm2, [ss2])
                    for a in range(2):
                        P.op("dve", lambda e: e.scalar_tensor_tensor(
                            out=mix.t[:, a * 512:(a + 1) * 512], in0=of[:, a * 512:(a + 1) * 512],
                            scalar=rs2.t[:, a:a + 1], in1=g_out.t[:, a * 512:(a + 1) * 512],
                            op0=ALU.mult, op1=ALU.mult), [o_t, rs2, g_out], [mix])

                    yield

                    def trm(e):
                        for c in range(8):
                            ins = e.transpose(pTm.t[:, c, :], mix.t[:, c * 128:(c + 1) * 128], ident_b.t[:])
                        return ins
                    P.op("pe", trm, [mix, ident_b], [pTm])
                    P.op("act", lambda e: e.copy(out=mT.t[:], in_=pTm.t[:]), [pTm], [mT])
                    for hf, pO in ((0, pOa), (1, pOb)):
                        def mmo(e):
                            for c in range(8):
                                ins = e.matmul(pO.t[:], lhsT=mT.t[:, c, :], rhs=wout.t[:, c, hf * 512:(hf + 1) * 512],
                                               start=(c == 0), stop=(c == 7))
                            return ins
                        P.op("pe", mmo, [mT, wout], [pO])
                        P.op("dve", lambda e: e.tensor_tensor(out=xm.t[:, hf * 512:(hf + 1) * 512],
                                                              in0=xt.t[:, hf * 512:(hf + 1) * 512], in1=pO.t[:],
                                                              op=ALU.add), [xt, pO], [xm])
                    P.dma("pool", XM[rs_, :], xm.t[:], reads=[xm])
                    P.op("act", lambda e: e.activation(out=junk.t[:], in_=xm.t[:], func=AF.Square, accum_out=ss3.t[:]),
                         [xm], [junk, ss3])
                    rstd_from_ss(ss3.t[:], D, rs3, tm3, [ss3])
                    P.op("dve", lambda e: e.scalar_tensor_tensor(out=h2.t[:], in0=xm.t[:], scalar=rs3.t[:, 0:1],
                                                                 in1=g_ffn.t[:], op0=ALU.mult, op1=ALU.mult),
                         [xm, rs3, g_ffn], [h2])
                    yield
                    for hf in range(2):
                        def trh(e):
                            for c in range(4):
                                cc = hf * 4 + c
                                ins = e.transpose(pTh[hf].t[:, c, :], h2.t[:, cc * 128:(cc + 1) * 128], ident_f.t[:])
                            return ins
                        P.op("pe", trh, [h2, ident_f], [pTh[hf]])
                        P.op("act", lambda e: e.copy(out=h2Tf.t[:, hf * 4:(hf + 1) * 4, :], in_=pTh[hf].t[:]),
                             [pTh[hf]], [h2Tf])
                        P.op("dve", lambda e: e.tensor_copy(out=h2Tb.t[:, hf * 4:(hf + 1) * 4, :], in_=pTh[hf].t[:]),
                             [pTh[hf]], [h2Tb])
                    P.dma("pool", H2T.rearrange("(c p) t -> p c t", p=128)[:, :, rs_], h2Tb.t[:], reads=[h2Tb])


                    def mmr(e):
                        for c in range(8):
                            ins = e.matmul(pR.t[:], lhsT=h2Tf.t[:, c, :], rhs=wr.t[:, c, :], start=(c == 0), stop=(c == 7))
                        return ins
                    P.op("pe", mmr, [h2Tf, wr], [pR])
                    P.op("act", lambda e: e.copy(out=LG.t[:, i, :], in_=pR.t[:]), [pR], [LG])
                run_pipeline(range(NTL), bodyC)
                BIG = 1e30
                NTc = NTL
                lgG = LG.t[:, 0:NTc, 0:4]
                bc4 = lambda t_: t_.t[:, 0:NTc].unsqueeze(2).to_broadcast([128, NTc, 4])
                bc32 = lambda t_: t_.t[:, 0:NTc].unsqueeze(2).to_broadcast([128, NTc, 32])
                P.op("dve", lambda e: e.tensor_reduce(out=gmax.t[:, 0:NTc], in_=lgG, axis=AX.X, op=ALU.max), [LG], [gmax])
                P.op("dve", lambda e: e.tensor_tensor(out=ex4.t[:, 0:NTc, :], in0=lgG, in1=bc4(gmax), op=ALU.subtract),
                     [LG, gmax], [ex4])
                P.op("act", lambda e: e.activation(out=ex4.t[:, 0:NTc, :], in_=ex4.t[:, 0:NTc, :], func=AF.Exp), [ex4], [ex4])
                P.op("dve", lambda e: e.tensor_reduce(out=gtop.t[:, 0:NTc], in_=ex4.t[:, 0:NTc, :], axis=AX.X, op=ALU.add),
                     [ex4], [gtop])
                P.op("dve", lambda e: e.reciprocal(out=gtop.t[:, 0:NTc], in_=gtop.t[:, 0:NTc]), [gtop], [gtop])
                P.op("dve", lambda e: e.tensor_tensor(out=gm.t[:, 0:NTc, :], in0=lgG, in1=bc4(gmax), op=ALU.is_lt),
                     [LG, gmax], [gm])
                P.op("dve", lambda e: e.tensor_scalar(out=gm.t[:, 0:NTc, :], in0=gm.t[:, 0:NTc, :], scalar1=-BIG, scalar2=None,
                                                      op0=ALU.mult), [gm], [gm])
                P.op("dve", lambda e: e.tensor_tensor(
                    out=lem.t[:, 0:NTc, :].rearrange("p t (g e) -> p t g e", e=8),
                    in0=LG.t[:, 0:NTc, 4:36].rearrange("p t (g e) -> p t g e", e=8),
                    in1=gm.t[:, 0:NTc, :].unsqueeze(3).to_broadcast([128, NTc, 4, 8]), op=ALU.add), [LG, gm], [lem])
                lv = lem.t[:, 0:NTc, :]
                P.op("dve", lambda e: e.tensor_reduce(out=m1.t[:, 0:NTc], in_=lv, axis=AX.X, op=ALU.max), [lem], [m1])
                P.op("dve", lambda e: e.tensor_tensor(out=m1k.t[:, 0:NTc, :], in0=lv, in1=bc32(m1), op=ALU.is_ge),
                     [lem, m1], [m1k])
                P.op("dve", lambda e: e.tensor_scalar(out=m2k.t[:, 0:NTc, :], in0=m1k.t[:, 0:NTc, :], scalar1=-BIG, scalar2=None,
                                                      op0=ALU.mult), [m1k], [m2k])
                P.op("dve", lambda e: e.tensor_tensor(out=lv, in0=lv, in1=m2k.t[:, 0:NTc, :], op=ALU.add), [lem, m2k], [lem])
                P.op("dve", lambda e: e.tensor_reduce(out=m2.t[:, 0:NTc], in_=lv, axis=AX.X, op=ALU.max), [lem], [m2])
                P.op("dve", lambda e: e.tensor_tensor(out=m2k.t[:, 0:NTc, :], in0=lv, in1=bc32(m2), op=ALU.is_ge),
                     [lem, m2], [m2k])
                P.op("dve", lambda e: e.tensor_tensor(out=m1.t[:, 0:NTc], in0=m1.t[:, 0:NTc], in1=m2.t[:, 0:NTc],
                                                      op=ALU.subtract), [m1, m2], [m1])
                P.op("act", lambda e: e.activation(out=m1.t[:, 0:NTc], in_=m1.t[:, 0:NTc], func=AF.Exp, scale=-1.0), [m1], [m1])
                P.op("dve", lambda e: e.tensor_scalar(out=m1.t[:, 0:NTc], in0=m1.t[:, 0:NTc], scalar1=1.0, scalar2=None,
                                                      op0=ALU.add), [m1], [m1])
                P.op("dve", lambda e: e.reciprocal(out=m1.t[:, 0:NTc], in_=m1.t[:, 0:NTc]), [m1], [m1])
                P.op("dve", lambda e: e.tensor_tensor(out=m1.t[:, 0:NTc], in0=m1.t[:, 0:NTc], in1=gtop.t[:, 0:NTc],
                                                      op=ALU.mult), [m1, gtop], [m1])
                P.op("dve", lambda e: e.tensor_tensor(out=m2.t[:, 0:NTc], in0=gtop.t[:, 0:NTc], in1=m1.t[:, 0:NTc],
                                                      op=ALU.subtract), [gtop, m1], [m2])
                P.op("dve", lambda e: e.tensor_tensor(out=m1k.t[:, 0:NTc, :], in0=m1k.t[:, 0:NTc, :], in1=bc32(m1),
                                                      op=ALU.mult), [m1k, m1], [m1k])
                P.op("dve", lambda e: e.tensor_tensor(out=m2k.t[:, 0:NTc, :], in0=m2k.t[:, 0:NTc, :], in1=bc32(m2),
                                                      op=ALU.mult), [m2k, m2], [m2k])
                P.op("dve", lambda e: e.tensor_tensor(out=m1k.t[:, 0:NTc, :], in0=m1k.t[:, 0:NTc, :], in1=m2k.t[:, 0:NTc, :],
                                                      op=ALU.add), [m1k, m2k], [m1k])
                P.dma("sp", GS[0:NTc * 128, :].rearrange("(i p) e -> p i e", p=128), m1k.t[:, 0:NTc, :], reads=[m1k])
                P.barrier()

            if "D" in phases:
              with ExitStack() as sd:
                NTS = TS // 128
                hT = sb(sd, "hT", [128, 8, TS], BF16)
                Gt = sb(sd, "Gt", [128, NTS, NE], F32)
                yacc = sb(sd, "yacc", [128, NTS, D], F32)
                wgu_r = Ring([sb(sd, "wgu%d" % i, [128, 8, 2 * FE], BF16) for i in range(2)])
                wdn_r = Ring([sb(sd, "wdn%d" % i, [128, 4, D], BF16) for i in range(2)])
                sg_r = Ring([sb(sd, "sg%d" % i, [128, 512], F32) for i in range(2)])
                aT_r = Ring([sb(sd, "aT%d" % i, [128, 4, 512], BF16) for i in range(2)])
                xm_r = Ring([sb(sd, "xmD%d" % i, [128, D], F32) for i in range(2)])
                pg_r = Ring([ps(sd, "pg%d" % i, [128, 512], F32) for i in range(2)])
                pu_r = Ring([ps(sd, "pu%d" % i, [128, 512], F32) for i in range(2)])
                py_r = Ring([(ps(sd, "pya%d" % i, [128, 512], F32), ps(sd, "pyb%d" % i, [128, 512], F32))
                             for i in range(2)])
                for s_ in range(S // TS):
                    t0 = s_ * TS
                    P.dma("sp", hT.t[:], H2T.rearrange("(c p) t -> p c t", p=128)[:, :, t0:t0 + TS], writes=[hT])
                    P.dma("sp", Gt.t[:], GS[t0:t0 + TS, :].rearrange("(i p) e -> p i e", p=128), writes=[Gt])
                    P.op("dve", lambda e: e.memset(yacc.t[:], 0.0), [], [yacc])
                    for ex in range(n_exp):
                        wgu = wgu_r.next(); wdn = wdn_r.next()
                        for hf in range(2):
                            P.dma("pool", wgu.t[:, hf * 4:(hf + 1) * 4, :],
                                  w_gu[l, ex, hf * 512:(hf + 1) * 512, :].rearrange("(c p) n -> p c n", p=128),
                                  writes=[wgu])
                        P.dma("pool", wdn.t[:], w_dn[l, ex, :, :].rearrange("(c p) n -> p c n", p=128), writes=[wdn])
                        for g in range(TS // 512):
                            aT = aT_r.next()
                            tk = slice(g * 512, (g + 1) * 512)
                            for c in range(4):
                                pg = pg_r.next(); pu = pu_r.next(); sg = sg_r.next()

                                def mmg(e):
                                    for k in range(8):
                                        ins = e.matmul(pg.t[:], lhsT=wgu.t[:, k, c * 128:(c + 1) * 128], rhs=hT.t[:, k, tk],
                                                       start=(k == 0), stop=(k == 7))
                                    return ins

                                def mmu(e):
                                    for k in range(8):
                                        ins = e.matmul(pu.t[:], lhsT=wgu.t[:, k, FE + c * 128:FE + (c + 1) * 128],
                                                       rhs=hT.t[:, k, tk], start=(k == 0), stop=(k == 7))
                                    return ins
                                P.op("pe", mmg, [wgu, hT], [pg])
                                P.op("pe", mmu, [wgu, hT], [pu])
                                P.op("act", lambda e: e.activation(out=sg.t[:], in_=pg.t[:], func=AF.Silu), [pg], [sg])
                                P.op("dve", lambda e: e.tensor_tensor(out=aT.t[:, c, :], in0=sg.t[:], in1=pu.t[:],
                                                                      op=ALU.mult), [sg, pu], [aT])
                            for t in range(4):
                                ti = g * 4 + t
                                pya, pyb = py_r.next()
                                for hf, py in ((0, pya), (1, pyb)):
                                    def mmd(e):
                                        for c in range(4):
                                            ins = e.matmul(py.t[:], lhsT=aT.t[:, c, t * 128:(t + 1) * 128],
                                                           rhs=wdn.t[:, c, hf * 512:(hf + 1) * 512],
                                                           start=(c == 0), stop=(c == 3))
                                        return ins
                                    P.op("pe", mmd, [aT, wdn], [py])
                                    P.op("dve", lambda e: e.scalar_tensor_tensor(
                                        out=yacc.t[:, ti, hf * 512:(hf + 1) * 512], in0=py.t[:],
                                        scalar=Gt.t[:, ti, ex:ex + 1], in1=yacc.t[:, ti, hf * 512:(hf + 1) * 512],
                                        op0=ALU.mult, op1=ALU.add), [py, Gt, yacc], [yacc])
                    for ti in range(NTS):
                        xm = xm_r.next()
                        rs_ = slice(t0 + ti * 128, t0 + (ti + 1) * 128)
                        P.dma("sp", xm.t[:], XM[rs_, :], writes=[xm])
                        P.op("dve", lambda e: e.tensor_tensor(out=xm.t[:], in0=xm.t[:], in1=yacc.t[:, ti, :], op=ALU.add),
                             [xm, yacc], [xm])
                        P.dma("sp", xdst[rs_, :], xm.t[:], reads=[xm])
                P.barrier()
        P.barrier()
    return nc


def _consts():
    k = np.arange(128)[:, None]
    q = np.arange(128)[None, :]
    mask = np.stack([(k >= q), (k <= q), (k > q), (k <= q)], axis=1).astype(np.float32)
    ident = np.eye(128, dtype=np.float32)
    invf = (500000.0 ** (-np.arange(0, 16, 2, dtype=np.float32) / np.float32(16))).astype(np.float32)
    invf = np.ascontiguousarray(np.broadcast_to(invf[None, :], (128, 8)))
    return ident, np.ascontiguousarray(mask), invf


def prep_shared(inputs, L):
    f = lambda k: np.asarray(inputs[k], dtype=np.float32)
    rep = lambda v: np.broadcast_to(v[:, None, :], (L, 128, v.shape[-1]))
    gvec = np.stack([rep(f("attn_norm")), rep(np.concatenate([f("out_norm_a"), f("out_norm_b")], axis=-1)),
                     rep(f("ffn_norm"))], axis=2)
    qa, ka, qb, kb = f("q_norm_a"), f("k_norm_a"), f("q_norm_b"), f("k_norm_b")
    gq = np.concatenate([np.tile(qa, (1, 8)), np.tile(qb, (1, 8)), np.tile(ka, (1, 8)), np.tile(kb, (1, 2))], axis=-1)
    w = f("w_in")
    w_in_p = np.concatenate([w[:, :, 0:512], w[:, :, 1536:2048], w[:, :, 512:1024], w[:, :, 2048:2176],
                             w[:, :, 1024:1536], w[:, :, 2176:2304]], axis=-1)
    ident, mask, invf = _consts()
    return dict(
        c_ident=ident, c_mask=mask, c_invf=invf,
        gvec=np.ascontiguousarray(gvec), gqk=np.ascontiguousarray(rep(gq)),
        sinks=np.ascontiguousarray(rep(f("sinks_b"))),
        w_in=np.ascontiguousarray(w_in_p), w_out=f("w_out"),
        w_r=np.ascontiguousarray(np.concatenate([f("w_router_group"), f("w_router_expert")], axis=-1)),
        w_gu=f("w_gate_up"), w_dn=f("w_down"))


def core_map(shared, x_b, pos_b):
    S = x_b.shape[0]
    m = dict(shared)
    m["x"] = np.ascontiguousarray(x_b, dtype=np.float32)
    m["pos_t"] = np.ascontiguousarray(np.asarray(pos_b, dtype=np.int32).reshape(S // 128, 128).T)
    return m


def kernel(**inputs):
    x = np.asarray(inputs["x"])
    pos = np.asarray(inputs["positions"])
    B, S, _ = x.shape
    L = np.asarray(inputs["attn_norm"]).shape[0]
    shared = prep_shared(inputs, L)
    nc = build(S, L)
    in_maps = [core_map(shared, x[b], pos[b]) for b in range(B)]
    res = run_bass_kernel_spmd(nc, in_maps, core_ids=list(range(B)))
    return np.stack([np.asarray(r["out"]).reshape(S, D) for r in res.results], axis=0).astype(np.float32)
```

```python
import numpy as np
from contextlib import ExitStack
import concourse.bass as bass
import concourse.mybir as mybir
from concourse.bass_utils import run_bass_kernel_spmd

F32 = mybir.dt.float32
BF16 = mybir.dt.bfloat16
I32 = mybir.dt.int32
ALU = mybir.AluOpType
AF = mybir.ActivationFunctionType
AX = mybir.AxisListType

D = 1024
PW = 2304
NH = 8
HD = 64
NE = 32
FE = 512
VW = 80
EPS = 1e-6
TWO_PI_HI = 6.28125
TWO_PI_LO = 2.0 * np.pi - 6.28125


class Buf:
    __slots__ = ("name", "w", "r", "excl")

    def __init__(self, name):
        self.name = name
        self.w = None
        self.r = {}
        self.excl = False


class T:
    def __init__(self, t, name):
        self.t = t
        self.b = Buf(name)


class Prog:
    def __init__(self, nc, es, n_dsem=22):
        self.nc = nc
        self.es = es
        self.eng = dict(pe=nc.tensor, act=nc.scalar, dve=nc.vector, pool=nc.gpsimd, sp=nc.sync)
        self.sem = {e: es.enter_context(nc.semaphore("s_" + e)) for e in ("pe", "act", "dve", "pool")}
        self.cnt = {e: 0 for e in self.sem}
        self.dsem = [es.enter_context(nc.semaphore("d%d" % i)) for i in range(n_dsem)]
        self.dcnt = [0] * n_dsem
        self.dnext = {"hw": 0, "sw": 0}
        self.n_sw = 6
        self.seen = {e: {} for e in self.eng}

    def _semof(self, key):
        return self.sem[key] if isinstance(key, str) else self.dsem[key[1]]

    def _wait(self, e, tok):
        if tok is None:
            return
        key, val = tok
        if key == e and e == "pe":
            return
        if self.seen[e].get(key, 0) >= val:
            return
        self.eng[e].wait_ge(self._semof(key), val)
        self.seen[e][key] = val

    def _deps(self, e, reads, writes):
        for b in reads:
            self._wait(e, b.w)
        for b in writes:
            self._wait(e, b.w)
            for k, v in b.r.items():
                self._wait(e, (k, v))

    def _record(self, tok, reads, writes):
        key, val = tok
        for b in reads:
            if b.r.get(key, 0) < val:
                b.r[key] = val
        for b in writes:
            b.w = tok
            b.r = {}

    def op(self, e, fn, reads=(), writes=()):
        reads = [x.b if isinstance(x, T) else x for x in reads]
        writes = [x.b if isinstance(x, T) else x for x in writes]
        writes = writes + [b for b in reads if b.excl]
        reads = [b for b in reads if not b.excl]
        self._deps(e, reads, writes)
        ins = fn(self.eng[e])
        self.cnt[e] += 1
        ins.then_inc(self.sem[e], 1)
        self._record((e, self.cnt[e]), reads, writes)

    def dma(self, q, out, in_, reads=(), writes=()):
        reads = [x.b if isinstance(x, T) else x for x in reads]
        writes = [x.b if isinstance(x, T) else x for x in writes]
        if q == "pool":
            i = self.dnext["sw"]
            self.dnext["sw"] = (i + 1) % self.n_sw
        else:
            i = self.n_sw + self.dnext["hw"]
            self.dnext["hw"] = (self.dnext["hw"] + 1) % (len(self.dsem) - self.n_sw)
        if self.dcnt[i]:
            self._wait(q, (("d", i), self.dcnt[i]))
        self._deps(q, reads, writes)
        ins = self.eng[q].dma_start(out=out, in_=in_)
        self.dcnt[i] += 16
        ins.then_inc(self.dsem[i], 16)
        self._record((("d", i), self.dcnt[i]), reads, writes)

    def barrier(self):
        for e in self.eng:
            for o in self.sem:
                if o != e and self.cnt[o]:
                    self._wait(e, (o, self.cnt[o]))
            for i, c in enumerate(self.dcnt):
                if c:
                    self._wait(e, (("d", i), c))

    def final_wait(self):
        for i, c in enumerate(self.dcnt):
            if c:
                self._wait("sp", (("d", i), c))


def run_pipeline(items, body):
    gens = []
    it = iter(items)
    pending = True
    while pending or gens:
        nxt = next(it, None) if pending else None
        if nxt is None:
            pending = False
        else:
            gens.append(body(*nxt) if isinstance(nxt, tuple) else body(nxt))
        alive = []
        for g in gens:
            try:
                next(g)
                alive.append(g)
            except StopIteration:
                pass
        gens = alive


class Ring:
    def __init__(self, tiles):
        self.tiles = tiles
        self.i = 0

    def next(self):
        t = self.tiles[self.i % len(self.tiles)]
        self.i += 1
        return t


LIMIT_NT = None
DBG = dict(c_stop=9, b_passes=None, b_blocks=None, act_dma=False, split_exp=True)


def build(S, L, n_exp=NE, phases="ABCD", dbg=False):
    NT = S // 128
    NTL = NT if LIMIT_NT is None else min(NT, LIMIT_NT)
    TS = min(2048, S)
    nc = bass.Bass("TRN2", target_bir_lowering=False)
    dram = lambda name, shape, dt, kind="ExternalInput": nc.dram_tensor(name, shape, dt, kind=kind).ap()
    x_in = dram("x", [S, D], F32)
    pos_t = dram("pos_t", [128, NT], I32)
    c_ident = dram("c_ident", [128, 128], F32)
    c_mask = dram("c_mask", [128, 4, 128], F32)
    c_invf = dram("c_invf", [128, 8], F32)
    gvec = dram("gvec", [L, 128, 3, D], F32)
    gqk = dram("gqk", [L, 128, 26 * HD], F32)
    sinks = dram("sinks", [L, 128, NH], F32)
    w_in = dram("w_in", [L, D, PW], F32)
    w_out = dram("w_out", [L, D, D], F32)
    w_r = dram("w_r", [L, D, 36], F32)
    w_gu = dram("w_gu", [L, NE, D, 2 * FE], F32)
    w_dn = dram("w_dn", [L, NE, FE, D], F32)
    out = dram("out", [S, D], F32, kind="ExternalOutput")
    okind = "ExternalOutput" if dbg else "Internal"
    QKV = dram("QKV", [S, PW], BF16, kind=okind)
    ACC = dram("ACC", [4, S, NH * 65], F32, kind=okind)
    XM = dram("XM", [S, D], F32, kind=okind)
    H2T = dram("H2T", [D, S], BF16, kind=okind)
    GS = dram("GS", [S, NE], F32, kind=okind)
    X1 = dram("X1", [S, D], F32, kind=okind)

    with ExitStack() as es:
        P = Prog(nc, es)

        uid = [0]

        def sb(stack, name, shape, dt):
            uid[0] += 1
            name = "%s_%d" % (name, uid[0])
            return T(stack.enter_context(nc.sbuf_tensor(name, shape, dt)), name)

        def ps(stack, name, shape, dt):
            uid[0] += 1
            name = "%s_%d" % (name, uid[0])
            nfull = 512 if dt == F32 else 1024
            full = stack.enter_context(nc.psum_tensor(name, [128, nfull], dt))
            n = int(np.prod(shape[1:]))
            assert n <= nfull, (name, shape)
            v = full[:, 0:n]
            if len(shape) == 3:
                v = v.rearrange("p (a b) -> p a b", a=shape[1], b=shape[2])
            elif len(shape) == 4:
                v = v.rearrange("p (a b c) -> p a b c", a=shape[1], b=shape[2], c=shape[3])
            t = T(v, name)
            t.b.excl = True
            return t

        ident_f = sb(es, "ident_f", [128, 128], F32)
        ident_b = sb(es, "ident_b", [128, 128], BF16)
        mask_f = sb(es, "mask_f", [128, 4, 128], F32)
        mask_b = sb(es, "mask_b", [128, 4, 128], BF16)
        invf = sb(es, "invf", [128, 8], F32)
        cos_t = sb(es, "cos_t", [128, NT, 8], F32)
        sin_t = sb(es, "sin_t", [128, NT, 8], F32)
        P.dma("sp", ident_f.t[:], c_ident[:, :], writes=[ident_f])
        P.dma("sp", mask_f.t[:], c_mask[:, :, :], writes=[mask_f])
        P.dma("sp", invf.t[:], c_invf[:, :], writes=[invf])
        P.op("dve", lambda e: e.tensor_copy(out=ident_b.t[:], in_=ident_f.t[:]), [ident_f], [ident_b])
        P.op("dve", lambda e: e.tensor_copy(out=mask_b.t[:], in_=mask_f.t[:]), [mask_f], [mask_b])
        with ExitStack() as s0:
            pos_i = sb(s0, "pos_i", [128, NT], I32)
            pos_f = sb(s0, "pos_f", [128, NT], F32)
            ang = sb(s0, "ang", [128, NT, 8], F32)
            ang2 = sb(s0, "ang2", [128, NT, 8], F32)
            kf = sb(s0, "kf", [128, NT, 8], F32)
            ki = sb(s0, "ki", [128, NT, 8], I32)
            P.dma("sp", pos_i.t[:], pos_t[:, :], writes=[pos_i])
            P.op("dve", lambda e: e.tensor_copy(out=pos_f.t[:], in_=pos_i.t[:]), [pos_i], [pos_f])
            P.op("dve", lambda e: e.tensor_tensor(
                out=ang.t[:], in0=pos_f.t[:].unsqueeze(2).to_broadcast([128, NT, 8]),
                in1=invf.t[:].unsqueeze(1).to_broadcast([128, NT, 8]), op=ALU.mult), [pos_f, invf], [ang])
            for which, dst in ((0, sin_t), (1, cos_t)):
                src = ang
                if which == 1:
                    P.op("dve", lambda e: e.tensor_scalar(out=ang2.t[:], in0=ang.t[:], scalar1=float(np.pi / 2),
                                                          scalar2=None, op0=ALU.add), [ang], [ang2])
                    src = ang2
                P.op("dve", lambda e: e.tensor_scalar(out=kf.t[:], in0=src.t[:], scalar1=float(1.0 / (2 * np.pi)),
                                                      scalar2=None, op0=ALU.mult), [src], [kf])
                P.op("dve", lambda e: e.tensor_copy(out=ki.t[:], in_=kf.t[:]), [kf], [ki])
                P.op("dve", lambda e: e.tensor_copy(out=kf.t[:], in_=ki.t[:]), [ki], [kf])
                P.op("dve", lambda e: e.scalar_tensor_tensor(out=dst.t[:], in0=kf.t[:], scalar=-TWO_PI_HI, in1=src.t[:],
                                                             op0=ALU.mult, op1=ALU.add), [kf, src], [dst])
                P.op("dve", lambda e: e.scalar_tensor_tensor(out=dst.t[:], in0=kf.t[:], scalar=-TWO_PI_LO, in1=dst.t[:],
                                                             op0=ALU.mult, op1=ALU.add), [kf, dst], [dst])
                P.op("dve", lambda e: e.tensor_scalar(out=dst.t[:], in0=dst.t[:], scalar1=float(np.pi), scalar2=float(-np.pi),
                                                      op0=ALU.min, op1=ALU.max), [dst], [dst])
                P.op("act", lambda e: e.activation(out=dst.t[:], in_=dst.t[:], func=AF.Sin), [dst], [dst])
            P.barrier()

        def rstd_from_ss(ss_ap, n_feat, out_t, tmp_t, deps_r):
            P.op("dve", lambda e: e.tensor_scalar(out=tmp_t.t[:], in0=ss_ap, scalar1=1.0 / n_feat, scalar2=EPS,
                                                  op0=ALU.mult, op1=ALU.add), deps_r, [tmp_t])
            P.op("act", lambda e: e.activation(out=tmp_t.t[:], in_=tmp_t.t[:], func=AF.Sqrt), [tmp_t], [tmp_t])
            P.op("dve", lambda e: e.reciprocal(out=out_t.t[:], in_=tmp_t.t[:]), [tmp_t], [out_t])

        for l in range(L):
            xsrc = x_in if l == 0 else X1
            xdst = out if l == L - 1 else X1
            if "A" in phases:
              with ExitStack() as sa:
                win = sb(sa, "win", [128, 8, PW], BF16)
                g_attn = sb(sa, "g_attn", [128, D], F32)
                g_qk = sb(sa, "g_qk", [128, 26, HD], F32)
                for c in range(8):
                    P.dma("pool", win.t[:, c, :], w_in[l, c * 128:(c + 1) * 128, :], writes=[win])
                P.dma("sp", g_attn.t[:], gvec[l, :, 0, :], writes=[g_attn])
                P.dma("sp", g_qk.t[:].rearrange("p h d -> p (h d)"), gqk[l, :, :], writes=[g_qk])
                xt_r = Ring([sb(sa, "xt%d" % i, [128, D], F32) for i in range(3)])
                junk = sb(sa, "junkA", [128, D], BF16)
                xg_r = Ring([sb(sa, "xg%d" % i, [128, D], BF16) for i in range(3)])
                xT_r = Ring([sb(sa, "xT%d" % i, [128, 8, 128], BF16) for i in range(3)])
                ss_r = Ring([sb(sa, "ssA%d" % i, [128, 1], F32) for i in range(3)])
                rs_r = Ring([sb(sa, "rsA%d" % i, [128, 1], F32) for i in range(3)])
                tm_r = Ring([sb(sa, "tmA%d" % i, [128, 1], F32) for i in range(3)])
                proj_r = Ring([sb(sa, "proj%d" % i, [128, PW], F32) for i in range(5)])
                sq_r = Ring([sb(sa, "sqA%d" % i, [128, 26, HD], F32) for i in range(4)])
                ssh_r = Ring([sb(sa, "ssh%d" % i, [128, 26], F32) for i in range(3)])
                rsh_r = Ring([sb(sa, "rsh%d" % i, [128, 26], F32) for i in range(3)])
                tmh_r = Ring([sb(sa, "tmh%d" % i, [128, 26], F32) for i in range(3)])
                rr_r = Ring([[sb(sa, "r%d_%d" % (k, i), [128, 26, 8], F32) for k in range(4)] for i in range(3)])
                qkvb_r = Ring([sb(sa, "qkvb%d" % i, [128, PW], BF16) for i in range(3)])
                pT_r = Ring([ps(sa, "pTA%d" % i, [128, 8, 128], BF16) for i in range(2)])
                pP_r = [Ring([ps(sa, "pP%d_%d" % (g, i), [128, 512], F32) for i in range(1)]) for g in range(5)]

                def bodyA(i):
                    sq = sq_r.next(); ssh = ssh_r.next(); rsh = rsh_r.next(); tmh = tmh_r.next()
                    r1, r2, r3, r4 = rr_r.next()
                    xt = xt_r.next(); xg = xg_r.next(); xT = xT_r.next(); ss = ss_r.next(); rs = rs_r.next()
                    tm = tm_r.next(); proj = proj_r.next(); qkvb = qkvb_r.next(); pT = pT_r.next()
                    P.dma("sp", xt.t[:], xsrc[i * 128:(i + 1) * 128, :], writes=[xt])
                    yield
                    P.op("act", lambda e: e.activation(out=junk.t[:], in_=xt.t[:], func=AF.Square, accum_out=ss.t[:]),
                         [xt], [junk, ss])
                    P.op("dve", lambda e: e.tensor_tensor(out=xg.t[:], in0=xt.t[:], in1=g_attn.t[:], op=ALU.mult),
                         [xt, g_attn], [xg])

                    yield

                    def tr(e):
                        for c in range(8):
                            ins = e.transpose(pT.t[:, c, :], xg.t[:, c * 128:(c + 1) * 128], ident_b.t[:])
                        return ins
                    P.op("pe", tr, [xg, ident_b], [pT])
                    P.op("act", lambda e: e.copy(out=xT.t[:], in_=pT.t[:]), [pT], [xT])
                    rstd_from_ss(ss.t[:], D, rs, tm, [ss])
                    yield
                    for g in range(5):
                        w = 512 if g < 4 else 256
                        pP = pP_r[g].next()

                        def mm(e):
                            for c in range(8):
                                ins = e.matmul(pP.t[:, 0:w], lhsT=xT.t[:, c, :], rhs=win.t[:, c, g * 512:g * 512 + w],
                                               start=(c == 0), stop=(c == 7))
                            return ins
                        P.op("pe", mm, [xT, win], [pP])
                        eng = "act" if g % 2 == 0 else "dve"
                        if eng == "act":
                            P.op("act", lambda e: e.activation(out=proj.t[:, g * 512:g * 512 + w], in_=pP.t[:, 0:w],
                                                               func=AF.Copy, scale=rs.t[:, 0:1]), [pP, rs], [proj])
                        else:
                            P.op("dve", lambda e: e.tensor_scalar(out=proj.t[:, g * 512:g * 512 + w], in0=pP.t[:, 0:w],
                                                                  scalar1=rs.t[:, 0:1], scalar2=None, op0=ALU.mult),
                                 [pP, rs], [proj])
                    yield
                    qk = proj.t[:, 0:26 * HD].rearrange("p (h d) -> p h d", d=HD)
                    P.op("pool", lambda e: e.tensor_tensor(out=sq.t[:], in0=qk, in1=qk, op=ALU.mult), [proj], [sq])
                    P.op("dve", lambda e: e.tensor_reduce(out=ssh.t[:], in_=sq.t[:], axis=AX.X, op=ALU.add), [sq], [ssh])
                    rstd_from_ss(ssh.t[:], HD, rsh, tmh, [ssh])
                    yield
                    P.op("dve", lambda e: e.tensor_tensor(out=sq.t[:], in0=qk,
                                                          in1=rsh.t[:].unsqueeze(2).to_broadcast([128, 26, HD]),
                                                          op=ALU.mult), [proj, rsh], [sq])
                    P.op("pool", lambda e: e.tensor_tensor(out=sq.t[:], in0=sq.t[:], in1=g_qk.t[:], op=ALU.mult),
                         [sq, g_qk], [sq])
                    yield
                    P.op("act", lambda e: e.copy(out=qkvb.t[:, 0:26 * HD], in_=sq.t[:].rearrange("p h d -> p (h d)")),
                         [sq], [qkvb])
                    P.op("pool", lambda e: e.tensor_copy(out=qkvb.t[:, 26 * HD:PW], in_=proj.t[:, 26 * HD:PW]),
                         [proj], [qkvb])
                    cb = cos_t.t[:, i, :].unsqueeze(1).to_broadcast([128, 26, 8])
                    sn = sin_t.t[:, i, :].unsqueeze(1).to_broadcast([128, 26, 8])
                    x1 = sq.t[:, :, 0:8]
                    x2 = sq.t[:, :, 8:16]
                    P.op("dve", lambda e: e.tensor_tensor(out=r1.t[:], in0=x1, in1=cb, op=ALU.mult), [sq, cos_t], [r1])
                    P.op("dve", lambda e: e.tensor_tensor(out=r2.t[:], in0=x2, in1=sn, op=ALU.mult), [sq, sin_t], [r2])
                    P.op("pool", lambda e: e.tensor_tensor(out=r3.t[:], in0=x2, in1=cb, op=ALU.mult), [sq, cos_t], [r3])
                    P.op("pool", lambda e: e.tensor_tensor(out=r4.t[:], in0=x1, in1=sn, op=ALU.mult), [sq, sin_t], [r4])
                    yield
                    qv = qkvb.t[:, 0:26 * HD].rearrange("p (h d) -> p h d", d=HD)
                    P.op("dve", lambda e: e.tensor_tensor(out=qv[:, :, 0:8], in0=r1.t[:], in1=r2.t[:], op=ALU.subtract),
                         [r1, r2], [qkvb])
                    P.op("dve", lambda e: e.tensor_tensor(out=qv[:, :, 8:16], in0=r3.t[:], in1=r4.t[:], op=ALU.add),
                         [r3, r4], [qkvb])
                    P.dma("pool", QKV[i * 128:(i + 1) * 128, :], qkvb.t[:], reads=[qkvb])
                run_pipeline(range(NTL), bodyA)
                P.barrier()

            if "B" in phases:
              with ExitStack() as sbk:
                qrow_r = Ring([sb(sbk, "qrow%d" % i, [128, 512], BF16) for i in range(2)])
                krow_r = Ring([sb(sbk, "krow%d" % i, [128, 512], BF16) for i in range(2)])
                kdup_r = Ring([sb(sbk, "kdup%d" % i, [128, 2, 2, HD], BF16) for i in range(2)])
                vaug_r = Ring([sb(sbk, "vaug%d" % i, [128, NH, VW], BF16) for i in range(7)])
                for v in vaug_r.tiles:
                    P.op("pool", lambda e: e.memset(v.t[:], 1.0), [], [v])
                qT_r = Ring([sb(sbk, "qT%d" % i, [128, 4, 2, 128], BF16) for i in range(6)])
                for v in qT_r.tiles:
                    P.op("pool", lambda e: e.memset(v.t[:], 0.0), [], [v])
                kT_r = Ring([sb(sbk, "kT%d" % i, [128, 4, 128], BF16) for i in range(7)])
                pt_r = Ring([sb(sbk, "pt%d" % i, [128, 4, 2, 128], BF16) for i in range(6)])
                osb_r = Ring([sb(sbk, "osb%d" % i, [128, NH, 65], F32) for i in range(4)])
                pTq = ps(sbk, "pTq", [128, 4, 128], BF16)
                pTk = ps(sbk, "pTk", [128, 4, 128], BF16)
                pS_r = Ring([(ps(sbk, "pSa%d" % i, [128, 2, 2, 128], F32), ps(sbk, "pSb%d" % i, [128, 2, 2, 128], F32)) for i in range(2)])
                pO_r = Ring([ps(sbk, "pO%d" % i, [128, 4, VW], F32) for i in range(2)])
                passes = [(0, 1, r) for r in range(1)] + [(1, 4, r) for r in range(4)] + \
                         [(2, 16, r) for r in range(16)] + [(3, 1, 0)]
                if DBG['b_passes'] is not None:
                    passes = passes[:DBG['b_passes']]
                items = []
                for (pi, d, r) in passes:
                    nb = S // d // 128
                    if DBG['b_blocks'] is not None:
                        nb = min(nb, DBG['b_blocks'])
                    items += [(pi, d, r, j) for j in range(nb)]
                state = dict(prev=None)

                def bodyB(pi, d, r, j):
                    isB = (pi == 3)
                    qc0 = 512 if isB else 0
                    kc0, kw = (1536, 128) if isB else (1024, 512)
                    vc0, nvh = (2176, 2) if isB else (1664, 8)
                    rows = QKV.rearrange("(m d) c -> d m c", d=d)
                    accv = ACC.rearrange("a (m d) c -> a d m c", d=d)
                    qrow = qrow_r.next(); krow = krow_r.next(); vaug = vaug_r.next(); qT = qT_r.next()
                    kT = kT_r.next(); osb = osb_r.next()
                    prev = state['prev'] if j > 0 else None
                    state['prev'] = (kT, vaug)
                    rs_ = slice(j * 128, (j + 1) * 128)
                    P.dma("sp", qrow.t[:], rows[r, rs_, qc0:qc0 + 512], writes=[qrow])
                    P.dma("sp", krow.t[:, 0:kw], rows[r, rs_, kc0:kc0 + kw], writes=[krow])
                    P.dma("act" if DBG['act_dma'] else "sp", vaug.t[:, 0:nvh, 0:HD],
                          rows[r, rs_, vc0:vc0 + nvh * HD].rearrange("p (h d) -> p h d", d=HD), writes=[vaug])

                    def trq(e):
                        for c in range(4):
                            ins = e.transpose(pTq.t[:, c, :], qrow.t[:, c * 128:(c + 1) * 128], ident_b.t[:])
                        return ins
                    P.op("pe", trq, [qrow, ident_b], [pTq])
                    P.op("dve", lambda e: e.tensor_copy(out=qT.t[0:64, :, 0, :], in_=pTq.t[0:64, :, :]), [pTq], [qT])
                    P.op("dve", lambda e: e.tensor_copy(out=qT.t[64:128, :, 1, :], in_=pTq.t[64:128, :, :]), [pTq], [qT])
                    if isB:
                        kdup = kdup_r.next()
                        kv = krow.t[:, 0:128].rearrange("p (g d) -> p g d", d=HD)
                        P.op("pool", lambda e: e.tensor_copy(
                            out=kdup.t[:], in_=kv.unsqueeze(2).to_broadcast([128, 2, 2, HD])), [krow], [kdup])

                        def trk(e):
                            for c in range(2):
                                ins = e.transpose(pTk.t[:, c, :], kdup.t[:, c, :, :].rearrange("p a d -> p (a d)"),
                                                  ident_b.t[:])
                            return ins
                        P.op("pe", trk, [kdup, ident_b], [pTk])
                        P.op("act", lambda e: e.copy(out=kT.t[:, 0:2, :], in_=pTk.t[:, 0:2, :]), [pTk], [kT])
                    else:
                        def trk(e):
                            for c in range(4):
                                ins = e.transpose(pTk.t[:, c, :], krow.t[:, c * 128:(c + 1) * 128], ident_b.t[:])
                            return ins
                        P.op("pe", trk, [krow, ident_b], [pTk])
                        P.op("act", lambda e: e.copy(out=kT.t[:], in_=pTk.t[:]), [pTk], [kT])
                    for hg in range(2):
                        yield
                        pS = pS_r.next(); pt = pt_r.next()
                        s0 = 0 if prev is not None else 1

                        def kslice(kt, h):
                            if isB:
                                return kt.t[:, h // 4, :]
                            return kt.t[:, h // 2, :]

                        def sc(e):
                            for hh in range(4):
                                h = hg * 4 + hh
                                qs = qT.t[:, h // 2, h % 2, :]
                                pSh = pS[hh // 2]
                                if prev is not None:
                                    e.matmul(pSh.t[:, hh % 2, 0, :], lhsT=kslice(prev[0], h), rhs=qs, start=True, stop=True)
                                ins = e.matmul(pSh.t[:, hh % 2, 1, :], lhsT=kslice(kT, h), rhs=qs, start=True, stop=True)
                            return ins
                        rd = [qT, kT] + ([prev[0]] if prev is not None else [])
                        P.op("pe", sc, rd, [pS[0], pS[1]])
                        for hh2 in range(2):
                            P.op("act", lambda e: e.activation(
                                out=pt.t[:, 2 * hh2:2 * hh2 + 2, s0:2, :], in_=pS[hh2].t[:, :, s0:2, :],
                                func=AF.Exp, scale=0.125), [pS[hh2]], [pt])
                        mk = mask_b.t[:, 2 * (1 if isB else 0) + s0:2 * (1 if isB else 0) + 2, :]
                        meng = "dve" if hg == 0 else "pool"
                        P.op(meng, lambda e: e.tensor_tensor(
                            out=pt.t[:, :, s0:2, :], in0=pt.t[:, :, s0:2, :],
                            in1=mk.unsqueeze(1).to_broadcast([128, 4, 2 - s0, 128]), op=ALU.mult), [pt, mask_b], [pt])

                        yield
                        pO = pO_r.next()

                        def pv(e):
                            for hh in range(4):
                                h = hg * 4 + hh
                                vh = (h // 4) if isB else h
                                if prev is not None:
                                    e.matmul(pO.t[:, hh, :], lhsT=pt.t[:, hh, 0, :], rhs=prev[1].t[:, vh, :],
                                             start=True, stop=False)
                                ins = e.matmul(pO.t[:, hh, :], lhsT=pt.t[:, hh, 1, :], rhs=vaug.t[:, vh, :],
                                               start=(prev is None), stop=True)
                            return ins
                        rd = [pt, vaug] + ([prev[1]] if prev is not None else [])
                        P.op("pe", pv, rd, [pO])
                        if hg == 0:
                            P.op("act", lambda e: e.copy(out=osb.t[:, 0:4, :], in_=pO.t[:, :, 0:65]), [pO], [osb])
                        else:
                            P.op("dve", lambda e: e.tensor_copy(out=osb.t[:, 4:8, :], in_=pO.t[:, :, 0:65]), [pO], [osb])
                    P.dma("pool", accv[pi, r, rs_, :], osb.t[:].rearrange("p h d -> p (h d)"), reads=[osb])
                run_pipeline(items, bodyB)
                P.barrier()

            if "C" in phases:
              with ExitStack() as sc_:
                wout = sb(sc_, "wout", [128, 8, D], BF16)
                wr = sb(sc_, "wr", [128, 8, 36], F32)
                g_out = sb(sc_, "g_out", [128, D], F32)
                g_ffn = sb(sc_, "g_ffn", [128, D], F32)
                esink = sb(sc_, "esink", [128, NH], F32)
                for c in range(8):
                    P.dma("pool", wout.t[:, c, :], w_out[l, c * 128:(c + 1) * 128, :], writes=[wout])
                    P.dma("sp", wr.t[:, c, :], w_r[l, c * 128:(c + 1) * 128, :], writes=[wr])
                P.dma("sp", g_out.t[:], gvec[l, :, 1, :], writes=[g_out])
                P.dma("sp", g_ffn.t[:], gvec[l, :, 2, :], writes=[g_ffn])
                P.dma("sp", esink.t[:], sinks[l, :, :], writes=[esink])
                P.op("act", lambda e: e.activation(out=esink.t[:], in_=esink.t[:], func=AF.Exp), [esink], [esink])
                acc_rC = Ring([sb(sc_, "accC%d" % i, [128, 4, NH, 65], F32) for i in range(2)])
                xt_rC = Ring([sb(sc_, "xtC%d" % i, [128, D], F32) for i in range(3)])
                den_rC = Ring([sb(sc_, "denC%d" % i, [128, 2, NH], F32) for i in range(2)])
                o_t_rC = Ring([sb(sc_, "o_tC%d" % i, [128, 2, NH, HD], F32) for i in range(2)])
                ss2_rC = Ring([sb(sc_, "ss2C%d" % i, [128, 2], F32) for i in range(2)])
                rs2_rC = Ring([sb(sc_, "rs2C%d" % i, [128, 2], F32) for i in range(2)])
                tm2_rC = Ring([sb(sc_, "tm2C%d" % i, [128, 2], F32) for i in range(2)])
                mix_rC = Ring([sb(sc_, "mixC%d" % i, [128, D], BF16) for i in range(3)])
                mT_rC = Ring([sb(sc_, "mTC%d" % i, [128, 8, 128], BF16) for i in range(2)])
                xm_rC = Ring([sb(sc_, "xmC%d" % i, [128, D], F32) for i in range(2)])
                ss3_rC = Ring([sb(sc_, "ss3C%d" % i, [128, 1], F32) for i in range(2)])
                rs3_rC = Ring([sb(sc_, "rs3C%d" % i, [128, 1], F32) for i in range(2)])
                tm3_rC = Ring([sb(sc_, "tm3C%d" % i, [128, 1], F32) for i in range(2)])
                h2_rC = Ring([sb(sc_, "h2C%d" % i, [128, D], F32) for i in range(3)])
                h2Tf_rC = Ring([sb(sc_, "h2TfC%d" % i, [128, 8, 128], F32) for i in range(2)])
                h2Tb_rC = Ring([sb(sc_, "h2TbC%d" % i, [128, 8, 128], BF16) for i in range(2)])
                junk = sb(sc_, "junkC", [128, D], BF16)
                LG = sb(sc_, "LG", [128, NT, 36], F32)
                gmax = sb(sc_, "gmax", [128, NT], F32)
                gtop = sb(sc_, "gtop", [128, NT], F32)
                m1 = sb(sc_, "m1", [128, NT], F32)
                m2 = sb(sc_, "m2", [128, NT], F32)
                ex4 = sb(sc_, "ex4", [128, NT, 4], F32)
                gm = sb(sc_, "gm", [128, NT, 4], F32)
                lem = sb(sc_, "lem", [128, NT, 32], F32)
                m1k = sb(sc_, "m1k", [128, NT, 32], F32)
                m2k = sb(sc_, "m2k", [128, NT, 32], F32)
                pTm = ps(sc_, "pTm", [128, 8, 128], BF16)
                pOa = ps(sc_, "pOa", [128, 512], F32)
                pOb = ps(sc_, "pOb", [128, 512], F32)
                pTh = [ps(sc_, "pTh%d" % i, [128, 4, 128], F32) for i in range(2)]
                pR = ps(sc_, "pR", [128, 36], F32)
                def bodyC(i):
                    acc = acc_rC.next()
                    xt = xt_rC.next()
                    den = den_rC.next()
                    o_t = o_t_rC.next()
                    ss2 = ss2_rC.next()
                    rs2 = rs2_rC.next()
                    tm2 = tm2_rC.next()
                    mix = mix_rC.next()
                    mT = mT_rC.next()
                    xm = xm_rC.next()
                    ss3 = ss3_rC.next()
                    rs3 = rs3_rC.next()
                    tm3 = tm3_rC.next()
                    h2 = h2_rC.next()
                    h2Tf = h2Tf_rC.next()
                    h2Tb = h2Tb_rC.next()
                    rs_ = slice(i * 128, (i + 1) * 128)
                    for a in range(4):
                        P.dma("sp", acc.t[:, a, :, :].rearrange("p h d -> p (h d)"),
                              ACC[a, rs_, :], writes=[acc])
                    P.dma("sp", xt.t[:], xsrc[rs_, :], writes=[xt])
                    P.op("dve", lambda e: e.tensor_tensor(out=acc.t[:, 0], in0=acc.t[:, 0], in1=acc.t[:, 1], op=ALU.add),
                         [acc], [acc])
                    P.op("dve", lambda e: e.tensor_tensor(out=acc.t[:, 0], in0=acc.t[:, 0], in1=acc.t[:, 2], op=ALU.add),
                         [acc], [acc])
                    P.op("dve", lambda e: e.tensor_copy(out=den.t[:, 0, :], in_=acc.t[:, 0, :, 64]), [acc], [den])
                    P.op("dve", lambda e: e.tensor_tensor(out=den.t[:, 1, :], in0=acc.t[:, 3, :, 64], in1=esink.t[:],
                                                          op=ALU.add), [acc, esink], [den])
                    P.op("dve", lambda e: e.reciprocal(out=den.t[:], in_=den.t[:]), [den], [den])
                    P.op("dve", lambda e: e.tensor_tensor(
                        out=o_t.t[:, 0], in0=acc.t[:, 0, :, 0:HD],
                        in1=den.t[:, 0, :].unsqueeze(2).to_broadcast([128, NH, HD]), op=ALU.mult), [acc, den], [o_t])
                    P.op("pool", lambda e: e.tensor_tensor(
                        out=o_t.t[:, 1], in0=acc.t[:, 3, :, 0:HD],
                        in1=den.t[:, 1, :].unsqueeze(2).to_broadcast([128, NH, HD]), op=ALU.mult), [acc, den], [o_t])
                    of = o_t.t[:].rearrange("p a h d -> p (a h d)")
                    for a in range(2):
                        P.op("act", lambda e: e.activation(out=junk.t[:, a * 512:(a + 1) * 512],
                                                           in_=of[:, a * 512:(a + 1) * 512], func=AF.Square,
                                                           accum_out=ss2.t[:, a:a + 1]), [o_t], [junk, ss2])
                    rstd_from_ss(ss2.t[:], 512, rs2, tm2, [ss2])
                    for a in range(2):
                        P.op("dve", lambda e: e.scalar_tensor_tensor(
                            out=mix.t[:, a * 512:(a + 1) * 512], in0=of[:, a * 512:(a + 1) * 512],
                            scalar=rs2.t[:, a:a + 1], in1=g_out.t[:, a * 512:(a + 1) * 512],
                            op0=ALU.mult, op1=ALU.mult), [o_t, rs2, g_out], [mix])

                    yield

                    def trm(e):
                        for c in range(8):
                            ins = e.transpose(pTm.t[:, c, :], mix.t[:, c * 128:(c + 1) * 128], ident_b.t[:])
                        return ins
                    P.op("pe", trm, [mix, ident_b], [pTm])
                    P.op("act", lambda e: e.copy(out=mT.t[:], in_=pTm.t[:]), [pTm], [mT])
                    for hf, pO in ((0, pOa), (1, pOb)):
                        def mmo(e):
                            for c in range(8):
                                ins = e.matmul(pO.t[:], lhsT=mT.t[:, c, :], rhs=wout.t[:, c, hf * 512:(hf + 1) * 512],
                                               start=(c == 0), stop=(c == 7))
                            return ins
                        P.op("pe", mmo, [mT, wout], [pO])
                        P.op("dve", lambda e: e.tensor_tensor(out=xm.t[:, hf * 512:(hf + 1) * 512],
                                                              in0=xt.t[:, hf * 512:(hf + 1) * 512], in1=pO.t[:],
                                                              op=ALU.add), [xt, pO], [xm])
                    P.dma("pool", XM[rs_, :], xm.t[:], reads=[xm])
                    P.op("act", lambda e: e.activation(out=junk.t[:], in_=xm.t[:], func=AF.Square, accum_out=ss3.t[:]),
                         [xm], [junk, ss3])
                    rstd_from_ss(ss3.t[:], D, rs3, tm3, [ss3])
                    P.op("dve", lambda e: e.scalar_tensor_tensor(out=h2.t[:], in0=xm.t[:], scalar=rs3.t[:, 0:1],
                                                                 in1=g_ffn.t[:], op0=ALU.mult, op1=ALU.mult),
                         [xm, rs3, g_ffn], [h2])
                    yield
                    for hf in range(2):
                        def trh(e):
                            for c in range(4):
                                cc = hf * 4 + c
                                ins = e.transpose(pTh[hf].t[:, c, :], h2.t[:, cc * 128:(cc + 1) * 128], ident_f.t[:])
                            return ins
                        P.op("pe", trh, [h2, ident_f], [pTh[hf]])
                        P.op("act", lambda e: e.copy(out=h2Tf.t[:, hf * 4:(hf + 1) * 4, :], in_=pTh[hf].t[:]),
                             [pTh[hf]], [h2Tf])
                        P.op("dve", lambda e: e.tensor_copy(out=h2Tb.t[:, hf * 4:(hf + 1) * 4, :], in_=pTh[hf].t[:]),
                             [pTh[hf]], [h2Tb])
                    P.dma("pool", H2T.rearrange("(c p) t -> p c t", p=128)[:, :, rs_], h2Tb.t[:], reads=[h2Tb])


                    def mmr(e):
                        for c in range(8):
                            ins = e.matmul(pR.t[:], lhsT=h2Tf.t[:, c, :], rhs=wr.t[:, c, :], start=(c == 0), stop=(c == 7))
                        return ins
                    P.op("pe", mmr, [h2Tf, wr], [pR])
                    P.op("act", lambda e: e.copy(out=LG.t[:, i, :], in_=pR.t[:]), [pR], [LG])
                run_pipeline(range(NTL), bodyC)
                BIG = 1e30
                NTc = NTL
                lgG = LG.t[:, 0:NTc, 0:4]
                bc4 = lambda t_: t_.t[:, 0:NTc].unsqueeze(2).to_broadcast([128, NTc, 4])
                bc32 = lambda t_: t_.t[:, 0:NTc].unsqueeze(2).to_broadcast([128, NTc, 32])
                P.op("dve", lambda e: e.tensor_reduce(out=gmax.t[:, 0:NTc], in_=lgG, axis=AX.X, op=ALU.max), [LG], [gmax])
                P.op("dve", lambda e: e.tensor_tensor(out=ex4.t[:, 0:NTc, :], in0=lgG, in1=bc4(gmax), op=ALU.subtract),
                     [LG, gmax], [ex4])
                P.op("act", lambda e: e.activation(out=ex4.t[:, 0:NTc, :], in_=ex4.t[:, 0:NTc, :], func=AF.Exp), [ex4], [ex4])
                P.op("dve", lambda e: e.tensor_reduce(out=gtop.t[:, 0:NTc], in_=ex4.t[:, 0:NTc, :], axis=AX.X, op=ALU.add),
                     [ex4], [gtop])
                P.op("dve", lambda e: e.reciprocal(out=gtop.t[:, 0:NTc], in_=gtop.t[:, 0:NTc]), [gtop], [gtop])
                P.op("dve", lambda e: e.tensor_tensor(out=gm.t[:, 0:NTc, :], in0=lgG, in1=bc4(gmax), op=ALU.is_lt),
                     [LG, gmax], [gm])
                P.op("dve", lambda e: e.tensor_scalar(out=gm.t[:, 0:NTc, :], in0=gm.t[:, 0:NTc, :], scalar1=-BIG, scalar2=None,
                                                      op0=ALU.mult), [gm], [gm])
                P.op("dve", lambda e: e.tensor_tensor(
                    out=lem.t[:, 0:NTc, :].rearrange("p t (g e) -> p t g e", e=8),
                    in0=LG.t[:, 0:NTc, 4:36].rearrange("p t (g e) -> p t g e", e=8),
                    in1=gm.t[:, 0:NTc, :].unsqueeze(3).to_broadcast([128, NTc, 4, 8]), op=ALU.add), [LG, gm], [lem])
                lv = lem.t[:, 0:NTc, :]
                P.op("dve", lambda e: e.tensor_reduce(out=m1.t[:, 0:NTc], in_=lv, axis=AX.X, op=ALU.max), [lem], [m1])
                P.op("dve", lambda e: e.tensor_tensor(out=m1k.t[:, 0:NTc, :], in0=lv, in1=bc32(m1), op=ALU.is_ge),
                     [lem, m1], [m1k])
                P.op("dve", lambda e: e.tensor_scalar(out=m2k.t[:, 0:NTc, :], in0=m1k.t[:, 0:NTc, :], scalar1=-BIG, scalar2=None,
                                                      op0=ALU.mult), [m1k], [m2k])
                P.op("dve", lambda e: e.tensor_tensor(out=lv, in0=lv, in1=m2k.t[:, 0:NTc, :], op=ALU.add), [lem, m2k], [lem])
                P.op("dve", lambda e: e.tensor_reduce(out=m2.t[:, 0:NTc], in_=lv, axis=AX.X, op=ALU.max), [lem], [m2])
                P.op("dve", lambda e: e.tensor_tensor(out=m2k.t[:, 0:NTc, :], in0=lv, in1=bc32(m2), op=ALU.is_ge),
                     [lem, m2], [m2k])
                P.op("dve", lambda e: e.tensor_tensor(out=m1.t[:, 0:NTc], in0=m1.t[:, 0:NTc], in1=m2.t[:, 0:NTc],
                                                      op=ALU.subtract), [m1, m2], [m1])
                P.op("act", lambda e: e.activation(out=m1.t[:, 0:NTc], in_=m1.t[:, 0:NTc], func=AF.Exp, scale=-1.0), [m1], [m1])
                P.op("dve", lambda e: e.tensor_scalar(out=m1.t[:, 0:NTc], in0=m1.t[:, 0:NTc], scalar1=1.0, scalar2=None,
                                                      op0=ALU.add), [m1], [m1])
                P.op("dve", lambda e: e.reciprocal(out=m1.t[:, 0:NTc], in_=m1.t[:, 0:NTc]), [m1], [m1])
                P.op("dve", lambda e: e.tensor_tensor(out=m1.t[:, 0:NTc], in0=m1.t[:, 0:NTc], in1=gtop.t[:, 0:NTc],
                                                      op=ALU.mult), [m1, gtop], [m1])
                P.op("dve", lambda e: e.tensor_tensor(out=m2.t[:, 0:NTc], in0=gtop.t[:, 0:NTc], in1=m1.t[:, 0:NTc],
                                                      op=ALU.subtract), [gtop, m1], [m2])
                P.op("dve", lambda e: e.tensor_tensor(out=m1k.t[:, 0:NTc, :], in0=m1k.t[:, 0:NTc, :], in1=bc32(m1),
                                                      op=ALU.mult), [m1k, m1], [m1k])
                P.op("dve", lambda e: e.tensor_tensor(out=m2k.t[:, 0:NTc, :], in0=m2k.t[:, 0:NTc, :], in1=bc32(m2),
                                                      op=ALU.mult), [m2k, m2], [m2k])
                P.op("dve", lambda e: e.tensor_tensor(out=m1k.t[:, 0:NTc, :], in0=m1k.t[:, 0:NTc, :], in1=m2k.t[:, 0:NTc, :],
                                                      op=ALU.add), [m1k, m2k], [m1k])
                P.dma("sp", GS[0:NTc * 128, :].rearrange("(i p) e -> p i e", p=128), m1k.t[:, 0:NTc, :], reads=[m1k])
                P.barrier()

            if "D" in phases:
              with ExitStack() as sd:
                NTS = TS // 128
                hT = sb(sd, "hT", [128, 8, TS], BF16)
                Gt = sb(sd, "Gt", [128, NTS, NE], F32)
                yacc = sb(sd, "yacc", [128, NTS, D], F32)
                wgu_r = Ring([sb(sd, "wgu%d" % i, [128, 8, 2 * FE], BF16) for i in range(2)])
                wdn_r = Ring([sb(sd, "wdn%d" % i, [128, 4, D], BF16) for i in range(2)])
                sg_r = Ring([sb(sd, "sg%d" % i, [128, 512], F32) for i in range(2)])
                aT_r = Ring([sb(sd, "aT%d" % i, [128, 4, 512], BF16) for i in range(2)])
                xm_r = Ring([sb(sd, "xmD%d" % i, [128, D], F32) for i in range(2)])
                pg_r = Ring([ps(sd, "pg%d" % i, [128, 512], F32) for i in range(2)])
                pu_r = Ring([ps(sd, "pu%d" % i, [128, 512], F32) for i in range(2)])
                py_r = Ring([(ps(sd, "pya%d" % i, [128, 512], F32), ps(sd, "pyb%d" % i, [128, 512], F32))
                             for i in range(2)])
                for s_ in range(S // TS):
                    t0 = s_ * TS
                    P.dma("sp", hT.t[:], H2T.rearrange("(c p) t -> p c t", p=128)[:, :, t0:t0 + TS], writes=[hT])
                    P.dma("sp", Gt.t[:], GS[t0:t0 + TS, :].rearrange("(i p) e -> p i e", p=128), writes=[Gt])
                    P.op("dve", lambda e: e.memset(yacc.t[:], 0.0), [], [yacc])
                    for ex in range(n_exp):
                        wgu = wgu_r.next(); wdn = wdn_r.next()
                        for hf in range(2):
                            P.dma("pool", wgu.t[:, hf * 4:(hf + 1) * 4, :],
                                  w_gu[l, ex, hf * 512:(hf + 1) * 512, :].rearrange("(c p) n -> p c n", p=128),
                                  writes=[wgu])
                        P.dma("pool", wdn.t[:], w_dn[l, ex, :, :].rearrange("(c p) n -> p c n", p=128), writes=[wdn])
                        for g in range(TS // 512):
                            aT = aT_r.next()
                            tk = slice(g * 512, (g + 1) * 512)
                            for c in range(4):
                                pg = pg_r.next(); pu = pu_r.next(); sg = sg_r.next()

                                def mmg(e):
                                    for k in range(8):
                                        ins = e.matmul(pg.t[:], lhsT=wgu.t[:, k, c * 128:(c + 1) * 128], rhs=hT.t[:, k, tk],
                                                       start=(k == 0), stop=(k == 7))
                                    return ins

                                def mmu(e):
                                    for k in range(8):
                                        ins = e.matmul(pu.t[:], lhsT=wgu.t[:, k, FE + c * 128:FE + (c + 1) * 128],
                                                       rhs=hT.t[:, k, tk], start=(k == 0), stop=(k == 7))
                                    return ins
                                P.op("pe", mmg, [wgu, hT], [pg])
                                P.op("pe", mmu, [wgu, hT], [pu])
                                P.op("act", lambda e: e.activation(out=sg.t[:], in_=pg.t[:], func=AF.Silu), [pg], [sg])
                                P.op("dve", lambda e: e.tensor_tensor(out=aT.t[:, c, :], in0=sg.t[:], in1=pu.t[:],
                                                                      op=ALU.mult), [sg, pu], [aT])
                            for t in range(4):
                                ti = g * 4 + t
                                pya, pyb = py_r.next()
                                for hf, py in ((0, pya), (1, pyb)):
                                    def mmd(e):
                                        for c in range(4):
                                            ins = e.matmul(py.t[:], lhsT=aT.t[:, c, t * 128:(t + 1) * 128],
                                                           rhs=wdn.t[:, c, hf * 512:(hf + 1) * 512],
                                                           start=(c == 0), stop=(c == 3))
                                        return ins
                                    P.op("pe", mmd, [aT, wdn], [py])
                                    P.op("dve", lambda e: e.scalar_tensor_tensor(
                                        out=yacc.t[:, ti, hf * 512:(hf + 1) * 512], in0=py.t[:],
                                        scalar=Gt.t[:, ti, ex:ex + 1], in1=yacc.t[:, ti, hf * 512:(hf + 1) * 512],
                                        op0=ALU.mult, op1=ALU.add), [py, Gt, yacc], [yacc])
                    for ti in range(NTS):
                        xm = xm_r.next()
                        rs_ = slice(t0 + ti * 128, t0 + (ti + 1) * 128)
                        P.dma("sp", xm.t[:], XM[rs_, :], writes=[xm])
                        P.op("dve", lambda e: e.tensor_tensor(out=xm.t[:], in0=xm.t[:], in1=yacc.t[:, ti, :], op=ALU.add),
                             [xm, yacc], [xm])
                        P.dma("sp", xdst[rs_, :], xm.t[:], reads=[xm])
                P.barrier()
        P.barrier()
    return nc


def _consts():
    k = np.arange(128)[:, None]
    q = np.arange(128)[None, :]
    mask = np.stack([(k >= q), (k <= q), (k > q), (k <= q)], axis=1).astype(np.float32)
    ident = np.eye(128, dtype=np.float32)
    invf = (500000.0 ** (-np.arange(0, 16, 2, dtype=np.float32) / np.float32(16))).astype(np.float32)
    invf = np.ascontiguousarray(np.broadcast_to(invf[None, :], (128, 8)))
    return ident, np.ascontiguousarray(mask), invf


def prep_shared(inputs, L):
    f = lambda k: np.asarray(inputs[k], dtype=np.float32)
    rep = lambda v: np.broadcast_to(v[:, None, :], (L, 128, v.shape[-1]))
    gvec = np.stack([rep(f("attn_norm")), rep(np.concatenate([f("out_norm_a"), f("out_norm_b")], axis=-1)),
                     rep(f("ffn_norm"))], axis=2)
    qa, ka, qb, kb = f("q_norm_a"), f("k_norm_a"), f("q_norm_b"), f("k_norm_b")
    gq = np.concatenate([np.tile(qa, (1, 8)), np.tile(qb, (1, 8)), np.tile(ka, (1, 8)), np.tile(kb, (1, 2))], axis=-1)
    w = f("w_in")
    w_in_p = np.concatenate([w[:, :, 0:512], w[:, :, 1536:2048], w[:, :, 512:1024], w[:, :, 2048:2176],
                             w[:, :, 1024:1536], w[:, :, 2176:2304]], axis=-1)
    ident, mask, invf = _consts()
    return dict(
        c_ident=ident, c_mask=mask, c_invf=invf,
        gvec=np.ascontiguousarray(gvec), gqk=np.ascontiguousarray(rep(gq)),
        sinks=np.ascontiguousarray(rep(f("sinks_b"))),
        w_in=np.ascontiguousarray(w_in_p), w_out=f("w_out"),
        w_r=np.ascontiguousarray(np.concatenate([f("w_router_group"), f("w_router_expert")], axis=-1)),
        w_gu=f("w_gate_up"), w_dn=f("w_down"))


def core_map(shared, x_b, pos_b):
    S = x_b.shape[0]
    m = dict(shared)
    m["x"] = np.ascontiguousarray(x_b, dtype=np.float32)
    m["pos_t"] = np.ascontiguousarray(np.asarray(pos_b, dtype=np.int32).reshape(S // 128, 128).T)
    return m


def kernel(**inputs):
    x = np.asarray(inputs["x"])
    pos = np.asarray(inputs["positions"])
    B, S, _ = x.shape
    L = np.asarray(inputs["attn_norm"]).shape[0]
    shared = prep_shared(inputs, L)
    nc = build(S, L)
    in_maps = [core_map(shared, x[b], pos[b]) for b in range(B)]
    res = run_bass_kernel_spmd(nc, in_maps, core_ids=list(range(B)))
    return np.stack([np.asarray(r["out"]).reshape(S, D) for r in res.results], axis=0).astype(np.float32)
```
